# Optimizing a Trainium2 kernel written in Bass

```python
import jax, jax.numpy as jnp
from jax import lax
import numpy as np

D_MODEL = 1024
BATCH = 16
SEQ = 2048
DEPTH = 4

HEAD_DIM = 64
ATT_WIDTH = D_MODEL // 2
CONV_WIDTH = D_MODEL - ATT_WIDTH
N_ATT_HEADS = ATT_WIDTH // HEAD_DIM
N_KV_GROUPS = 2
HEADS_PER_GROUP = N_ATT_HEADS // N_KV_GROUPS
CONV_K = 3
CMP_BLOCK = 32
CMP_STRIDE = 16
SEL_BLOCK = 64
N_SEL = 8
WINDOW = 512
Q_BLOCK = 128
D_FF = 2816
N_SUB = 3
EPS = 1e-6
NEG_INF = -1e30
FORCE_BONUS = 1e4
Q_COLS = N_ATT_HEADS * HEAD_DIM
KV_COLS = N_KV_GROUPS * HEAD_DIM
GATE_COLS = 3 * N_ATT_HEADS
IN_COLS = Q_COLS + 6 * KV_COLS + GATE_COLS + 3 * CONV_WIDTH

kernel_name = 'hymba_nsa_shortconv_macaron_adaln'


def rms_norm(x, g):
    xf = x.astype(jnp.float32)
    y = xf * lax.rsqrt(jnp.mean(xf * xf, axis=-1, keepdims=True) + EPS)
    return (y * g.astype(jnp.float32)).astype(x.dtype)


def pre_norm(x, g, shift, scale):
    return rms_norm(x, g) * (1 + scale[:, None, :]) + shift[:, None, :]


def swiglu(h, w_in, w_out):
    gate, up = jnp.split(h @ w_in, 2, axis=-1)
    return (jax.nn.silu(gate) * up) @ w_out


def masked_softmax(s, mask):
    s = jnp.where(mask, s.astype(jnp.float32), NEG_INF)
    return jax.nn.softmax(s, axis=-1)


def compress_blocks(k, pos, w1, w2):
    b, s, g, dh = k.shape
    nc = (s - CMP_BLOCK) // CMP_STRIDE + 1
    idx = jnp.arange(nc)[:, None] * CMP_STRIDE + jnp.arange(CMP_BLOCK)[None, :]
    blk = k[:, idx] + pos[None, None, :, None, :]
    flat = blk.transpose(0, 1, 3, 2, 4).reshape(b, nc, g, CMP_BLOCK * dh)
    return jax.nn.silu(flat @ w1) @ w2


def nsa_attention(q, kc, vc, ks, vs, kw, vw, gates, cmp_pos, w_cmp1, w_cmp2):
    b, s = q.shape[:2]
    q = q.reshape(b, s, N_KV_GROUPS, HEADS_PER_GROUP, HEAD_DIM) * (HEAD_DIM ** -0.5)
    t = jnp.arange(s)

    kcmp = compress_blocks(kc, cmp_pos[0], w_cmp1[0], w_cmp2[0])
    vcmp = compress_blocks(vc, cmp_pos[1], w_cmp1[1], w_cmp2[1])
    nc = kcmp.shape[1]
    blk_end = jnp.arange(nc) * CMP_STRIDE + CMP_BLOCK - 1
    cmp_mask = blk_end[None, :] <= t[:, None]
    p_cmp = masked_softmax(jnp.einsum('bsghd,bngd->bghsn', q, kcmp), cmp_mask)
    p_cmp = jnp.where(cmp_mask, p_cmp, 0.0)
    o_cmp = jnp.einsum('bghsn,bngd->bsghd', p_cmp.astype(vcmp.dtype), vcmp)

    nb = s // SEL_BLOCK
    cs = jnp.arange(nc) * CMP_STRIDE
    js = jnp.arange(nb) * SEL_BLOCK
    overlap = ((cs[:, None] < js[None, :] + SEL_BLOCK) &
               (cs[:, None] + CMP_BLOCK > js[None, :])).astype(jnp.float32)
    imp = jnp.einsum('bghsn,nj->bgsj', p_cmp, overlap)
    cur = t // SEL_BLOCK
    jb = jnp.arange(nb)
    blk_ok = jb[None, :] <= cur[:, None]
    forced = ((jb[None, :] == 0) | (jb[None, :] == cur[:, None]) |
              (jb[None, :] == cur[:, None] - 1)).astype(jnp.float32)
    imp = jnp.where(blk_ok, imp + FORCE_BONUS * forced, NEG_INF)
    n_sel = min(N_SEL, nb)
    top_val, top_idx = lax.top_k(imp, n_sel)
    top_ok = top_val > 0.5 * NEG_INF

    ks_blk = ks.reshape(b, nb, SEL_BLOCK, N_KV_GROUPS, HEAD_DIM).transpose(0, 3, 1, 2, 4)
    vs_blk = vs.reshape(b, nb, SEL_BLOCK, N_KV_GROUPS, HEAD_DIM).transpose(0, 3, 1, 2, 4)
    gather = jax.vmap(jax.vmap(lambda blocks, ids: blocks[ids]))
    kw_pad = jnp.pad(kw, ((0, 0), (WINDOW, 0), (0, 0), (0, 0)))
    vw_pad = jnp.pad(vw, ((0, 0), (WINDOW, 0), (0, 0), (0, 0)))
    n_keys = n_sel * SEL_BLOCK

    def query_block(i):
        t0 = i * Q_BLOCK
        tq = t0 + jnp.arange(Q_BLOCK)
        qc = lax.dynamic_slice_in_dim(q, t0, Q_BLOCK, axis=1)
        ic = lax.dynamic_slice_in_dim(top_idx, t0, Q_BLOCK, axis=2)
        okc = lax.dynamic_slice_in_dim(top_ok, t0, Q_BLOCK, axis=2)
        kg = gather(ks_blk, ic).reshape(b, N_KV_GROUPS, Q_BLOCK, n_keys, HEAD_DIM)
        vg = gather(vs_blk, ic).reshape(b, N_KV_GROUPS, Q_BLOCK, n_keys, HEAD_DIM)
        kpos = ic[..., None] * SEL_BLOCK + jnp.arange(SEL_BLOCK)
        sel_mask = ((kpos <= tq[:, None, None]) & okc[..., None]).reshape(
            b, N_KV_GROUPS, 1, Q_BLOCK, n_keys)
        p = masked_softmax(jnp.einsum('btghd,bgtkd->bghtk', qc, kg), sel_mask)
        o_sel = jnp.einsum('bghtk,bgtkd->btghd', p.astype(vg.dtype), vg)
        kwc = lax.dynamic_slice_in_dim(kw_pad, t0, Q_BLOCK + WINDOW, axis=1)
        vwc = lax.dynamic_slice_in_dim(vw_pad, t0, Q_BLOCK + WINDOW, axis=1)
        wpos = t0 - WINDOW + jnp.arange(Q_BLOCK + WINDOW)
        win_mask = ((wpos[None, :] <= tq[:, None]) & (wpos[None, :] > tq[:, None] - WINDOW) &
                    (wpos[None, :] >= 0))
        p = masked_softmax(jnp.einsum('btghd,bkgd->bghtk', qc, kwc), win_mask)
        o_win = jnp.einsum('bghtk,bkgd->btghd', p.astype(vwc.dtype), vwc)
        return o_sel, o_win

    o_sel, o_win = lax.map(query_block, jnp.arange(s // Q_BLOCK))

    def unblock(o):
        return o.transpose(1, 0, 2, 3, 4, 5).reshape(b, s, N_KV_GROUPS, HEADS_PER_GROUP, HEAD_DIM)

    g = jax.nn.sigmoid(gates.astype(jnp.float32)).astype(q.dtype).reshape(
        b, s, N_KV_GROUPS, HEADS_PER_GROUP, 3)
    o = g[..., 0:1] * o_cmp + g[..., 1:2] * unblock(o_sel) + g[..., 2:3] * unblock(o_win)
    return o.reshape(b, s, ATT_WIDTH)


def short_conv(h, b_gate, c_gate, conv_w, conv_b):
    u = c_gate * h
    s = u.shape[1]
    up = jnp.pad(u, ((0, 0), (CONV_K - 1, 0), (0, 0)))
    v = conv_b
    for k in range(CONV_K):
        v = v + up[:, k:k + s] * conv_w[k]
    return b_gate * v


def hybrid_mixer(h, w_in, cmp_pos, w_cmp1, w_cmp2, conv_w, conv_b, g_out, w_out):
    b, s, _ = h.shape
    z = h @ w_in
    sizes = [Q_COLS] + [KV_COLS] * 6 + [GATE_COLS] + [CONV_WIDTH] * 3
    offs = np.cumsum(sizes)[:-1].tolist()
    q, kc, vc, ks, vs, kw, vw, gates, hc, bg, cg = jnp.split(z, offs, axis=-1)

    def kv(a):
        return a.reshape(b, s, N_KV_GROUPS, HEAD_DIM)

    o_att = nsa_attention(q.reshape(b, s, N_ATT_HEADS, HEAD_DIM), kv(kc), kv(vc), kv(ks), kv(vs),
                          kv(kw), kv(vw), gates.reshape(b, s, N_ATT_HEADS, 3),
                          cmp_pos, w_cmp1, w_cmp2)
    o_conv = short_conv(hc, bg, cg, conv_w, conv_b)
    o = jnp.concatenate([rms_norm(o_att, g_out[:ATT_WIDTH]),
                         rms_norm(o_conv, g_out[ATT_WIDTH:])], axis=-1)
    return o @ w_out


def setup_inputs(seed: int = 0) -> dict:
    key = jax.random.key(seed)
    ks = jax.random.split(key, 16)

    def nrm(k, shape, s):
        return jax.random.normal(k, shape, jnp.float32) * s

    return {
        'x': nrm(ks[0], (BATCH, SEQ, D_MODEL), 1.0),
        'c': nrm(ks[1], (BATCH, D_MODEL), 1.0),
        'w_ada': nrm(ks[2], (DEPTH, D_MODEL, N_SUB * 3 * D_MODEL), D_MODEL ** -0.5),
        'b_ada': nrm(ks[3], (DEPTH, N_SUB * 3 * D_MODEL), 0.01),
        'g_norm': 1.0 + nrm(ks[4], (DEPTH, N_SUB, D_MODEL), 0.02),
        'w_ff_in': nrm(ks[5], (DEPTH, 2, D_MODEL, 2 * D_FF), D_MODEL ** -0.5),
        'w_ff_out': nrm(ks[6], (DEPTH, 2, D_FF, D_MODEL), D_FF ** -0.5),
        'w_mix_in': nrm(ks[7], (DEPTH, D_MODEL, IN_COLS), D_MODEL ** -0.5),
        'cmp_pos': nrm(ks[8], (DEPTH, 2, CMP_BLOCK, HEAD_DIM), 0.1),
        'w_cmp1': nrm(ks[9], (DEPTH, 2, CMP_BLOCK * HEAD_DIM, HEAD_DIM), (CMP_BLOCK * HEAD_DIM) ** -0.5),
        'w_cmp2': nrm(ks[10], (DEPTH, 2, HEAD_DIM, HEAD_DIM), HEAD_DIM ** -0.5),
        'conv_w': nrm(ks[11], (DEPTH, CONV_K, CONV_WIDTH), CONV_K ** -0.5),
        'conv_b': nrm(ks[12], (DEPTH, CONV_WIDTH), 0.01),
        'g_mix_out': 1.0 + nrm(ks[13], (DEPTH, D_MODEL), 0.02),
        'w_mix_out': nrm(ks[14], (DEPTH, D_MODEL, D_MODEL), D_MODEL ** -0.5),
        'g_final': 1.0 + nrm(ks[15], (D_MODEL,), 0.02),
    }


def reference(x, c, w_ada, b_ada, g_norm, w_ff_in, w_ff_out, w_mix_in, cmp_pos, w_cmp1, w_cmp2,
              conv_w, conv_b, g_mix_out, w_mix_out, g_final):
    bsz = c.shape[0]
    c_act = jax.nn.silu(c)
    for l in range(DEPTH):
        mod = (c_act @ w_ada[l] + b_ada[l]).reshape(bsz, N_SUB, 3, D_MODEL)
        h = pre_norm(x, g_norm[l, 0], mod[:, 0, 0], mod[:, 0, 1])
        x = x + 0.5 * mod[:, 0, 2][:, None, :] * swiglu(h, w_ff_in[l, 0], w_ff_out[l, 0])
        h = pre_norm(x, g_norm[l, 1], mod[:, 1, 0], mod[:, 1, 1])
        x = x + mod[:, 1, 2][:, None, :] * hybrid_mixer(
            h, w_mix_in[l], cmp_pos[l], w_cmp1[l], w_cmp2[l], conv_w[l], conv_b[l],
            g_mix_out[l], w_mix_out[l])
        h = pre_norm(x, g_norm[l, 2], mod[:, 2, 0], mod[:, 2, 1])
        x = x + 0.5 * mod[:, 2, 2][:, None, :] * swiglu(h, w_ff_in[l, 1], w_ff_out[l, 1])
    return rms_norm(x, g_final)
```

```python
import numpy as np
import concourse.bass as bass
import concourse.mybir as mybir
from concourse.bass_utils import run_bass_kernel_spmd
from contextlib import ExitStack

F32 = mybir.dt.float32
BF16 = mybir.dt.bfloat16
AF = mybir.ActivationFunctionType
ALU = mybir.AluOpType

D = 1024
SEQ = 2048
DFF = 2816
NCORES = 8
EPS = 1e-6
NEG = -30000.0
STRICT_WAR = True
CMPW = 2 * 2048 + 2 * 34 + 64 + 64
PF_G = 0


def pf_offsets(L, NSEQ):
    o = {}
    c = 0
    for name, n in (("g", L * 24), ("b", L * 72), ("cw", L * 12), ("cb", L * 4), ("gmo", L * 8), ("gf", 8),
                    ("c", 8 * NSEQ), ("eps", 1), ("fb", 512)):
        o[name] = c
        c += n
    o["_n"] = c
    return o


CB_ID, CB_T0, CB_T1, CB_ONE, CB_EP, CB_BC, CB_OV, CB_N = 0, 128, 256, 384, 512, 2560, 4608, 4640


class Res:
    __slots__ = ("name", "w", "r", "excl")

    def __init__(self, name, excl=False):
        self.name = name
        self.w = None
        self.r = {}
        self.excl = excl


def fence(news, olds):
    acc = {}
    for o in olds:
        if o.w is not None:
            k, v, src = o.w
            if acc.get(k, (0, None))[0] < v:
                acc[k] = (v, src)
        for k, (v, src) in o.r.items():
            if acc.get(k, (0, None))[0] < v:
                acc[k] = (v, src)
    for n in news:
        for k, (v, src) in acc.items():
            if n.r.get(k, (0, None))[0] < v:
                n.r[k] = (v, src)


class Sched:
    ENGS = ("pe", "act", "dve", "pool", "sp")

    def __init__(self, nc, es):
        self.nc = nc
        self.es = es
        self.sems = {}
        self.ops = {n: [] for n in self.ENGS}
        self.cnt = {n: 0 for n in self.ENGS}
        self.seen = {n: {} for n in self.ENGS}
        for n in self.ENGS:
            self.sems[n] = es.enter_context(nc.semaphore("s_" + n))
        self.dma_tot = {}

    def _dma_sem(self, key):
        k = ("dma", key)
        if k not in self.sems:
            self.sems[k] = self.es.enter_context(self.nc.semaphore("d_%d" % len(self.sems)))
            self.dma_tot[k] = 0
        return k

    def _deps(self, en, reads, writes):
        need = {}

        def add(k, v, src, kind):
            if src == en:
                if en == "pe" or (kind == "r" and not STRICT_WAR):
                    return
            if k in self.dma_tot:
                v = max(v, self.dma_tot[k])
            if need.get(k, 0) < v:
                need[k] = v

        for r in reads:
            if r.w is not None:
                add(r.w[0], r.w[1], r.w[2], "w")
            if r.excl:
                for k, (v, src) in r.r.items():
                    add(k, v, src, "r")
        for w in writes:
            if w.w is not None:
                add(w.w[0], w.w[1], w.w[2], "w")
            for k, (v, src) in w.r.items():
                add(k, v, src, "r")
        seen = self.seen[en]
        out = []
        for k, v in need.items():
            if seen.get(k, 0) < v:
                seen[k] = v
                out.append((k, v))
        return out

    @staticmethod
    def _mark(tok, reads, writes):
        k, v, src = tok
        for r in reads:
            r.r[k] = (v, src)
        for w in writes:
            w.w = tok
            w.r = {}

    def op(self, en, fn, reads=(), writes=()):
        waits = self._deps(en, reads, writes)
        self.cnt[en] += 1
        self._mark((en, self.cnt[en], en), reads, writes)
        self.ops[en].append((waits, fn, None))

    def dma(self, q, out_ap, in_ap, reads, writes, key):
        waits = self._deps(q, reads, writes)
        k = self._dma_sem(key)
        self.dma_tot[k] += 16
        self._mark((k, self.dma_tot[k], "dma"), reads, writes)
        self.ops[q].append((waits, (out_ap, in_ap), k))

    def final_wait(self, en, ress):
        waits = self._deps(en, ress, ress)
        self.ops[en].append((waits, None, None))

    def emit(self):
        nc = self.nc
        sems = self.sems
        ops = self.ops
        with nc.Block() as block:
            def body(en, e):
                mysem = sems[en]
                for waits, fn, dk in ops[en]:
                    for k, v in waits:
                        e.wait_ge(sems[k], v)
                    if fn is None:
                        continue
                    if dk is not None:
                        e.dma_start(out=fn[0], in_=fn[1]).then_inc(sems[dk], 16)
                    else:
                        fn(e).then_inc(mysem, 1)

            @block.tensor
            def _(e):
                body("pe", e)

            @block.scalar
            def _(e):
                body("act", e)

            @block.vector
            def _(e):
                body("dve", e)

            @block.gpsimd
            def _(e):
                body("pool", e)

            @block.sync
            def _(e):
                body("sp", e)


class Arena:
    def __init__(self, nc, base=16640, top=229344):
        self.nc = nc
        self.base = base
        self.top = top
        self.cur = base
        self.n = 0

    def alloc(self, name, shape, dtype, at=None):
        esz = 4 if dtype == F32 else 2
        nbytes = int(np.prod(shape[1:])) * esz
        nbytes = (nbytes + 63) // 64 * 64
        if at is None:
            off = self.cur
            self.cur += nbytes
        else:
            off = self.base + at
        assert off + nbytes <= self.top, (name, off, nbytes, self.top)
        self.n += 1
        t = self.nc.alloc_sbuf_tensor_at("%s_%d" % (name, self.n), list(shape), dtype, offset=off)
        return t, (off - self.base) + nbytes


def build_program(L=4, NSEQ=2, stop=None):
    nc = bass.Bass("TRN2", target_bir_lowering=False)
    PO = pf_offsets(L, NSEQ)

    def dram(name, shape, kind="ExternalInput"):
        return nc.dram_tensor(name, list(shape), F32, kind=kind).ap()

    xT_d = dram("xT", [NSEQ, D, SEQ])
    wada_d = dram("w_ada", [L, D, 9216])
    wfi_d = dram("w_ff_in", [L, 2, D, 2 * DFF])
    wfo_d = dram("w_ff_out", [L, 2, DFF, D])
    wmi_d = dram("w_mix_in", [L, D, 2840])
    wmo_d = dram("w_mix_out", [L, D, D])
    cmpw_d = dram("cmpw", [L, 128, CMPW])
    pf_d = dram("pf32", [128, PO["_n"]])
    cbf_d = dram("cbf", [128, CB_N])
    out_d = dram("yT", [NSEQ, D, SEQ], kind="ExternalOutput")

    es = ExitStack()
    with es:
        S = Sched(nc, es)
        ar = Arena(nc)
        xT, _ = ar.alloc("xT", [128, 8, SEQ], F32)
        RX = [[Res("x%d_%d" % (dc, tg)) for tg in range(4)] for dc in range(8)]
        cbf, _ = ar.alloc("cbf", [128, CB_N], BF16)
        R_cbf = Res("cbf")
        pf, _ = ar.alloc("pf", [128, PO["_n"]], F32)
        R_pf = Res("pf")
        modT, _ = ar.alloc("modT", [128, NSEQ, L, 72], F32)
        R_mod = [Res("modT%d" % i) for i in range(L)]
        modA, _ = ar.alloc("modA", [128, NSEQ, L, 3, 8], F32)
        modG, _ = ar.alloc("modG", [128, NSEQ, L, 3, 8], F32)
        R_modAG = [Res("modAG%d" % i) for i in range(L)]
        cact, _ = ar.alloc("cact", [128, 8 * NSEQ], BF16)
        R_cact = Res("cact")
        hT_off = ar.cur - ar.base
        hT, _ = ar.alloc("hT", [128, 8, 1024], BF16)
        RH = [Res("h0"), Res("h1")]
        wbuf_all, _ = ar.alloc("wbuf", [128, 2, 8, 512], BF16)
        R_wb = [Res("wb0"), Res("wb1")]
        rt, _ = ar.alloc("rt", [128, 512], F32)
        R_rt = Res("rt")
        rstd, _ = ar.alloc("rstd", [128, 512], F32)
        R_rstd = Res("rstd")
        tmpn = []
        R_tmpn = []
        for i in range(2):
            t, _ = ar.alloc("tmpn", [128, 512], F32)
            tmpn.append(t)
            R_tmpn.append(Res("tmpn%d" % i))
        sq = []
        R_sq = []
        for i in range(2):
            t, _ = ar.alloc("sq", [128, 512], BF16)
            sq.append(t)
            R_sq.append(Res("sq%d" % i))
        PR = ar.cur - ar.base
        cap = ar.top - ar.base

        aT, o = ar.alloc("aT", [128, 22, 1024], BF16, at=PR)
        RA = [Res("a0"), Res("a1")]
        wout = []
        R_wo = []
        for i in range(2):
            t, o = ar.alloc("wout", [128, 22, 256], BF16, at=o)
            wout.append(t)
            R_wo.append(Res("wo%d" % i))
        sg = []
        R_sg = []
        for i in range(2):
            t, o = ar.alloc("sg", [128, 512], BF16, at=o)
            sg.append(t)
            R_sg.append(Res("sg%d" % i))
        rslot = [rstd]
        R_rslot = [R_rstd]
        for i in range(4):
            t, o = ar.alloc("rslot", [128, 512], F32, at=o)
            rslot.append(t)
            R_rslot.append(Res("rslot%d" % i))
        adab2 = []
        R_ada2 = []
        for i in range(2):
            t, o = ar.alloc("adab2", [128, 8, 256], BF16, at=o)
            adab2.append(t)
            R_ada2.append(Res("ada2_%d" % i))
        assert o <= cap, o
        FFN_RES = RA + R_wo + R_sg + R_rslot[1:] + R_ada2
        adab = []
        R_ada = []
        o2 = PR
        for i in range(2):
            t, o2 = ar.alloc("adab", [128, 8, 1152], BF16, at=o2)
            adab.append(t)
            R_ada.append(Res("ada%d" % i))

        qT, o = ar.alloc("qT", [128, 4, SEQ], BF16, at=PR)
        RQ = [Res("q%d" % i) for i in range(4)]
        kpad, o = ar.alloc("kpad", [128, 2, 2, SEQ], BF16, at=o)
        RKS = [Res("ks%d" % i) for i in range(4)]
        vaug, o = ar.alloc("vaug", [128, 2, 16, 2, 65], BF16, at=o)
        RV = [Res("v%d" % i) for i in range(16)]
        sig, o = ar.alloc("sig", [128, 16, 24], F32, at=o)
        RSIG = [Res("sig%d" % i) for i in range(4)]
        ocT, o = ar.alloc("ocT", [128, 4, SEQ], BF16, at=o)
        ROC = [Res("oc%d" % i) for i in range(4)]
        kcmpT, o = ar.alloc("kcmpT", [128, 2, 128], BF16, at=o)
        R_kcmp = Res("kcmp")
        Rc, o = ar.alloc("Rc", [128, 2, 97], BF16, at=o)
        R_Rc = Res("Rc")
        bvec, o = ar.alloc("bvec", [128, 2], F32, at=o)
        R_bvec = Res("bvec")
        slb = []
        R_sl = []
        for i in range(2):
            t, o = ar.alloc("sl", [128, 128], BF16, at=o)
            slb.append(t)
            R_sl.append(Res("sl%d" % i))
        ATT_P = o
        kcvc, o = ar.alloc("kcvc", [128, 2, SEQ], BF16, at=ATT_P)
        R_kcvc = Res("kcvc")
        A_X = o
        ubuf, o = ar.alloc("ubuf", [128, 4, 514], F32, at=A_X)
        RU = [Res("u%d" % i) for i in range(4)]
        ybuf, o = ar.alloc("ybuf", [128, 4, 512], F32, at=o)
        RY = [Res("y%d" % i) for i in range(4)]
        hcs, R_hcs = tmpn[0], R_tmpn[0]
        v0b, R_v0 = tmpn[1], R_tmpn[1]
        assert o <= cap, o
        A_RES = RU + RY
        cmpw, oc_ = ar.alloc("cmpw", [128, CMPW], BF16, at=A_X)
        R_cmpw = Res("cmpw")
        OA, o = ar.alloc("OA", [128, 4, 512], F32, at=ATT_P)
        OA2, _ = ar.alloc("OA2", [128, 4, 512], F32, at=hT_off)
        R_OA = Res("OA")
        negselT, o = ar.alloc("negselT", [128, 2, 512], BF16, at=o)
        R_ns = [Res("ns")]
        ptb = []
        R_pt = []
        for i in range(5):
            t, o = ar.alloc("pt", [128, 512], BF16, at=o)
            ptb.append(t)
            R_pt.append(Res("pt%d" % i))
        oaT, o = ar.alloc("oaT", [128, 4, 512], BF16, at=o)
        R_oaT = Res("oaT")
        OAn, o = ar.alloc("OAn", [128, 2, 512], BF16, at=o)
        R_OAn = [Res("OAn0"), Res("OAn1")]
        imp, o = ar.alloc("imp", [128, 2, 4, 32], F32, at=o)
        R_imp = Res("imp")
        nsb, o = ar.alloc("nsb", [128, 2, 4, 32], BF16, at=o)
        R_nsb = Res("nsb")
        top8, o = ar.alloc("top8", [128, 4, 8], F32, at=o)
        R_top8 = Res("top8")
        smalls = []
        R_small = []
        for i in range(6):
            t, o = ar.alloc("sm", [128, 4, 1], F32, at=o)
            smalls.append(t)
            R_small.append(Res("sm%d" % i))
        junk, o = ar.alloc("junk", [128, 512], BF16, at=o)
        R_junk = Res("junk")
        assert o <= cap, o
        B_RES = [R_OA] + R_ns + R_pt + R_OAn + [R_oaT, R_imp, R_nsb, R_top8, R_junk] + R_small
        ATT_RES = (RQ + RKS + RV + RSIG + ROC + [R_kcmp, R_Rc, R_bvec, R_kcvc, R_cmpw] + R_sl + A_RES + B_RES)

        banks = [nc.alloc_psum_tensor("bk%d" % i, [128, 512], F32) for i in range(8)]
        PB = [Res("pb%d" % i, excl=True) for i in range(8)]

        ident = cbf[:, CB_ID:CB_ID + 128]
        T0 = cbf[:, CB_T0:CB_T0 + 128]
        T1 = cbf[:, CB_T1:CB_T1 + 128]
        ones = cbf[:, CB_ONE:CB_ONE + 128]
        eps_ap = pf[:, PO["eps"]:PO["eps"] + 1]

        def wb(i):
            return wbuf_all[:, i]

        for c0 in range(0, CB_N, 2048):
            c1 = min(CB_N, c0 + 2048)
            S.dma("pool", cbf[:, c0:c1], cbf_d[:, c0:c1], [], [R_cbf], "cbf")
        S.dma("sp", pf[:, :], pf_d[:, :], [], [R_pf], "pf")
        S.op("act", lambda e: e.activation(out=cact[:, :], in_=pf[:, PO["c"]:PO["c"] + 8 * NSEQ], func=AF.Silu),
             [R_pf], [R_cact])
        gi = 0
        for l in range(1):
            for cg in range(8):
                ab = adab[gi % 2]
                Rab = R_ada[gi % 2]
                src = wada_d[l].rearrange("(kc p) c -> p kc c", p=128)[:, :, cg * 1152:(cg + 1) * 1152]
                S.dma("pool", ab[:, :, :], src, [], [Rab], "ada%d" % (gi % 2))
                bk = gi % 2

                def f(e, ab=ab, bk=bk):
                    ins = None
                    for ch in range(9):
                        for kc in range(8):
                            ins = e.matmul(banks[bk][:, ch * NSEQ:(ch + 1) * NSEQ], lhsT=ab[:, kc, ch * 128:(ch + 1) * 128],
                                           rhs=cact[:, kc * NSEQ:(kc + 1) * NSEQ], start=(kc == 0), stop=(kc == 7),
                                           skip_group_check=True)
                    return ins
                S.op("pe", f, [Rab, R_cact], [PB[bk]])
                for s in range(NSEQ):
                    psv = banks[bk][:, 0:9 * NSEQ].rearrange("p (a b) -> p a b", b=NSEQ)[:, :, s:s + 1]
                    bcol = PO["b"] + l * 72 + cg * 9
                    S.op("dve", lambda e, psv=psv, s=s, l=l, cg=cg, bcol=bcol: e.tensor_tensor(
                        out=modT[:, s, l, cg * 9:(cg + 1) * 9].unsqueeze(2), in0=psv,
                        in1=pf[:, bcol:bcol + 9].unsqueeze(2), op=ALU.add), [PB[bk], R_pf], [R_mod[l]])
                gi += 1

        def mod_derived(l):
            for s in range(NSEQ):
                for sub in range(3):
                    gcol = PO["g"] + (l * 3 + sub) * 8
                    S.op("dve", lambda e, s=s, l=l, sub=sub, gcol=gcol: e.scalar_tensor_tensor(
                        out=modA[:, s, l, sub, :], in0=modT[:, s, l, sub * 24 + 8:sub * 24 + 16], scalar=1.0,
                        in1=pf[:, gcol:gcol + 8], op0=ALU.add, op1=ALU.mult), [R_mod[l], R_pf], [R_modAG[l]])
                    gm = 1.0 if sub == 1 else 0.5
                    S.op("dve", lambda e, s=s, l=l, sub=sub, gm=gm: e.tensor_scalar(
                        out=modG[:, s, l, sub, :], in0=modT[:, s, l, sub * 24 + 16:sub * 24 + 24], scalar1=gm,
                        scalar2=None, op0=ALU.mult), [R_mod[l]], [R_modAG[l]])

        mod_derived(0)
        ada_pending = []

        def ada_group(l, gidx):
            i = st["ada"] % 2
            st["ada"] += 1
            ab = adab2[i]
            src = wada_d[l].rearrange("(kc p) c -> p kc c", p=128)[:, :, gidx * 256:(gidx + 1) * 256]
            S.dma("pool", ab[:, :, :], src, [], [R_ada2[i]], "ada2_%d" % i)

            def f(e):
                ins = None
                for ch in range(2):
                    for kc in range(8):
                        ins = e.matmul(banks[7][:, ch * NSEQ:(ch + 1) * NSEQ], lhsT=ab[:, kc, ch * 128:(ch + 1) * 128],
                                       rhs=cact[:, kc * NSEQ:(kc + 1) * NSEQ], start=(kc == 0), stop=(kc == 7),
                                       skip_group_check=True)
                return ins
            S.op("pe", f, [R_ada2[i], R_cact], [PB[7]])
            for s in range(NSEQ):
                psv = banks[7][:, 0:2 * NSEQ].rearrange("p (a b) -> p a b", b=NSEQ)[:, :, s:s + 1]
                bcol = PO["b"] + l * 72 + gidx * 2
                S.op("dve", lambda e, psv=psv, s=s, bcol=bcol: e.tensor_tensor(
                    out=modT[:, s, l, gidx * 2:(gidx + 1) * 2].unsqueeze(2), in0=psv,
                    in1=pf[:, bcol:bcol + 2].unsqueeze(2), op=ALU.add), [PB[7], R_pf], [R_mod[l]])
            if gidx == 35:
                mod_derived(l)

        def ada_hook(n=2):
            for _ in range(n):
                if ada_pending:
                    ada_group(*ada_pending.pop(0))

        st = {"wb": 0, "wo": 0, "bank": 0, "sb": 0, "pt": 0, "ada": 0}

        def next_wb():
            i = st["wb"] % 2
            st["wb"] += 1
            return i

        def prenorm(s, l, sub, tok0, dst, R_dst):
            norm_stats(tok0, 0)
            norm_apply(s, l, sub, tok0, 0, dst, R_dst)

        def norm_stats(tok0, slot):
            tg = tok0 // 512
            rstd = rslot[slot]
            R_rstd = R_rslot[slot]
            for dc in range(8):
                b = dc % 2
                S.op("act", lambda e, dc=dc, b=b: e.activation(out=sq[b][:, :], in_=xT[:, dc, tok0:tok0 + 512],
                                                              func=AF.Square), [RX[dc][tg]], [R_sq[b]])
                S.op("pe", lambda e, dc=dc, b=b: e.matmul(banks[7][:, :], lhsT=ones, rhs=sq[b][:, :], start=(dc == 0),
                                                         stop=(dc == 7)), [R_sq[b], R_cbf], [PB[7]])
            S.op("act", lambda e: e.activation(out=rt[:, :], in_=banks[7][:, :], func=AF.Sqrt, scale=1.0 / D,
                                               bias=eps_ap), [PB[7], R_pf], [R_rt])
            S.op("dve", lambda e: e.reciprocal(out=rstd[:, :], in_=rt[:, :]), [R_rt], [R_rstd])

        def norm_apply(s, l, sub, tok0, slot, dst, R_dst):
            tg = tok0 // 512
            rstd = rslot[slot]
            R_rstd = R_rslot[slot]
            for dc in range(8):
                b = dc % 2
                S.op("dve", lambda e, dc=dc, b=b: e.scalar_tensor_tensor(
                    out=tmpn[b][:, :], in0=xT[:, dc, tok0:tok0 + 512], scalar=modA[:, s, l, sub, dc:dc + 1],
                    in1=rstd[:, :], op0=ALU.mult, op1=ALU.mult), [RX[dc][tg], R_modAG[l], R_rstd], [R_tmpn[b]])
                S.op("act", lambda e, dc=dc, b=b: e.activation(
                    out=dst(dc), in_=tmpn[b][:, :], func=AF.Identity,
                    bias=modT[:, s, l, sub * 24 + dc:sub * 24 + dc + 1], scale=1.0), [R_tmpn[b], R_mod[l]], [R_dst])

        def ffn_norm(s, l, j, half, stage=None, slots=(0, 0)):
            sub = 0 if j == 0 else 2
            for tgi in range(2):
                tok0 = half * 1024 + tgi * 512
                if stage in (None, "stats"):
                    norm_stats(tok0, slots[tgi])
                if stage in (None, "apply"):
                    norm_apply(s, l, sub, tok0, slots[tgi],
                               lambda dc, tgi=tgi: hT[:, dc, tgi * 512:(tgi + 1) * 512], RH[tgi])

        def ffn(s, l, j, norm_done=False, tail_hook=None):
            sub = 0 if j == 0 else 2
            fence(FFN_RES, ATT_RES + R_ada)
            if not norm_done:
                ffn_norm(s, l, j, 0)
            ffn_norm(s, l, j, 1, "stats", (1, 2))
            for half in range(2):
                if half == 1 and tail_hook is not None:
                    tail_hook("stats", (3, 4))
                for fg in range(11):
                    wi = next_wb()
                    w = wb(wi)
                    src = wfi_d[l, j].rearrange("(kc p) c -> p kc c", p=128)[:, :, fg * 512:(fg + 1) * 512]
                    S.dma("pool", w[:, :, :], src, [], [R_wb[wi]], "wb%d" % wi)
                    for tgi in range(2):
                        bs = 4 * (st["bank"] % 2)
                        st["bank"] += 1
                        for oc in range(4):
                            def f(e, w=w, oc=oc, tgi=tgi, bs=bs):
                                ins = None
                                for kc in range(8):
                                    ins = e.matmul(banks[bs + oc][:, :], lhsT=w[:, kc, oc * 128:(oc + 1) * 128],
                                                   rhs=hT[:, kc, tgi * 512:(tgi + 1) * 512], start=(kc == 0),
                                                   stop=(kc == 7))
                                return ins
                            S.op("pe", f, [R_wb[wi], RH[tgi]], [PB[bs + oc]])
                        for i in range(2):
                            S.op("act", lambda e, i=i, bs=bs: e.activation(out=sg[i][:, :], in_=banks[bs + i][:, :],
                                                                           func=AF.Silu), [PB[bs + i]], [R_sg[i]])
                            S.op("dve", lambda e, i=i, bs=bs, fg=fg, tgi=tgi: e.tensor_tensor(
                                out=aT[:, 2 * fg + i, tgi * 512:(tgi + 1) * 512], in0=sg[i][:, :],
                                in1=banks[bs + 2 + i][:, :], op=ALU.mult), [R_sg[i], PB[bs + 2 + i]], [RA[tgi]])
                    if j == 0 and s == 0:
                        ada_hook(2)
                if half == 0:
                    ffn_norm(s, l, j, 1, "apply", (1, 2))
                elif tail_hook is not None:
                    tail_hook("apply", (3, 4))
                for dmp in range(4):
                    oi = st["wo"] % 2
                    st["wo"] += 1
                    wo = wout[oi]
                    src = wfo_d[l, j].rearrange("(fc p) c -> p fc c", p=128)[:, :, dmp * 256:(dmp + 1) * 256]
                    S.dma("pool", wo[:, :, :], src, [], [R_wo[oi]], "wo%d" % oi)
                    for dmi in range(2):
                        dc = dmp * 2 + dmi
                        for tgi in range(2):
                            bk = st["sb"] % 8
                            st["sb"] += 1
                            tg = half * 2 + tgi

                            def f(e, wo=wo, dmi=dmi, tgi=tgi, bk=bk):
                                ins = None
                                for fc in range(22):
                                    ins = e.matmul(banks[bk][:, :], lhsT=wo[:, fc, dmi * 128:(dmi + 1) * 128],
                                                   rhs=aT[:, fc, tgi * 512:(tgi + 1) * 512], start=(fc == 0),
                                                   stop=(fc == 21))
                                return ins
                            S.op("pe", f, [R_wo[oi], RA[tgi]], [PB[bk]])
                            S.op("dve", lambda e, dc=dc, tg=tg, bk=bk: e.scalar_tensor_tensor(
                                out=xT[:, dc, tg * 512:(tg + 1) * 512], in0=banks[bk][:, :],
                                scalar=modG[:, s, l, sub, dc:dc + 1], in1=xT[:, dc, tg * 512:(tg + 1) * 512],
                                op0=ALU.mult, op1=ALU.add), [PB[bk], RX[dc][tg], R_modAG[l]], [RX[dc][tg]])

        def mixer_norm(s, l, tg, stage=None, slot=0):
            hb = tg % 2
            if stage in (None, "stats"):
                norm_stats(tg * 512, slot)
            if stage in (None, "apply"):
                norm_apply(s, l, 1, tg * 512, slot, lambda dc, hb=hb: hT[:, dc, hb * 512:(hb + 1) * 512], RH[hb])

        def mixer(s, l, norm_done=False, mid_hook=None):
            fence(ATT_RES, FFN_RES + R_ada)
            cwc = PO["cw"] + l * 12
            cbc = PO["cb"] + l * 4
            gmc = PO["gmo"] + l * 8
            S.op("dve", lambda e: e.memset(vaug[:, :, :, :, 64:65], 1.0), [], RV)
            S.op("dve", lambda e: e.memset(Rc[:, :, :], 0.0), [], [R_Rc])
            for g in range(2):
                S.op("dve", lambda e, g=g: e.tensor_copy(out=Rc[:, g, 0:32], in_=cbf[:, CB_OV:CB_OV + 32]),
                     [R_cbf], [R_Rc])
            S.op("dve", lambda e: e.memset(Rc[:, :, 96:97], 1.0), [], [R_Rc])
            S.op("dve", lambda e: e.memset(kcmpT[:, :, :], 0.0), [], [R_kcmp])
            S.op("dve", lambda e: e.memset(kpad[64:128, :, 0, :], 0.0), [], RKS)
            S.op("dve", lambda e: e.memset(kpad[0:64, :, 1, :], 0.0), [], RKS)
            S.op("dve", lambda e: e.memset(ubuf[:, :, 0:2], 0.0), [], RU)

            def load_w(c0, n):
                wi = next_wb()
                w = wb(wi)
                src = wmi_d[l].rearrange("(kc p) c -> p kc c", p=128)[:, :, c0:c0 + n]
                S.dma("pool", w[:, :, 0:n], src, [], [R_wb[wi]], "wb%d" % wi)
                return w, R_wb[wi]

            def proj(w, Rw, col, hb, bk):
                def f(e):
                    ins = None
                    for kc in range(8):
                        ins = e.matmul(banks[bk][:, :], lhsT=w[:, kc, col:col + 128],
                                       rhs=hT[:, kc, hb * 512:(hb + 1) * 512], start=(kc == 0), stop=(kc == 7))
                    return ins
                S.op("pe", f, [Rw, RH[hb]], [PB[bk]])

            def phaseA(tg):
                hb = tg % 2
                cs = slice(tg * 512, (tg + 1) * 512)
                if tg == 0 and not norm_done:
                    mixer_norm(s, l, 0)
                w, Rw = load_w(0, 512)
                for c in range(4):
                    proj(w, Rw, c * 128, hb, c)
                for c in range(4):
                    if c % 2 == 0:
                        S.op("act", lambda e, c=c: e.activation(out=qT[:, c, cs], in_=banks[c][:, :], func=AF.Identity,
                                                                scale=0.125), [PB[c]], [RQ[tg]])
                    else:
                        S.op("dve", lambda e, c=c: e.tensor_scalar(out=qT[:, c, cs], in0=banks[c][:, :], scalar1=0.125,
                                                                   scalar2=None, op0=ALU.mult), [PB[c]], [RQ[tg]])
                w, Rw = load_w(512, 512)
                for c in range(4):
                    proj(w, Rw, c * 128, hb, 4 + c)
                S.op("act", lambda e: e.copy(out=kcvc[:, 0, cs], in_=banks[4][:, :]), [PB[4]], [R_kcvc])
                S.op("dve", lambda e: e.tensor_copy(out=kcvc[:, 1, cs], in_=banks[5][:, :]), [PB[5]], [R_kcvc])
                for kind in range(2):
                    S.op("act", lambda e, kind=kind: e.copy(out=kpad[0:64, kind, 0, cs], in_=banks[6 + kind][0:64, :]),
                         [PB[6 + kind]], [RKS[tg]])
                    S.op("dve", lambda e, kind=kind: e.tensor_copy(out=kpad[64:128, kind, 1, cs],
                                                                   in_=banks[6 + kind][64:128, :]),
                         [PB[6 + kind]], [RKS[tg]])
                if tg < 3:
                    mixer_norm(s, l, tg + 1)
                for cc in range(4):
                    w, Rw = load_w(1024 + cc * 384, 384)
                    b0 = (cc * 3) % 8
                    bh, bb, bc_ = b0, (b0 + 1) % 8, (b0 + 2) % 8
                    proj(w, Rw, 0, hb, bh)
                    proj(w, Rw, 128, hb, bb)
                    proj(w, Rw, 256, hb, bc_)
                    S.op("act", lambda e, bh=bh: e.copy(out=hcs[:, :], in_=banks[bh][:, :]), [PB[bh]], [R_hcs])
                    S.op("dve", lambda e, cc=cc, bc_=bc_: e.tensor_tensor(out=ubuf[:, cc, 2:514], in0=hcs[:, :],
                                                                          in1=banks[bc_][:, :], op=ALU.mult),
                         [R_hcs, PB[bc_]], [RU[cc]])
                    S.op("act", lambda e, cc=cc: e.activation(
                        out=v0b[:, :], in_=ubuf[:, cc, 2:514], func=AF.Identity,
                        scale=pf[:, cwc + 8 + cc:cwc + 9 + cc], bias=pf[:, cbc + cc:cbc + cc + 1]),
                        [RU[cc], R_pf], [R_v0])
                    S.op("dve", lambda e, cc=cc: e.scalar_tensor_tensor(
                        out=v0b[:, :], in0=ubuf[:, cc, 1:513], scalar=pf[:, cwc + 4 + cc:cwc + 5 + cc], in1=v0b[:, :],
                        op0=ALU.mult, op1=ALU.add), [RU[cc], R_pf, R_v0], [R_v0])
                    S.op("dve", lambda e, cc=cc: e.scalar_tensor_tensor(
                        out=v0b[:, :], in0=ubuf[:, cc, 0:512], scalar=pf[:, cwc + cc:cwc + cc + 1], in1=v0b[:, :],
                        op0=ALU.mult, op1=ALU.add), [RU[cc], R_pf, R_v0], [R_v0])
                    S.op("dve", lambda e, cc=cc, bb=bb: e.tensor_tensor(out=ybuf[:, cc, :], in0=v0b[:, :],
                                                                        in1=banks[bb][:, :], op=ALU.mult),
                         [R_v0, PB[bb]], [RY[cc]])
                    S.op("act", lambda e, cc=cc: e.copy(out=ubuf[:, cc, 0:2], in_=ubuf[:, cc, 512:514]),
                         [RU[cc]], [RU[cc]])
                w, Rw = load_w(2560, 280)
                for tile in range(4):
                    ti = tg * 4 + tile
                    bk = 3 + tile

                    def f(e, w=w, tile=tile, bk=bk):
                        ins = None
                        for kc in range(8):
                            ins = e.matmul(banks[bk][:, 0:280],
                                           lhsT=hT[:, kc, hb * 512 + tile * 128:hb * 512 + (tile + 1) * 128],
                                           rhs=w[:, kc, 0:280], start=(kc == 0), stop=(kc == 7))
                        return ins
                    S.op("pe", f, [Rw, RH[hb]], [PB[bk]])
                    S.op("act", lambda e, ti=ti, bk=bk: e.copy(
                        out=vaug[:, 0, ti, :, 0:64], in_=banks[bk][:, 0:128].rearrange("p (a b) -> p a b", a=2)),
                        [PB[bk]], [RV[ti]])
                    S.op("dve", lambda e, ti=ti, bk=bk: e.tensor_copy(
                        out=vaug[:, 1, ti, :, 0:64], in_=banks[bk][:, 128:256].rearrange("p (a b) -> p a b", a=2)),
                        [PB[bk]], [RV[ti]])
                    S.op("act", lambda e, ti=ti, bk=bk: e.activation(out=sig[:, ti, :], in_=banks[bk][:, 256:280],
                                                                     func=AF.Sigmoid), [PB[bk]], [RSIG[tg]])
                for cc in range(4):
                    b = cc % 2
                    S.op("act", lambda e, cc=cc, b=b: e.activation(out=sq[b][:, :], in_=ybuf[:, cc, :], func=AF.Square),
                         [RY[cc]], [R_sq[b]])
                    S.op("pe", lambda e, cc=cc, b=b: e.matmul(banks[7][:, :], lhsT=ones, rhs=sq[b][:, :],
                                                              start=(cc == 0), stop=(cc == 3)),
                         [R_sq[b], R_cbf], [PB[7]])
                S.op("act", lambda e: e.activation(out=rt[:, :], in_=banks[7][:, :], func=AF.Sqrt, scale=1.0 / 512,
                                                   bias=eps_ap), [PB[7], R_pf], [R_rt])
                S.op("dve", lambda e: e.reciprocal(out=rstd[:, :], in_=rt[:, :]), [R_rt], [R_rstd])
                for cc in range(4):
                    S.op("dve", lambda e, cc=cc: e.scalar_tensor_tensor(
                        out=ocT[:, cc, cs], in0=ybuf[:, cc, :], scalar=pf[:, gmc + 4 + cc:gmc + 5 + cc],
                        in1=rstd[:, :], op0=ALU.mult, op1=ALU.mult), [RY[cc], R_pf, R_rstd], [ROC[tg]])

            for tg_ in range(4):
                phaseA(tg_)

            fence([R_cmpw], A_RES)
            for c0 in range(0, CMPW, 2048):
                c1 = min(CMPW, c0 + 2048)
                S.dma("pool", cmpw[:, c0:c1], cmpw_d[l, :, c0:c1], [], [R_cmpw], "cmpw")
            w2k = cmpw[0:64, 4164:4228]
            w2v = cmpw[0:64, 4228:4292]
            for kind in range(2):
                w1 = cmpw[:, kind * 2048:(kind + 1) * 2048]
                pos = cmpw[:, 4096 + kind * 34:4096 + (kind + 1) * 34]

                def fb(e, w1=w1, pos=pos, kind=kind):
                    ins = None
                    for lp in range(32):
                        ins = e.matmul(banks[2][0:64, kind * 2:kind * 2 + 2], lhsT=w1[0:64, lp * 64:(lp + 1) * 64],
                                       rhs=pos[0:64, lp:lp + 2], start=(lp == 0), stop=(lp == 31), skip_group_check=True)
                    return ins
                S.op("pe", fb, [R_cmpw], [PB[2]])
                S.op("dve", lambda e, kind=kind: e.tensor_copy(out=bvec[0:64, kind:kind + 1],
                                                               in_=banks[2][0:64, kind * 2:kind * 2 + 1]),
                     [PB[2]], [R_bvec])
                for g in range(2):
                    rows = slice(64 * g, 64 * g + 64)
                    sl = slb[g]

                    def fc(e, w1=w1, kind=kind, g=g, rows=rows):
                        ins = None
                        for lp in range(32):
                            ins = e.matmul(banks[g][0:64, 0:127], lhsT=w1[rows, lp * 64:(lp + 1) * 64],
                                           rhs=kcvc[rows, kind, lp:lp + 16 * 126 + 1:16], start=(lp == 0), stop=(lp == 31))
                        return ins
                    S.op("pe", fc, [R_cmpw, R_kcvc], [PB[g]])
                    S.op("act", lambda e, g=g, sl=sl, kind=kind: e.activation(
                        out=sl[0:64, 0:127], in_=banks[g][0:64, 0:127], func=AF.Silu, bias=bvec[0:64, kind:kind + 1],
                        scale=1.0), [PB[g], R_bvec], [R_sl[g]])
                    if kind == 0:
                        S.op("pe", lambda e, sl=sl, rows=rows: e.matmul(banks[3][rows, 0:127], lhsT=w2k,
                                                                        rhs=sl[0:64, 0:127], start=True, stop=True,
                                                                        skip_group_check=True),
                             [R_cmpw, R_sl[g]], [PB[3]])
                        S.op("dve", lambda e, rows=rows, g=g: e.tensor_copy(out=kcmpT[rows, g, 0:127],
                                                                            in_=banks[3][rows, 0:127]),
                             [PB[3]], [R_kcmp])
                    else:
                        S.op("pe", lambda e, sl=sl, g=g: e.matmul(banks[3][0:127, 128 + g * 64:192 + g * 64],
                                                                  lhsT=sl[0:64, 0:127], rhs=w2v, start=True, stop=True,
                                                                  skip_group_check=True),
                             [R_cmpw, R_sl[g]], [PB[3]])
                        S.op("dve", lambda e, g=g: e.tensor_copy(out=Rc[0:127, g, 32:96],
                                                                 in_=banks[3][0:127, 128 + g * 64:192 + g * 64]),
                             [PB[3]], [R_Rc])

            fence(B_RES, A_RES + [R_cmpw, R_kcvc])
            S.op("dve", lambda e: e.memset(negselT[:, :, :], 0.0), [], R_ns)
            wmo = wbuf_all.rearrange("p a k c -> p (a k c)").rearrange("p (k c) -> p k c", c=1024)
            for hf in range(2):
                src = wmo_d[l].rearrange("(kc p) c -> p kc c", p=128)[:, hf * 4:(hf + 1) * 4, :]
                S.dma("pool", wmo[:, hf * 4:(hf + 1) * 4, :], src, [], [R_wb[hf]], "wb%d" % hf)
            st["wb"] = 0
            rz, zc, rzg, ssq, rt4, rs4 = smalls
            R_rz, R_zc, R_rzg, R_ssq, R_rt4, R_rs4 = R_small
            fbc = PO["fb"]

            def next_sb():
                i = st["sb"] % 3
                st["sb"] += 1
                return i

            def next_pt():
                i = st["pt"] % 5
                st["pt"] += 1
                return i

            def next_sb5():
                i = (0, 1, 2, 5, 6)[st["sb"] % 5]
                st["sb"] += 1
                return i

            OAs = [OA, OA2]
            R_OAs = [[R_OA], [RH[0], RH[1]]]

            def finalize(bk, width, zcol, vcol, h, br, tg, first, clampz):
                OAt = OAs[tg % 2]
                R_OAt = R_OAs[tg % 2]
                view = banks[bk][:, 0:4 * width].rearrange("p (a b) -> p a b", b=width)
                if clampz:
                    S.op("dve", lambda e: e.tensor_scalar(out=zc[:, :, :], in0=view[:, :, zcol:zcol + 1], scalar1=1e-30,
                                                          scalar2=None, op0=ALU.max), [PB[bk]], [R_zc])
                    S.op("dve", lambda e: e.reciprocal(out=rz[:, :, :], in_=zc[:, :, :]), [R_zc], [R_rz])
                else:
                    S.op("dve", lambda e: e.reciprocal(out=rz[:, :, :], in_=view[:, :, zcol:zcol + 1]), [PB[bk]], [R_rz])
                gcol = h * 3 + br
                S.op("dve", lambda e: e.tensor_tensor(out=rzg[:, :, :], in0=rz[:, :, :],
                                                      in1=sig[:, tg * 4:(tg + 1) * 4, gcol:gcol + 1], op=ALU.mult),
                     [R_rz, RSIG[tg]], [R_rzg])
                for tile in range(4):
                    if first:
                        S.op("dve", lambda e, tile=tile: e.tensor_scalar(
                            out=OAt[:, tile, h * 64:(h + 1) * 64], in0=view[:, tile, vcol:vcol + 64],
                            scalar1=rzg[:, tile, :], scalar2=None, op0=ALU.mult), [PB[bk], R_rzg], R_OAt)
                    else:
                        S.op("dve", lambda e, tile=tile: e.scalar_tensor_tensor(
                            out=OAt[:, tile, h * 64:(h + 1) * 64], in0=view[:, tile, vcol:vcol + 64],
                            scalar=rzg[:, tile, :], in1=OAt[:, tile, h * 64:(h + 1) * 64], op0=ALU.mult, op1=ALU.add),
                            [PB[bk], R_rzg] + R_OAt, R_OAt)

            def pipeline(steps, skew, fillers=(), every=2, start_at=3):
                n = len(steps)
                fi = 0
                for i in range(n + skew):
                    if i < n:
                        steps[i][0]()
                        steps[i][1]()
                    if i - skew >= 0:
                        steps[i - skew][2]()
                    if fi < len(fillers) and i >= start_at and (i - start_at) % every == 0:
                        fillers[fi]()
                        fi += 1
                while fi < len(fillers):
                    fillers[fi]()
                    fi += 1

            def next_s4():
                i = (0, 1, 2, 5)[st["sb"] % 4]
                st["sb"] += 1
                return i

            def cmp_step(tg, h, tail=None):
                q0 = tg * 512
                g, hh = h // 4, h % 4
                cb_ = 6
                rs_ = {}

                def A():
                    sb = rs_["sb"] = next_s4()
                    rs_["pi"] = next_pt()

                    def f(e):
                        e.matmul(banks[sb][:, :], lhsT=kcmpT[:, g, :], rhs=qT[:, hh, q0:q0 + 512], start=True,
                                 stop=False)
                        return e.matmul(banks[sb][:, :], lhsT=ident, rhs=cbf[:, CB_BC + q0:CB_BC + q0 + 512],
                                        start=False, stop=True)
                    S.op("pe", f, [R_kcmp, RQ[tg], R_cbf], [PB[sb]])

                def B():
                    sb, pi = rs_["sb"], rs_["pi"]
                    pt = ptb[pi]
                    S.op("act", lambda e: e.activation(out=pt[:, :], in_=banks[sb][:, :], func=AF.Exp),
                         [PB[sb]], [R_pt[pi]])

                def C():
                    pi = rs_["pi"]
                    pt = ptb[pi]

                    def f2(e):
                        ins = None
                        for tile in range(4):
                            ins = e.matmul(banks[cb_][:, tile * 97:(tile + 1) * 97],
                                           lhsT=pt[:, tile * 128:(tile + 1) * 128], rhs=Rc[:, g, :],
                                           start=(tile == 0), stop=(tile == 3), skip_group_check=True)
                        return ins
                    S.op("pe", f2, [R_pt[pi], R_Rc], [PB[cb_]])
                    finalize(cb_, 97, 96, 32, h, 0, tg, True, True)
                    view = banks[cb_][:, 0:4 * 97].rearrange("p (a b) -> p a b", b=97)
                    for tile in range(4):
                        if hh == 0:
                            S.op("dve", lambda e, tile=tile: e.tensor_scalar(
                                out=imp[:, g, tile, :], in0=view[:, tile, 0:32], scalar1=rz[:, tile, :],
                                scalar2=None, op0=ALU.mult), [PB[cb_], R_rz], [R_imp])
                        else:
                            S.op("dve", lambda e, tile=tile: e.scalar_tensor_tensor(
                                out=imp[:, g, tile, :], in0=view[:, tile, 0:32], scalar=rz[:, tile, :],
                                in1=imp[:, g, tile, :], op0=ALU.mult, op1=ALU.add), [PB[cb_], R_rz, R_imp],
                                [R_imp])
                    if tail is not None:
                        tail()
                return (A, B, C)

            def select(tg):
                for g in range(2):
                    S.op("dve", lambda e, g=g: e.tensor_tensor(
                        out=imp[:, g, :, :], in0=imp[:, g, :, :],
                        in1=pf[:, fbc + tg * 128:fbc + (tg + 1) * 128].rearrange("p (a b) -> p a b", b=32), op=ALU.add),
                        [R_imp, R_pf], [R_imp])
                    for tile in range(4):
                        S.op("dve", lambda e, tile=tile, g=g: e.max(out=top8[:, tile, :], in_=imp[:, g, tile, :]),
                             [R_imp], [R_top8])
                        S.op("dve", lambda e, tile=tile, g=g: e.tensor_scalar(
                            out=nsb[:, g, tile, :], in0=imp[:, g, tile, :], scalar1=top8[:, tile, 7:8], scalar2=NEG,
                            op0=ALU.is_lt, op1=ALU.mult), [R_imp, R_top8], [R_nsb])

            pst = banks[7][0:32, 0:512].bitcast(BF16)

            def sel_mask():
                def ftn(e):
                    ins = None
                    for g in range(2):
                        for tile in range(4):
                            ins = e.transpose(out=pst[:, (g * 4 + tile) * 128:(g * 4 + tile + 1) * 128],
                                              in_=nsb[:, g, tile, :], identity=ident)
                    return ins
                S.op("pe", ftn, [R_nsb, R_cbf], [PB[7]])
                S.op("act", lambda e: e.copy(out=negselT[0:32, :, :], in_=pst.rearrange("p (g c) -> p g c", g=2)),
                     [PB[7]], R_ns)

            def att_step(tg, br, h, g, hh, ob, c, lo, hi, diag, TB, first, last):
                q0 = tg * 512
                rs_ = {}
                bt = lo if TB is T0 else hi - 128

                def A():
                    sb = rs_["sb"] = next_s4()
                    rs_["pi"] = next_pt()

                    def f(e):
                        ins = e.matmul(banks[sb][:, lo:hi], lhsT=kpad[:, br - 1, g, c * 128:(c + 1) * 128],
                                       rhs=qT[:, hh, q0 + lo:q0 + hi], start=True, stop=False,
                                       skip_group_check=True)
                        if br == 1:
                            ins = e.matmul(banks[sb][:, lo:hi], lhsT=cbf[:, CB_EP + c * 128:CB_EP + (c + 1) * 128],
                                           rhs=negselT[:, g, lo:hi], start=False, stop=(not diag),
                                           skip_group_check=True)
                        if diag:
                            ins = e.matmul(banks[sb][:, bt:bt + 128], lhsT=ident, rhs=TB, start=False, stop=True,
                                           skip_group_check=True)
                        return ins
                    rd = [RKS[c // 4], RQ[tg], R_cbf] + (R_ns if br == 1 else [])
                    S.op("pe", f, rd, [PB[sb]])

                def B():
                    sb, pi = rs_["sb"], rs_["pi"]
                    pt = ptb[pi]
                    S.op("act", lambda e: e.activation(out=pt[:, lo:hi], in_=banks[sb][:, lo:hi], func=AF.Exp),
                         [PB[sb]], [R_pt[pi]])

                def C():
                    pi = rs_["pi"]
                    pt = ptb[pi]

                    def f2(e):
                        ins = None
                        for tile in range(lo // 128, hi // 128):
                            ins = e.matmul(banks[ob][:, tile * 65:(tile + 1) * 65],
                                           lhsT=pt[:, tile * 128:(tile + 1) * 128], rhs=vaug[:, br - 1, c, g, :],
                                           start=(first and tile == lo // 128), stop=True, skip_group_check=True)
                        return ins
                    S.op("pe", f2, [R_pt[pi], RV[c]], [PB[ob]])
                    if last:
                        finalize(ob, 65, 64, 0, h, br, tg, False, False)
                return (A, B, C)

            def att_steps(tg, br):
                q0 = tg * 512
                heads = []
                for h in range(8):
                    steps = []
                    heads.append(steps)
                    g, hh = h // 4, h % 4
                    ob = 3 + (st["bank"] % 2)
                    st["bank"] += 1
                    if br == 1:
                        chunks = [(c, max(0, c * 128 - q0), 512, (c * 128 >= q0), T0) for c in range(4 * tg + 4)]
                    else:
                        chunks = []
                        for c in range(max(0, 4 * tg - 4), 4 * tg + 4):
                            m = c - (4 * tg - 4)
                            if m <= 3:
                                chunks.append((c, 0, 128 * (m + 1), True, T1))
                            else:
                                chunks.append((c, 128 * (m - 4), 512, True, T0))
                    for ci, (c, lo, hi, diag, TB) in enumerate(chunks):
                        steps.append(att_step(tg, br, h, g, hh, ob, c, lo, hi, diag, TB, ci == 0,
                                              ci == len(chunks) - 1))
                return heads

            def post_thunks(tg):
                q0 = tg * 512
                OAt = OAs[tg % 2]
                R_OAt = R_OAs[tg % 2]
                th = []

                def t0():
                    for tile in range(4):
                        S.op("act", lambda e, tile=tile: e.activation(out=junk[:, :], in_=OAt[:, tile, :],
                                                                      func=AF.Square, accum_out=ssq[:, tile, :]),
                             R_OAt, [R_junk, R_ssq])
                    S.op("act", lambda e: e.activation(out=rt4[:, :, :], in_=ssq[:, :, :], func=AF.Sqrt,
                                                       scale=1.0 / 512, bias=eps_ap), [R_ssq, R_pf], [R_rt4])
                    S.op("dve", lambda e: e.reciprocal(out=rs4[:, :, :], in_=rt4[:, :, :]), [R_rt4], [R_rs4])
                th.append(t0)
                for tile in range(4):
                    def tt(tile=tile):
                        oi = tile % 2
                        S.op("act", lambda e: e.activation(out=OAn[:, oi, :], in_=OAt[:, tile, :], func=AF.Identity,
                                                           scale=rs4[:, tile, :]), R_OAt + [R_rs4], [R_OAn[oi]])
                        pso = banks[7][:, 0:512].bitcast(BF16)[:, oi * 512:(oi + 1) * 512]

                        def ft(e):
                            ins = None
                            for j in range(4):
                                ins = e.transpose(out=pso[:, j * 128:(j + 1) * 128],
                                                  in_=OAn[:, oi, j * 128:(j + 1) * 128], identity=ident)
                            return ins
                        S.op("pe", ft, [R_OAn[oi], R_cbf], [PB[7]])
                        for j in range(4):
                            S.op("dve", lambda e, j=j: e.tensor_scalar(
                                out=oaT[:, j, tile * 128:(tile + 1) * 128], in0=pso[:, j * 128:(j + 1) * 128],
                                scalar1=pf[:, gmc + j:gmc + j + 1], scalar2=None, op0=ALU.mult),
                                [PB[7], R_pf], [R_oaT])
                    th.append(tt)
                for dc in range(8):
                    def to(dc=dc):
                        def f(e):
                            ins = None
                            for ic in range(8):
                                rhs = oaT[:, ic, :] if ic < 4 else ocT[:, ic - 4, q0:q0 + 512]
                                ins = e.matmul(banks[7][:, :], lhsT=wmo[:, ic, dc * 128:(dc + 1) * 128], rhs=rhs,
                                               start=(ic == 0), stop=(ic == 7))
                            return ins
                        S.op("pe", f, [R_wb[0], R_wb[1], R_oaT, ROC[tg]], [PB[7]])
                        S.op("dve", lambda e: e.scalar_tensor_tensor(
                            out=xT[:, dc, q0:q0 + 512], in0=banks[7][:, :], scalar=modG[:, s, l, 1, dc:dc + 1],
                            in1=xT[:, dc, q0:q0 + 512], op0=ALU.mult, op1=ALU.add), [PB[7], RX[dc][tg], R_modAG[l]],
                            [RX[dc][tg]])
                    th.append(to)
                return th

            fillers = []
            for tg in range(4):
                csteps = [cmp_step(tg, h, tail=((lambda tg=tg: select(tg)) if h == 7 else None)) for h in range(8)]
                wheads = att_steps(tg, 2)
                ssteps = [x for hd in att_steps(tg, 1) for x in hd]
                a0 = ssteps[0][0]
                ssteps[0] = ((lambda a0=a0: (sel_mask(), a0())), ssteps[0][1], ssteps[0][2])
                order = [csteps[0], csteps[1]]
                for h in range(8):
                    half = (len(wheads[h]) + 1) // 2
                    order += wheads[h][:half]
                    if h + 2 < 8:
                        order.append(csteps[h + 2])
                    order += wheads[h][half:]
                pipeline(order + ssteps, 3, fillers)
                fillers = post_thunks(tg)
            for f_ in fillers:
                f_()
            if mid_hook is not None:
                mid_hook()

        out_res = []
        for s in range(NSEQ):
            for dc in range(8):
                S.dma("sp", xT[:, dc, :], xT_d[s, dc * 128:(dc + 1) * 128, :], [], RX[dc], "x%d" % dc)
            done = False
            for l in range(L):
                if s == 0 and l + 1 < L:
                    ada_pending.extend((l + 1, gq) for gq in range(36))
                ffn(s, l, 0, norm_done=(l > 0), tail_hook=(lambda stage, slots, s=s, l=l: mixer_norm(s, l, 0, stage, slots[0])))
                if stop == (l, "ffn1"):
                    done = True
                    break
                mixer(s, l, norm_done=True, mid_hook=None)
                if stop == (l, "mix"):
                    done = True
                    break
                ffn(s, l, 1, norm_done=False,
                    tail_hook=((lambda stage, slots, s=s, l=l: ffn_norm(s, l + 1, 0, 0, stage, slots)) if l + 1 < L else None))
                if stop == (l, "ffn2"):
                    done = True
                    break
            if done:
                for dc in range(8):
                    S.dma("sp", out_d[s, dc * 128:(dc + 1) * 128, :], xT[:, dc, :], RX[dc], [], "x%d" % dc)
                continue
            gfc = PO["gf"]
            for tg in range(4):
                t0 = tg * 512
                for dc in range(8):
                    b = dc % 2
                    S.op("act", lambda e, dc=dc, b=b, t0=t0: e.activation(out=sq[b][:, :], in_=xT[:, dc, t0:t0 + 512],
                                                                          func=AF.Square), [RX[dc][tg]], [R_sq[b]])
                    S.op("pe", lambda e, dc=dc, b=b: e.matmul(banks[7][:, :], lhsT=ones, rhs=sq[b][:, :],
                                                              start=(dc == 0), stop=(dc == 7)), [R_sq[b], R_cbf], [PB[7]])
                S.op("act", lambda e: e.activation(out=rt[:, :], in_=banks[7][:, :], func=AF.Sqrt, scale=1.0 / D,
                                                   bias=eps_ap), [PB[7], R_pf], [R_rt])
                S.op("dve", lambda e: e.reciprocal(out=rstd[:, :], in_=rt[:, :]), [R_rt], [R_rstd])
                for dc in range(8):
                    b = dc % 2
                    S.op("dve", lambda e, dc=dc, b=b, t0=t0: e.scalar_tensor_tensor(
                        out=tmpn[b][:, :], in0=xT[:, dc, t0:t0 + 512], scalar=pf[:, gfc + dc:gfc + dc + 1],
                        in1=rstd[:, :], op0=ALU.mult, op1=ALU.mult), [RX[dc][tg], R_pf, R_rstd], [R_tmpn[b]])
                    S.dma("sp", out_d[s, dc * 128:(dc + 1) * 128, t0:t0 + 512], tmpn[b][:, :], [R_tmpn[b]], [],
                          "tmpn%d" % b)
        allres = [r for row in RX for r in row] + R_tmpn
        S.final_wait("sp", allres)
        S.emit()
    return nc


def _consts():
    cb = np.zeros((128, CB_N), np.float32)
    k = np.arange(128)[:, None]
    t = np.arange(128)[None, :]
    cb[:, CB_ID:CB_ID + 128] = np.eye(128, dtype=np.float32)
    cb[:, CB_T0:CB_T0 + 128] = np.where(t >= k, 0.0, NEG)
    cb[:, CB_T1:CB_T1 + 128] = np.where(t < k, 0.0, NEG)
    cb[:, CB_ONE:CB_ONE + 128] = 1.0
    kk = np.arange(2048)[None, :]
    j = np.arange(128)[:, None]
    cb[:, CB_EP:CB_EP + 2048] = ((kk // 64) == j) & (j < 32)
    n = np.arange(128)[:, None]
    cb[:, CB_BC:CB_BC + 2048] = np.where((16 * n + 31 <= kk) & (n < 127), 0.0, NEG)
    jj = np.arange(32)[None, :]
    cb[:, CB_OV:CB_OV + 32] = (n >= 4 * jj - 1) & (n <= 4 * jj + 3) & (n < 127)
    tt = (np.arange(16)[None, :, None] * 128 + np.arange(128)[:, None, None])
    cur = tt // 64
    jb = np.arange(32)[None, None, :]
    fb = np.where(jb > cur, -1e30, np.where((jb == 0) | (jb == cur) | (jb == cur - 1), 1e4, 0.0)).astype(np.float32)
    return cb, fb.reshape(128, 512)


def _prep_shared(L, inp):
    f32 = np.float32
    sh = {}
    sh["w_ada"] = np.ascontiguousarray(inp["w_ada"][:L], dtype=f32)
    wfi = np.asarray(inp["w_ff_in"][:L], dtype=f32)
    g = wfi[..., :DFF].reshape(L, 2, D, 11, 256)
    u = wfi[..., DFF:].reshape(L, 2, D, 11, 256)
    sh["w_ff_in"] = np.ascontiguousarray(np.concatenate([g, u], axis=-1).reshape(L, 2, D, 2 * DFF))
    sh["w_ff_out"] = np.ascontiguousarray(inp["w_ff_out"][:L], dtype=f32)
    wmi = np.asarray(inp["w_mix_in"][:L], dtype=f32)
    q = wmi[:, :, 0:512].reshape(L, D, 8, 64)
    qperm = np.stack([q[:, :, [j, 4 + j], :].reshape(L, D, 128) for j in range(4)], axis=2).reshape(L, D, 512)
    kc, vc, ks, vs, kw, vw = (wmi[:, :, 512 + i * 128:640 + i * 128] for i in range(6))
    gates = wmi[:, :, 1280:1304]
    hc = wmi[:, :, 1304:1816].reshape(L, D, 4, 128)
    bg = wmi[:, :, 1816:2328].reshape(L, D, 4, 128)
    cg = wmi[:, :, 2328:2840].reshape(L, D, 4, 128)
    conv = np.stack([hc, bg, cg], axis=3).reshape(L, D, 1536)
    sh["w_mix_in"] = np.ascontiguousarray(np.concatenate([qperm, kc, vc, ks, kw, conv, vs, vw, gates], axis=-1))
    assert sh["w_mix_in"].shape[-1] == 2840
    sh["w_mix_out"] = np.ascontiguousarray(inp["w_mix_out"][:L], dtype=f32)
    cw = np.zeros((L, 128, CMPW), f32)
    for l in range(L):
        for kind in range(2):
            w1 = np.asarray(inp["w_cmp1"][l, kind], f32).reshape(32, 64, 64).transpose(1, 0, 2).reshape(64, 2048)
            cw[l, :, kind * 2048:(kind + 1) * 2048] = np.concatenate([w1, w1], axis=0)
            pos = np.asarray(inp["cmp_pos"][l, kind], f32).T
            cw[l, :, 4096 + kind * 34:4096 + kind * 34 + 32] = np.concatenate([pos, pos], axis=0)
        cw[l, 0:64, 4164:4228] = inp["w_cmp2"][l, 0]
        cw[l, 0:64, 4228:4292] = inp["w_cmp2"][l, 1]
    sh["cmpw"] = cw
    cb, fb = _consts()
    sh["cbf"] = cb
    return sh, fb


def _prep_pf(L, NSEQ, inp, fb, c_rows):
    PO = pf_offsets(L, NSEQ)
    pf = np.zeros((128, PO["_n"]), np.float32)

    def put(name, arr):
        a = np.asarray(arr, np.float32)
        a = a.reshape(-1, a.shape[-1] // 128, 128)
        a = a.transpose(2, 0, 1).reshape(128, -1)
        pf[:, PO[name]:PO[name] + a.shape[1]] = a

    put("g", inp["g_norm"][:L])
    put("b", inp["b_ada"][:L])
    put("cw", inp["conv_w"][:L])
    put("cb", inp["conv_b"][:L])
    put("gmo", inp["g_mix_out"][:L])
    put("gf", inp["g_final"])
    c = np.asarray(c_rows, np.float32)
    pf[:, PO["c"]:PO["c"] + 8 * NSEQ] = c.reshape(NSEQ, 8, 128).transpose(2, 1, 0).reshape(128, 8 * NSEQ)
    pf[:, PO["eps"]] = EPS
    pf[:, PO["fb"]:PO["fb"] + 512] = fb
    return pf


def run(inp, L=4, NSEQ=2, ncores=NCORES, stop=None, trace=False):
    sh, fb = _prep_shared(L, inp)
    x = np.asarray(inp["x"], np.float32)
    c = np.asarray(inp["c"], np.float32)
    in_maps = []
    for r in range(ncores):
        m = dict(sh)
        xs = x[r * NSEQ:(r + 1) * NSEQ]
        m["xT"] = np.ascontiguousarray(xs.transpose(0, 2, 1))
        m["pf32"] = _prep_pf(L, NSEQ, inp, fb, c[r * NSEQ:(r + 1) * NSEQ])
        in_maps.append(m)
    nc = build_program(L=L, NSEQ=NSEQ, stop=stop)
    res = run_bass_kernel_spmd(nc, in_maps, core_ids=list(range(ncores)), **({"trace": True} if trace else {}))
    out = np.concatenate([np.asarray(r["yT"]).transpose(0, 2, 1) for r in res.results], axis=0)
    return np.ascontiguousarray(out.astype(np.float32)), res


def kernel(x, c, w_ada, b_ada, g_norm, w_ff_in, w_ff_out, w_mix_in, cmp_pos, w_cmp1, w_cmp2, conv_w, conv_b,
           g_mix_out, w_mix_out, g_final):
    inp = dict(x=x, c=c, w_ada=w_ada, b_ada=b_ada, g_norm=g_norm, w_ff_in=w_ff_in, w_ff_out=w_ff_out,
               w_mix_in=w_mix_in, cmp_pos=cmp_pos, w_cmp1=w_cmp1, w_cmp2=w_cmp2, conv_w=conv_w, conv_b=conv_b,
               g_mix_out=g_mix_out, w_mix_out=w_mix_out, g_final=g_final)
    inp = {k: np.asarray(v) for k, v in inp.items()}
    out, _ = run(inp, L=4, NSEQ=2, ncores=NCORES)
    return out
```

```python
import numpy as np
import concourse.bass as bass
import concourse.mybir as mybir
from concourse.bass_utils import run_bass_kernel_spmd
from contextlib import ExitStack

F32 = mybir.dt.float32
BF16 = mybir.dt.bfloat16
AF = mybir.ActivationFunctionType
ALU = mybir.AluOpType

D = 1024
SEQ = 2048
DFF = 2816
NCORES = 8
EPS = 1e-6
NEG = -30000.0
STRICT_WAR = True
CMPW = 2 * 2048 + 2 * 34 + 64 + 64
PF_G = 0


def pf_offsets(L, NSEQ):
    o = {}
    c = 0
    for name, n in (("g", L * 24), ("b", L * 72), ("cw", L * 12), ("cb", L * 4), ("gmo", L * 8), ("gf", 8),
                    ("c", 8 * NSEQ), ("eps", 1), ("fb", 512)):
        o[name] = c
        c += n
    o["_n"] = c
    return o


CB_ID, CB_T0, CB_T1, CB_ONE, CB_EP, CB_BC, CB_OV, CB_N = 0, 128, 256, 384, 512, 2560, 4608, 4640


class Res:
    __slots__ = ("name", "w", "r", "excl")

    def __init__(self, name, excl=False):
        self.name = name
        self.w = None
        self.r = {}
        self.excl = excl


def fence(news, olds):
    acc = {}
    for o in olds:
        if o.w is not None:
            k, v, src = o.w
            if acc.get(k, (0, None))[0] < v:
                acc[k] = (v, src)
        for k, (v, src) in o.r.items():
            if acc.get(k, (0, None))[0] < v:
                acc[k] = (v, src)
    for n in news:
        for k, (v, src) in acc.items():
            if n.r.get(k, (0, None))[0] < v:
                n.r[k] = (v, src)


class Sched:
    ENGS = ("pe", "act", "dve", "pool", "sp")

    def __init__(self, nc, es):
        self.nc = nc
        self.es = es
        self.sems = {}
        self.ops = {n: [] for n in self.ENGS}
        self.cnt = {n: 0 for n in self.ENGS}
        self.seen = {n: {} for n in self.ENGS}
        for n in self.ENGS:
            self.sems[n] = es.enter_context(nc.semaphore("s_" + n))
        self.dma_tot = {}

    def _dma_sem(self, key):
        k = ("dma", key)
        if k not in self.sems:
            self.sems[k] = self.es.enter_context(self.nc.semaphore("d_%d" % len(self.sems)))
            self.dma_tot[k] = 0
        return k

    def _deps(self, en, reads, writes):
        need = {}

        def add(k, v, src, kind):
            if src == en:
                if en == "pe" or (kind == "r" and not STRICT_WAR):
                    return
            if k in self.dma_tot:
                v = max(v, self.dma_tot[k])
            if need.get(k, 0) < v:
                need[k] = v

        for r in reads:
            if r.w is not None:
                add(r.w[0], r.w[1], r.w[2], "w")
            if r.excl:
                for k, (v, src) in r.r.items():
                    add(k, v, src, "r")
        for w in writes:
            if w.w is not None:
                add(w.w[0], w.w[1], w.w[2], "w")
            for k, (v, src) in w.r.items():
                add(k, v, src, "r")
        seen = self.seen[en]
        out = []
        for k, v in need.items():
            if seen.get(k, 0) < v:
                seen[k] = v
                out.append((k, v))
        return out

    @staticmethod
    def _mark(tok, reads, writes):
        k, v, src = tok
        for r in reads:
            r.r[k] = (v, src)
        for w in writes:
            w.w = tok
            w.r = {}

    def op(self, en, fn, reads=(), writes=()):
        waits = self._deps(en, reads, writes)
        self.cnt[en] += 1
        self._mark((en, self.cnt[en], en), reads, writes)
        self.ops[en].append((waits, fn, None))

    def dma(self, q, out_ap, in_ap, reads, writes, key):
        waits = self._deps(q, reads, writes)
        k = self._dma_sem(key)
        self.dma_tot[k] += 16
        self._mark((k, self.dma_tot[k], "dma"), reads, writes)
        self.ops[q].append((waits, (out_ap, in_ap), k))

    def final_wait(self, en, ress):
        waits = self._deps(en, ress, ress)
        self.ops[en].append((waits, None, None))

    def emit(self):
        nc = self.nc
        sems = self.sems
        ops = self.ops
        with nc.Block() as block:
            def body(en, e):
                mysem = sems[en]
                for waits, fn, dk in ops[en]:
                    for k, v in waits:
                        e.wait_ge(sems[k], v)
                    if fn is None:
                        continue
                    if dk is not None:
                        e.dma_start(out=fn[0], in_=fn[1]).then_inc(sems[dk], 16)
                    else:
                        fn(e).then_inc(mysem, 1)

            @block.tensor
            def _(e):
                body("pe", e)

            @block.scalar
            def _(e):
                body("act", e)

            @block.vector
            def _(e):
                body("dve", e)

            @block.gpsimd
            def _(e):
                body("pool", e)

            @block.sync
            def _(e):
                body("sp", e)


class Arena:
    def __init__(self, nc, base=16640, top=229344):
        self.nc = nc
        self.base = base
        self.top = top
        self.cur = base
        self.n = 0

    def alloc(self, name, shape, dtype, at=None):
        esz = 4 if dtype == F32 else 2
        nbytes = int(np.prod(shape[1:])) * esz
        nbytes = (nbytes + 63) // 64 * 64
        if at is None:
            off = self.cur
            self.cur += nbytes
        else:
            off = self.base + at
        assert off + nbytes <= self.top, (name, off, nbytes, self.top)
        self.n += 1
        t = self.nc.alloc_sbuf_tensor_at("%s_%d" % (name, self.n), list(shape), dtype, offset=off)
        return t, (off - self.base) + nbytes


def build_program(L=4, NSEQ=2, stop=None):
    nc = bass.Bass("TRN2", target_bir_lowering=False)
    PO = pf_offsets(L, NSEQ)

    def dram(name, shape, kind="ExternalInput"):
        return nc.dram_tensor(name, list(shape), F32, kind=kind).ap()

    xT_d = dram("xT", [NSEQ, D, SEQ])
    wada_d = dram("w_ada", [L, D, 9216])
    wfi_d = dram("w_ff_in", [L, 2, D, 2 * DFF])
    wfo_d = dram("w_ff_out", [L, 2, DFF, D])
    wmi_d = dram("w_mix_in", [L, D, 2840])
    wmo_d = dram("w_mix_out", [L, D, D])
    cmpw_d = dram("cmpw", [L, 128, CMPW])
    pf_d = dram("pf32", [128, PO["_n"]])
    cbf_d = dram("cbf", [128, CB_N])
    out_d = dram("yT", [NSEQ, D, SEQ], kind="ExternalOutput")

    es = ExitStack()
    with es:
        S = Sched(nc, es)
        ar = Arena(nc)
        xT, _ = ar.alloc("xT", [128, 8, SEQ], F32)
        RX = [[Res("x%d_%d" % (dc, tg)) for tg in range(4)] for dc in range(8)]
        cbf, _ = ar.alloc("cbf", [128, CB_N], BF16)
        R_cbf = Res("cbf")
        pf, _ = ar.alloc("pf", [128, PO["_n"]], F32)
        R_pf = Res("pf")
        modT, _ = ar.alloc("modT", [128, NSEQ, L, 72], F32)
        R_mod = [Res("modT%d" % i) for i in range(L)]
        modA, _ = ar.alloc("modA", [128, NSEQ, L, 3, 8], F32)
        modG, _ = ar.alloc("modG", [128, NSEQ, L, 3, 8], F32)
        R_modAG = [Res("modAG%d" % i) for i in range(L)]
        cact, _ = ar.alloc("cact", [128, 8 * NSEQ], BF16)
        R_cact = Res("cact")
        hT_off = ar.cur - ar.base
        hT, _ = ar.alloc("hT", [128, 8, 1024], BF16)
        RH = [Res("h0"), Res("h1")]
        wbuf_all, _ = ar.alloc("wbuf", [128, 2, 8, 512], BF16)
        R_wb = [Res("wb0"), Res("wb1")]
        rt, _ = ar.alloc("rt", [128, 512], F32)
        R_rt = Res("rt")
        rstd, _ = ar.alloc("rstd", [128, 512], F32)
        R_rstd = Res("rstd")
        tmpn = []
        R_tmpn = []
        for i in range(2):
            t, _ = ar.alloc("tmpn", [128, 512], F32)
            tmpn.append(t)
            R_tmpn.append(Res("tmpn%d" % i))
        sq = []
        R_sq = []
        for i in range(2):
            t, _ = ar.alloc("sq", [128, 512], BF16)
            sq.append(t)
            R_sq.append(Res("sq%d" % i))
        PR = ar.cur - ar.base
        cap = ar.top - ar.base

        aT, o = ar.alloc("aT", [128, 22, 1024], BF16, at=PR)
        RA = [Res("a0"), Res("a1")]
        wout = []
        R_wo = []
        for i in range(2):
            t, o = ar.alloc("wout", [128, 22, 256], BF16, at=o)
            wout.append(t)
            R_wo.append(Res("wo%d" % i))
        sg = []
        R_sg = []
        for i in range(2):
            t, o = ar.alloc("sg", [128, 512], BF16, at=o)
            sg.append(t)
            R_sg.append(Res("sg%d" % i))
        rslot = [rstd]
        R_rslot = [R_rstd]
        for i in range(4):
            t, o = ar.alloc("rslot", [128, 512], F32, at=o)
            rslot.append(t)
            R_rslot.append(Res("rslot%d" % i))
        adab2 = []
        R_ada2 = []
        for i in range(2):
            t, o = ar.alloc("adab2", [128, 8, 256], BF16, at=o)
            adab2.append(t)
            R_ada2.append(Res("ada2_%d" % i))
        assert o <= cap, o
        FFN_RES = RA + R_wo + R_sg + R_rslot[1:] + R_ada2
        adab = []
        R_ada = []
        o2 = PR
        for i in range(2):
            t, o2 = ar.alloc("adab", [128, 8, 1152], BF16, at=o2)
            adab.append(t)
            R_ada.append(Res("ada%d" % i))

        qT, o = ar.alloc("qT", [128, 4, SEQ], BF16, at=PR)
        RQ = [Res("q%d" % i) for i in range(4)]
        kpad, o = ar.alloc("kpad", [128, 2, 2, SEQ], BF16, at=o)
        RKS = [Res("ks%d" % i) for i in range(4)]
        vaug, o = ar.alloc("vaug", [128, 2, 16, 2, 65], BF16, at=o)
        RV = [Res("v%d" % i) for i in range(16)]
        sig, o = ar.alloc("sig", [128, 16, 24], F32, at=o)
        RSIG = [Res("sig%d" % i) for i in range(4)]
        ocT, o = ar.alloc("ocT", [128, 4, SEQ], BF16, at=o)
        ROC = [Res("oc%d" % i) for i in range(4)]
        kcmpT, o = ar.alloc("kcmpT", [128, 2, 128], BF16, at=o)
        R_kcmp = Res("kcmp")
        Rc, o = ar.alloc("Rc", [128, 2, 97], BF16, at=o)
        R_Rc = Res("Rc")
        bvec, o = ar.alloc("bvec", [128, 2], F32, at=o)
        R_bvec = Res("bvec")
        slb = []
        R_sl = []
        for i in range(2):
            t, o = ar.alloc("sl", [128, 128], BF16, at=o)
            slb.append(t)
            R_sl.append(Res("sl%d" % i))
        ATT_P = o
        kcvc, o = ar.alloc("kcvc", [128, 2, SEQ], BF16, at=ATT_P)
        R_kcvc = Res("kcvc")
        A_X = o
        ubuf, o = ar.alloc("ubuf", [128, 4, 514], F32, at=A_X)
        RU = [Res("u%d" % i) for i in range(4)]
        ybuf, o = ar.alloc("ybuf", [128, 4, 512], F32, at=o)
        RY = [Res("y%d" % i) for i in range(4)]
        hcs, R_hcs = tmpn[0], R_tmpn[0]
        v0b, R_v0 = tmpn[1], R_tmpn[1]
        assert o <= cap, o
        A_RES = RU + RY
        cmpw, oc_ = ar.alloc("cmpw", [128, CMPW], BF16, at=A_X)
        R_cmpw = Res("cmpw")
        OA, o = ar.alloc("OA", [128, 4, 512], F32, at=ATT_P)
        OA2, _ = ar.alloc("OA2", [128, 4, 512], F32, at=hT_off)
        R_OA = Res("OA")
        negselT, o = ar.alloc("negselT", [128, 2, 512], BF16, at=o)
        R_ns = [Res("ns")]
        ptb = []
        R_pt = []
        for i in range(5):
            t, o = ar.alloc("pt", [128, 512], BF16, at=o)
            ptb.append(t)
            R_pt.append(Res("pt%d" % i))
        oaT, o = ar.alloc("oaT", [128, 4, 512], BF16, at=o)
        R_oaT = Res("oaT")
        OAn, o = ar.alloc("OAn", [128, 2, 512], BF16, at=o)
        R_OAn = [Res("OAn0"), Res("OAn1")]
        imp, o = ar.alloc("imp", [128, 2, 4, 32], F32, at=o)
        R_imp = Res("imp")
        nsb, o = ar.alloc("nsb", [128, 2, 4, 32], BF16, at=o)
        R_nsb = Res("nsb")
        top8, o = ar.alloc("top8", [128, 4, 8], F32, at=o)
        R_top8 = Res("top8")
        smalls = []
        R_small = []
        for i in range(6):
            t, o = ar.alloc("sm", [128, 4, 1], F32, at=o)
            smalls.append(t)
            R_small.append(Res("sm%d" % i))
        junk, o = ar.alloc("junk", [128, 512], BF16, at=o)
        R_junk = Res("junk")
        assert o <= cap, o
        B_RES = [R_OA] + R_ns + R_pt + R_OAn + [R_oaT, R_imp, R_nsb, R_top8, R_junk] + R_small
        ATT_RES = (RQ + RKS + RV + RSIG + ROC + [R_kcmp, R_Rc, R_bvec, R_kcvc, R_cmpw] + R_sl + A_RES + B_RES)

        banks = [nc.alloc_psum_tensor("bk%d" % i, [128, 512], F32) for i in range(8)]
        PB = [Res("pb%d" % i, excl=True) for i in range(8)]

        ident = cbf[:, CB_ID:CB_ID + 128]
        T0 = cbf[:, CB_T0:CB_T0 + 128]
        T1 = cbf[:, CB_T1:CB_T1 + 128]
        ones = cbf[:, CB_ONE:CB_ONE + 128]
        eps_ap = pf[:, PO["eps"]:PO["eps"] + 1]

        def wb(i):
            return wbuf_all[:, i]

        for c0 in range(0, CB_N, 2048):
            c1 = min(CB_N, c0 + 2048)
            S.dma("pool", cbf[:, c0:c1], cbf_d[:, c0:c1], [], [R_cbf], "cbf")
        S.dma("sp", pf[:, :], pf_d[:, :], [], [R_pf], "pf")
        S.op("act", lambda e: e.activation(out=cact[:, :], in_=pf[:, PO["c"]:PO["c"] + 8 * NSEQ], func=AF.Silu),
             [R_pf], [R_cact])
        gi = 0
        for l in range(1):
            for cg in range(8):
                ab = adab[gi % 2]
                Rab = R_ada[gi % 2]
                src = wada_d[l].rearrange("(kc p) c -> p kc c", p=128)[:, :, cg * 1152:(cg + 1) * 1152]
                S.dma("pool", ab[:, :, :], src, [], [Rab], "ada%d" % (gi % 2))
                bk = gi % 2

                def f(e, ab=ab, bk=bk):
                    ins = None
                    for ch in range(9):
                        for kc in range(8):
                            ins = e.matmul(banks[bk][:, ch * NSEQ:(ch + 1) * NSEQ], lhsT=ab[:, kc, ch * 128:(ch + 1) * 128],
                                           rhs=cact[:, kc * NSEQ:(kc + 1) * NSEQ], start=(kc == 0), stop=(kc == 7),
                                           skip_group_check=True)
                    return ins
                S.op("pe", f, [Rab, R_cact], [PB[bk]])
                for s in range(NSEQ):
                    psv = banks[bk][:, 0:9 * NSEQ].rearrange("p (a b) -> p a b", b=NSEQ)[:, :, s:s + 1]
                    bcol = PO["b"] + l * 72 + cg * 9
                    S.op("dve", lambda e, psv=psv, s=s, l=l, cg=cg, bcol=bcol: e.tensor_tensor(
                        out=modT[:, s, l, cg * 9:(cg + 1) * 9].unsqueeze(2), in0=psv,
                        in1=pf[:, bcol:bcol + 9].unsqueeze(2), op=ALU.add), [PB[bk], R_pf], [R_mod[l]])
                gi += 1

        def mod_derived(l):
            for s in range(NSEQ):
                for sub in range(3):
                    gcol = PO["g"] + (l * 3 + sub) * 8
                    S.op("dve", lambda e, s=s, l=l, sub=sub, gcol=gcol: e.scalar_tensor_tensor(
                        out=modA[:, s, l, sub, :], in0=modT[:, s, l, sub * 24 + 8:sub * 24 + 16], scalar=1.0,
                        in1=pf[:, gcol:gcol + 8], op0=ALU.add, op1=ALU.mult), [R_mod[l], R_pf], [R_modAG[l]])
                    gm = 1.0 if sub == 1 else 0.5
                    S.op("dve", lambda e, s=s, l=l, sub=sub, gm=gm: e.tensor_scalar(
                        out=modG[:, s, l, sub, :], in0=modT[:, s, l, sub * 24 + 16:sub * 24 + 24], scalar1=gm,
                        scalar2=None, op0=ALU.mult), [R_mod[l]], [R_modAG[l]])

        mod_derived(0)
        ada_pending = []

        def ada_group(l, gidx):
            i = st["ada"] % 2
            st["ada"] += 1
            ab = adab2[i]
            src = wada_d[l].rearrange("(kc p) c -> p kc c", p=128)[:, :, gidx * 256:(gidx + 1) * 256]
            S.dma("pool", ab[:, :, :], src, [], [R_ada2[i]], "ada2_%d" % i)

            def f(e):
                ins = None
                for ch in range(2):
                    for kc in range(8):
                        ins = e.matmul(banks[7][:, ch * NSEQ:(ch + 1) * NSEQ], lhsT=ab[:, kc, ch * 128:(ch + 1) * 128],
                                       rhs=cact[:, kc * NSEQ:(kc + 1) * NSEQ], start=(kc == 0), stop=(kc == 7),
                                       skip_group_check=True)
                return ins
            S.op("pe", f, [R_ada2[i], R_cact], [PB[7]])
            for s in range(NSEQ):
                psv = banks[7][:, 0:2 * NSEQ].rearrange("p (a b) -> p a b", b=NSEQ)[:, :, s:s + 1]
                bcol = PO["b"] + l * 72 + gidx * 2
                S.op("dve", lambda e, psv=psv, s=s, bcol=bcol: e.tensor_tensor(
                    out=modT[:, s, l, gidx * 2:(gidx + 1) * 2].unsqueeze(2), in0=psv,
                    in1=pf[:, bcol:bcol + 2].unsqueeze(2), op=ALU.add), [PB[7], R_pf], [R_mod[l]])
            if gidx == 35:
                mod_derived(l)

        def ada_hook(n=2):
            for _ in range(n):
                if ada_pending:
                    ada_group(*ada_pending.pop(0))

        st = {"wb": 0, "wo": 0, "bank": 0, "sb": 0, "pt": 0, "ada": 0}

        def next_wb():
            i = st["wb"] % 2
            st["wb"] += 1
            return i

        def prenorm(s, l, sub, tok0, dst, R_dst):
            norm_stats(tok0, 0)
            norm_apply(s, l, sub, tok0, 0, dst, R_dst)

        def norm_stats(tok0, slot):
            tg = tok0 // 512
            rstd = rslot[slot]
            R_rstd = R_rslot[slot]
            for dc in range(8):
                b = dc % 2
                S.op("act", lambda e, dc=dc, b=b: e.activation(out=sq[b][:, :], in_=xT[:, dc, tok0:tok0 + 512],
                                                              func=AF.Square), [RX[dc][tg]], [R_sq[b]])
                S.op("pe", lambda e, dc=dc, b=b: e.matmul(banks[7][:, :], lhsT=ones, rhs=sq[b][:, :], start=(dc == 0),
                                                         stop=(dc == 7)), [R_sq[b], R_cbf], [PB[7]])
            S.op("act", lambda e: e.activation(out=rt[:, :], in_=banks[7][:, :], func=AF.Sqrt, scale=1.0 / D,
                                               bias=eps_ap), [PB[7], R_pf], [R_rt])
            S.op("dve", lambda e: e.reciprocal(out=rstd[:, :], in_=rt[:, :]), [R_rt], [R_rstd])

        def norm_apply(s, l, sub, tok0, slot, dst, R_dst):
            tg = tok0 // 512
            rstd = rslot[slot]
            R_rstd = R_rslot[slot]
            for dc in range(8):
                b = dc % 2
                S.op("dve", lambda e, dc=dc, b=b: e.scalar_tensor_tensor(
                    out=tmpn[b][:, :], in0=xT[:, dc, tok0:tok0 + 512], scalar=modA[:, s, l, sub, dc:dc + 1],
                    in1=rstd[:, :], op0=ALU.mult, op1=ALU.mult), [RX[dc][tg], R_modAG[l], R_rstd], [R_tmpn[b]])
                S.op("act", lambda e, dc=dc, b=b: e.activation(
                    out=dst(dc), in_=tmpn[b][:, :], func=AF.Identity,
                    bias=modT[:, s, l, sub * 24 + dc:sub * 24 + dc + 1], scale=1.0), [R_tmpn[b], R_mod[l]], [R_dst])

        def ffn_norm(s, l, j, half, stage=None, slots=(0, 0)):
            sub = 0 if j == 0 else 2
            for tgi in range(2):
                tok0 = half * 1024 + tgi * 512
                if stage in (None, "stats"):
                    norm_stats(tok0, slots[tgi])
                if stage in (None, "apply"):
                    norm_apply(s, l, sub, tok0, slots[tgi],
                               lambda dc, tgi=tgi: hT[:, dc, tgi * 512:(tgi + 1) * 512], RH[tgi])

        def ffn(s, l, j, norm_done=False, tail_hook=None):
            sub = 0 if j == 0 else 2
            fence(FFN_RES, ATT_RES + R_ada)
            if not norm_done:
                ffn_norm(s, l, j, 0)
            for half in range(2):
                for fg in range(11):
                    if fg == 2:
                        if half == 0:
                            ffn_norm(s, l, j, 1, "stats", (1, 2))
                        elif tail_hook is not None:
                            tail_hook("stats", (3, 4))
                    wi = next_wb()
                    w = wb(wi)
                    src = wfi_d[l, j].rearrange("(kc p) c -> p kc c", p=128)[:, :, fg * 512:(fg + 1) * 512]
                    S.dma("pool", w[:, :, :], src, [], [R_wb[wi]], "wb%d" % wi)
                    for tgi in range(2):
                        bs = 4 * (st["bank"] % 2)
                        st["bank"] += 1
                        for oc in range(4):
                            def f(e, w=w, oc=oc, tgi=tgi, bs=bs):
                                ins = None
                                for kc in range(8):
                                    ins = e.matmul(banks[bs + oc][:, :], lhsT=w[:, kc, oc * 128:(oc + 1) * 128],
                                                   rhs=hT[:, kc, tgi * 512:(tgi + 1) * 512], start=(kc == 0),
                                                   stop=(kc == 7))
                                return ins
                            S.op("pe", f, [R_wb[wi], RH[tgi]], [PB[bs + oc]])
                        for i in range(2):
                            S.op("act", lambda e, i=i, bs=bs: e.activation(out=sg[i][:, :], in_=banks[bs + i][:, :],
                                                                           func=AF.Silu), [PB[bs + i]], [R_sg[i]])
                            S.op("dve", lambda e, i=i, bs=bs, fg=fg, tgi=tgi: e.tensor_tensor(
                                out=aT[:, 2 * fg + i, tgi * 512:(tgi + 1) * 512], in0=sg[i][:, :],
                                in1=banks[bs + 2 + i][:, :], op=ALU.mult), [R_sg[i], PB[bs + 2 + i]], [RA[tgi]])
                    if j == 0 and s == 0:
                        ada_hook(2)
                if half == 0:
                    ffn_norm(s, l, j, 1, "apply", (1, 2))
                elif tail_hook is not None:
                    tail_hook("apply", (3, 4))
                for dmp in range(4):
                    oi = st["wo"] % 2
                    st["wo"] += 1
                    wo = wout[oi]
                    src = wfo_d[l, j].rearrange("(fc p) c -> p fc c", p=128)[:, :, dmp * 256:(dmp + 1) * 256]
                    S.dma("pool", wo[:, :, :], src, [], [R_wo[oi]], "wo%d" % oi)
                    for dmi in range(2):
                        dc = dmp * 2 + dmi
                        for tgi in range(2):
                            bk = st["sb"] % 8
                            st["sb"] += 1
                            tg = half * 2 + tgi

                            def f(e, wo=wo, dmi=dmi, tgi=tgi, bk=bk):
                                ins = None
                                for fc in range(22):
                                    ins = e.matmul(banks[bk][:, :], lhsT=wo[:, fc, dmi * 128:(dmi + 1) * 128],
                                                   rhs=aT[:, fc, tgi * 512:(tgi + 1) * 512], start=(fc == 0),
                                                   stop=(fc == 21))
                                return ins
                            S.op("pe", f, [R_wo[oi], RA[tgi]], [PB[bk]])
                            S.op("dve", lambda e, dc=dc, tg=tg, bk=bk: e.scalar_tensor_tensor(
                                out=xT[:, dc, tg * 512:(tg + 1) * 512], in0=banks[bk][:, :],
                                scalar=modG[:, s, l, sub, dc:dc + 1], in1=xT[:, dc, tg * 512:(tg + 1) * 512],
                                op0=ALU.mult, op1=ALU.add), [PB[bk], RX[dc][tg], R_modAG[l]], [RX[dc][tg]])

        def mixer_norm(s, l, tg, stage=None, slot=0):
            hb = tg % 2
            if stage in (None, "stats"):
                norm_stats(tg * 512, slot)
            if stage in (None, "apply"):
                norm_apply(s, l, 1, tg * 512, slot, lambda dc, hb=hb: hT[:, dc, hb * 512:(hb + 1) * 512], RH[hb])

        def mixer(s, l, norm_done=False, mid_hook=None):
            fence(ATT_RES, FFN_RES + R_ada)
            cwc = PO["cw"] + l * 12
            cbc = PO["cb"] + l * 4
            gmc = PO["gmo"] + l * 8
            S.op("dve", lambda e: e.memset(vaug[:, :, :, :, 64:65], 1.0), [], RV)
            S.op("dve", lambda e: e.memset(Rc[:, :, :], 0.0), [], [R_Rc])
            for g in range(2):
                S.op("dve", lambda e, g=g: e.tensor_copy(out=Rc[:, g, 0:32], in_=cbf[:, CB_OV:CB_OV + 32]),
                     [R_cbf], [R_Rc])
            S.op("dve", lambda e: e.memset(Rc[:, :, 96:97], 1.0), [], [R_Rc])
            S.op("dve", lambda e: e.memset(kcmpT[:, :, :], 0.0), [], [R_kcmp])
            S.op("dve", lambda e: e.memset(kpad[64:128, :, 0, :], 0.0), [], RKS)
            S.op("dve", lambda e: e.memset(kpad[0:64, :, 1, :], 0.0), [], RKS)
            S.op("dve", lambda e: e.memset(ubuf[:, :, 0:2], 0.0), [], RU)

            def load_w(c0, n):
                wi = next_wb()
                w = wb(wi)
                src = wmi_d[l].rearrange("(kc p) c -> p kc c", p=128)[:, :, c0:c0 + n]
                S.dma("pool", w[:, :, 0:n], src, [], [R_wb[wi]], "wb%d" % wi)
                return w, R_wb[wi]

            def proj(w, Rw, col, hb, bk):
                def f(e):
                    ins = None
                    for kc in range(8):
                        ins = e.matmul(banks[bk][:, :], lhsT=w[:, kc, col:col + 128],
                                       rhs=hT[:, kc, hb * 512:(hb + 1) * 512], start=(kc == 0), stop=(kc == 7))
                    return ins
                S.op("pe", f, [Rw, RH[hb]], [PB[bk]])

            def phaseA(tg):
                hb = tg % 2
                cs = slice(tg * 512, (tg + 1) * 512)
                if tg == 0 and not norm_done:
                    mixer_norm(s, l, 0)
                w, Rw = load_w(0, 512)
                for c in range(4):
                    proj(w, Rw, c * 128, hb, c)
                for c in range(4):
                    if c % 2 == 0:
                        S.op("act", lambda e, c=c: e.activation(out=qT[:, c, cs], in_=banks[c][:, :], func=AF.Identity,
                                                                scale=0.125), [PB[c]], [RQ[tg]])
                    else:
                        S.op("dve", lambda e, c=c: e.tensor_scalar(out=qT[:, c, cs], in0=banks[c][:, :], scalar1=0.125,
                                                                   scalar2=None, op0=ALU.mult), [PB[c]], [RQ[tg]])
                w, Rw = load_w(512, 512)
                for c in range(4):
                    proj(w, Rw, c * 128, hb, 4 + c)
                S.op("act", lambda e: e.copy(out=kcvc[:, 0, cs], in_=banks[4][:, :]), [PB[4]], [R_kcvc])
                S.op("dve", lambda e: e.tensor_copy(out=kcvc[:, 1, cs], in_=banks[5][:, :]), [PB[5]], [R_kcvc])
                for kind in range(2):
                    S.op("act", lambda e, kind=kind: e.copy(out=kpad[0:64, kind, 0, cs], in_=banks[6 + kind][0:64, :]),
                         [PB[6 + kind]], [RKS[tg]])
                    S.op("dve", lambda e, kind=kind: e.tensor_copy(out=kpad[64:128, kind, 1, cs],
                                                                   in_=banks[6 + kind][64:128, :]),
                         [PB[6 + kind]], [RKS[tg]])
                if tg < 3:
                    mixer_norm(s, l, tg + 1)
                for cc in range(4):
                    w, Rw = load_w(1024 + cc * 384, 384)
                    b0 = (cc * 3) % 8
                    bh, bb, bc_ = b0, (b0 + 1) % 8, (b0 + 2) % 8
                    proj(w, Rw, 0, hb, bh)
                    proj(w, Rw, 128, hb, bb)
                    proj(w, Rw, 256, hb, bc_)
                    S.op("act", lambda e, bh=bh: e.copy(out=hcs[:, :], in_=banks[bh][:, :]), [PB[bh]], [R_hcs])
                    S.op("dve", lambda e, cc=cc, bc_=bc_: e.tensor_tensor(out=ubuf[:, cc, 2:514], in0=hcs[:, :],
                                                                          in1=banks[bc_][:, :], op=ALU.mult),
                         [R_hcs, PB[bc_]], [RU[cc]])
                    S.op("act", lambda e, cc=cc: e.activation(
                        out=v0b[:, :], in_=ubuf[:, cc, 2:514], func=AF.Identity,
                        scale=pf[:, cwc + 8 + cc:cwc + 9 + cc], bias=pf[:, cbc + cc:cbc + cc + 1]),
                        [RU[cc], R_pf], [R_v0])
                    S.op("dve", lambda e, cc=cc: e.scalar_tensor_tensor(
                        out=v0b[:, :], in0=ubuf[:, cc, 1:513], scalar=pf[:, cwc + 4 + cc:cwc + 5 + cc], in1=v0b[:, :],
                        op0=ALU.mult, op1=ALU.add), [RU[cc], R_pf, R_v0], [R_v0])
                    S.op("dve", lambda e, cc=cc: e.scalar_tensor_tensor(
                        out=v0b[:, :], in0=ubuf[:, cc, 0:512], scalar=pf[:, cwc + cc:cwc + cc + 1], in1=v0b[:, :],
                        op0=ALU.mult, op1=ALU.add), [RU[cc], R_pf, R_v0], [R_v0])
                    S.op("dve", lambda e, cc=cc, bb=bb: e.tensor_tensor(out=ybuf[:, cc, :], in0=v0b[:, :],
                                                                        in1=banks[bb][:, :], op=ALU.mult),
                         [R_v0, PB[bb]], [RY[cc]])
                    S.op("act", lambda e, cc=cc: e.copy(out=ubuf[:, cc, 0:2], in_=ubuf[:, cc, 512:514]),
                         [RU[cc]], [RU[cc]])
                w, Rw = load_w(2560, 280)
                for tile in range(4):
                    ti = tg * 4 + tile
                    bk = 3 + tile

                    def f(e, w=w, tile=tile, bk=bk):
                        ins = None
                        for kc in range(8):
                            ins = e.matmul(banks[bk][:, 0:280],
                                           lhsT=hT[:, kc, hb * 512 + tile * 128:hb * 512 + (tile + 1) * 128],
                                           rhs=w[:, kc, 0:280], start=(kc == 0), stop=(kc == 7))
                        return ins
                    S.op("pe", f, [Rw, RH[hb]], [PB[bk]])
                    S.op("act", lambda e, ti=ti, bk=bk: e.copy(
                        out=vaug[:, 0, ti, :, 0:64], in_=banks[bk][:, 0:128].rearrange("p (a b) -> p a b", a=2)),
                        [PB[bk]], [RV[ti]])
                    S.op("dve", lambda e, ti=ti, bk=bk: e.tensor_copy(
                        out=vaug[:, 1, ti, :, 0:64], in_=banks[bk][:, 128:256].rearrange("p (a b) -> p a b", a=2)),
                        [PB[bk]], [RV[ti]])
                    S.op("act", lambda e, ti=ti, bk=bk: e.activation(out=sig[:, ti, :], in_=banks[bk][:, 256:280],
                                                                     func=AF.Sigmoid), [PB[bk]], [RSIG[tg]])
                for cc in range(4):
                    b = cc % 2
                    S.op("act", lambda e, cc=cc, b=b: e.activation(out=sq[b][:, :], in_=ybuf[:, cc, :], func=AF.Square),
                         [RY[cc]], [R_sq[b]])
                    S.op("pe", lambda e, cc=cc, b=b: e.matmul(banks[7][:, :], lhsT=ones, rhs=sq[b][:, :],
                                                              start=(cc == 0), stop=(cc == 3)),
                         [R_sq[b], R_cbf], [PB[7]])
                S.op("act", lambda e: e.activation(out=rt[:, :], in_=banks[7][:, :], func=AF.Sqrt, scale=1.0 / 512,
                                                   bias=eps_ap), [PB[7], R_pf], [R_rt])
                S.op("dve", lambda e: e.reciprocal(out=rstd[:, :], in_=rt[:, :]), [R_rt], [R_rstd])
                for cc in range(4):
                    S.op("dve", lambda e, cc=cc: e.scalar_tensor_tensor(
                        out=ocT[:, cc, cs], in0=ybuf[:, cc, :], scalar=pf[:, gmc + 4 + cc:gmc + 5 + cc],
                        in1=rstd[:, :], op0=ALU.mult, op1=ALU.mult), [RY[cc], R_pf, R_rstd], [ROC[tg]])

            for tg_ in range(4):
                phaseA(tg_)

            fence([R_cmpw], A_RES)
            for c0 in range(0, CMPW, 2048):
                c1 = min(CMPW, c0 + 2048)
                S.dma("pool", cmpw[:, c0:c1], cmpw_d[l, :, c0:c1], [], [R_cmpw], "cmpw")
            w2k = cmpw[0:64, 4164:4228]
            w2v = cmpw[0:64, 4228:4292]
            for kind in range(2):
                w1 = cmpw[:, kind * 2048:(kind + 1) * 2048]
                pos = cmpw[:, 4096 + kind * 34:4096 + (kind + 1) * 34]

                def fb(e, w1=w1, pos=pos, kind=kind):
                    ins = None
                    for lp in range(32):
                        ins = e.matmul(banks[2][0:64, kind * 2:kind * 2 + 2], lhsT=w1[0:64, lp * 64:(lp + 1) * 64],
                                       rhs=pos[0:64, lp:lp + 2], start=(lp == 0), stop=(lp == 31), skip_group_check=True)
                    return ins
                S.op("pe", fb, [R_cmpw], [PB[2]])
                S.op("dve", lambda e, kind=kind: e.tensor_copy(out=bvec[0:64, kind:kind + 1],
                                                               in_=banks[2][0:64, kind * 2:kind * 2 + 1]),
                     [PB[2]], [R_bvec])
                for g in range(2):
                    rows = slice(64 * g, 64 * g + 64)
                    sl = slb[g]

                    def fc(e, w1=w1, kind=kind, g=g, rows=rows):
                        ins = None
                        for lp in range(32):
                            ins = e.matmul(banks[g][0:64, 0:127], lhsT=w1[rows, lp * 64:(lp + 1) * 64],
                                           rhs=kcvc[rows, kind, lp:lp + 16 * 126 + 1:16], start=(lp == 0), stop=(lp == 31))
                        return ins
                    S.op("pe", fc, [R_cmpw, R_kcvc], [PB[g]])
                    S.op("act", lambda e, g=g, sl=sl, kind=kind: e.activation(
                        out=sl[0:64, 0:127], in_=banks[g][0:64, 0:127], func=AF.Silu, bias=bvec[0:64, kind:kind + 1],
                        scale=1.0), [PB[g], R_bvec], [R_sl[g]])
                    if kind == 0:
                        S.op("pe", lambda e, sl=sl, rows=rows: e.matmul(banks[3][rows, 0:127], lhsT=w2k,
                                                                        rhs=sl[0:64, 0:127], start=True, stop=True,
                                                                        skip_group_check=True),
                             [R_cmpw, R_sl[g]], [PB[3]])
                        S.op("dve", lambda e, rows=rows, g=g: e.tensor_copy(out=kcmpT[rows, g, 0:127],
                                                                            in_=banks[3][rows, 0:127]),
                             [PB[3]], [R_kcmp])
                    else:
                        S.op("pe", lambda e, sl=sl, g=g: e.matmul(banks[3][0:127, 128 + g * 64:192 + g * 64],
                                                                  lhsT=sl[0:64, 0:127], rhs=w2v, start=True, stop=True,
                                                                  skip_group_check=True),
                             [R_cmpw, R_sl[g]], [PB[3]])
                        S.op("dve", lambda e, g=g: e.tensor_copy(out=Rc[0:127, g, 32:96],
                                                                 in_=banks[3][0:127, 128 + g * 64:192 + g * 64]),
                             [PB[3]], [R_Rc])

            fence(B_RES, A_RES + [R_cmpw, R_kcvc])
            S.op("dve", lambda e: e.memset(negselT[:, :, :], 0.0), [], R_ns)
            wmo = wbuf_all.rearrange("p a k c -> p (a k c)").rearrange("p (k c) -> p k c", c=1024)
            for hf in range(2):
                src = wmo_d[l].rearrange("(kc p) c -> p kc c", p=128)[:, hf * 4:(hf + 1) * 4, :]
                S.dma("pool", wmo[:, hf * 4:(hf + 1) * 4, :], src, [], [R_wb[hf]], "wb%d" % hf)
            st["wb"] = 0
            rz, zc, rzg, ssq, rt4, rs4 = smalls
            R_rz, R_zc, R_rzg, R_ssq, R_rt4, R_rs4 = R_small
            fbc = PO["fb"]

            def next_sb():
                i = st["sb"] % 3
                st["sb"] += 1
                return i

            def next_pt():
                i = st["pt"] % 5
                st["pt"] += 1
                return i

            def next_sb5():
                i = (0, 1, 2, 5, 6)[st["sb"] % 5]
                st["sb"] += 1
                return i

            OAs = [OA2, OA]
            R_OAs = [[RH[0], RH[1]], [R_OA]]

            def finalize(bk, width, zcol, vcol, h, br, tg, first, clampz):
                OAt = OAs[tg % 2]
                R_OAt = R_OAs[tg % 2]
                view = banks[bk][:, 0:4 * width].rearrange("p (a b) -> p a b", b=width)
                if clampz:
                    S.op("dve", lambda e: e.tensor_scalar(out=zc[:, :, :], in0=view[:, :, zcol:zcol + 1], scalar1=1e-30,
                                                          scalar2=None, op0=ALU.max), [PB[bk]], [R_zc])
                    S.op("dve", lambda e: e.reciprocal(out=rz[:, :, :], in_=zc[:, :, :]), [R_zc], [R_rz])
                else:
                    S.op("dve", lambda e: e.reciprocal(out=rz[:, :, :], in_=view[:, :, zcol:zcol + 1]), [PB[bk]], [R_rz])
                gcol = h * 3 + br
                S.op("dve", lambda e: e.tensor_tensor(out=rzg[:, :, :], in0=rz[:, :, :],
                                                      in1=sig[:, tg * 4:(tg + 1) * 4, gcol:gcol + 1], op=ALU.mult),
                     [R_rz, RSIG[tg]], [R_rzg])
                for tile in range(4):
                    if first:
                        S.op("dve", lambda e, tile=tile: e.tensor_scalar(
                            out=OAt[:, tile, h * 64:(h + 1) * 64], in0=view[:, tile, vcol:vcol + 64],
                            scalar1=rzg[:, tile, :], scalar2=None, op0=ALU.mult), [PB[bk], R_rzg], R_OAt)
                    else:
                        S.op("dve", lambda e, tile=tile: e.scalar_tensor_tensor(
                            out=OAt[:, tile, h * 64:(h + 1) * 64], in0=view[:, tile, vcol:vcol + 64],
                            scalar=rzg[:, tile, :], in1=OAt[:, tile, h * 64:(h + 1) * 64], op0=ALU.mult, op1=ALU.add),
                            [PB[bk], R_rzg] + R_OAt, R_OAt)

            def pipeline(steps, skew, fillers=(), every=2, start_at=3):
                n = len(steps)
                fi = 0
                for i in range(n + skew):
                    if i < n:
                        steps[i][0]()
                        steps[i][1]()
                    if i - skew >= 0:
                        steps[i - skew][2]()
                    if fi < len(fillers) and i >= start_at and (i - start_at) % every == 0:
                        fillers[fi]()
                        fi += 1
                while fi < len(fillers):
                    fillers[fi]()
                    fi += 1

            def next_s4():
                i = (0, 1, 2, 5)[st["sb"] % 4]
                st["sb"] += 1
                return i

            def cmp_step(tg, h, tail=None):
                q0 = tg * 512
                g, hh = h // 4, h % 4
                cb_ = 6
                rs_ = {}

                def A():
                    sb = rs_["sb"] = next_s4()
                    rs_["pi"] = next_pt()

                    def f(e):
                        e.matmul(banks[sb][:, :], lhsT=kcmpT[:, g, :], rhs=qT[:, hh, q0:q0 + 512], start=True,
                                 stop=False)
                        return e.matmul(banks[sb][:, :], lhsT=ident, rhs=cbf[:, CB_BC + q0:CB_BC + q0 + 512],
                                        start=False, stop=True)
                    S.op("pe", f, [R_kcmp, RQ[tg], R_cbf], [PB[sb]])

                def B():
                    sb, pi = rs_["sb"], rs_["pi"]
                    pt = ptb[pi]
                    S.op("act", lambda e: e.activation(out=pt[:, :], in_=banks[sb][:, :], func=AF.Exp),
                         [PB[sb]], [R_pt[pi]])

                def C():
                    pi = rs_["pi"]
                    pt = ptb[pi]

                    def f2(e):
                        ins = None
                        for tile in range(4):
                            ins = e.matmul(banks[cb_][:, tile * 97:(tile + 1) * 97],
                                           lhsT=pt[:, tile * 128:(tile + 1) * 128], rhs=Rc[:, g, :],
                                           start=(tile == 0), stop=(tile == 3), skip_group_check=True)
                        return ins
                    S.op("pe", f2, [R_pt[pi], R_Rc], [PB[cb_]])
                    finalize(cb_, 97, 96, 32, h, 0, tg, True, True)
                    view = banks[cb_][:, 0:4 * 97].rearrange("p (a b) -> p a b", b=97)
                    for tile in range(4):
                        if hh == 0:
                            S.op("dve", lambda e, tile=tile: e.tensor_scalar(
                                out=imp[:, g, tile, :], in0=view[:, tile, 0:32], scalar1=rz[:, tile, :],
                                scalar2=None, op0=ALU.mult), [PB[cb_], R_rz], [R_imp])
                        else:
                            S.op("dve", lambda e, tile=tile: e.scalar_tensor_tensor(
                                out=imp[:, g, tile, :], in0=view[:, tile, 0:32], scalar=rz[:, tile, :],
                                in1=imp[:, g, tile, :], op0=ALU.mult, op1=ALU.add), [PB[cb_], R_rz, R_imp],
                                [R_imp])
                    if tail is not None:
                        tail()
                return (A, B, C)

            def select(tg):
                for g in range(2):
                    S.op("dve", lambda e, g=g: e.tensor_tensor(
                        out=imp[:, g, :, :], in0=imp[:, g, :, :],
                        in1=pf[:, fbc + tg * 128:fbc + (tg + 1) * 128].rearrange("p (a b) -> p a b", b=32), op=ALU.add),
                        [R_imp, R_pf], [R_imp])
                    for tile in range(4):
                        S.op("dve", lambda e, tile=tile, g=g: e.max(out=top8[:, tile, :], in_=imp[:, g, tile, :]),
                             [R_imp], [R_top8])
                        S.op("dve", lambda e, tile=tile, g=g: e.tensor_scalar(
                            out=nsb[:, g, tile, :], in0=imp[:, g, tile, :], scalar1=top8[:, tile, 7:8], scalar2=NEG,
                            op0=ALU.is_lt, op1=ALU.mult), [R_imp, R_top8], [R_nsb])

            pst = banks[7][0:32, 0:512].bitcast(BF16)

            def sel_mask():
                def ftn(e):
                    ins = None
                    for g in range(2):
                        for tile in range(4):
                            ins = e.transpose(out=pst[:, (g * 4 + tile) * 128:(g * 4 + tile + 1) * 128],
                                              in_=nsb[:, g, tile, :], identity=ident)
                    return ins
                S.op("pe", ftn, [R_nsb, R_cbf], [PB[7]])
                S.op("act", lambda e: e.copy(out=negselT[0:32, :, :], in_=pst.rearrange("p (g c) -> p g c", g=2)),
                     [PB[7]], R_ns)

            def att_step(tg, br, h, g, hh, ob, c, lo, hi, diag, TB, first, last):
                q0 = tg * 512
                rs_ = {}
                bt = lo if TB is T0 else hi - 128

                def A():
                    sb = rs_["sb"] = next_s4()
                    rs_["pi"] = next_pt()

                    def f(e):
                        ins = e.matmul(banks[sb][:, lo:hi], lhsT=kpad[:, br - 1, g, c * 128:(c + 1) * 128],
                                       rhs=qT[:, hh, q0 + lo:q0 + hi], start=True, stop=False,
                                       skip_group_check=True)
                        if br == 1:
                            ins = e.matmul(banks[sb][:, lo:hi], lhsT=cbf[:, CB_EP + c * 128:CB_EP + (c + 1) * 128],
                                           rhs=negselT[:, g, lo:hi], start=False, stop=(not diag),
                                           skip_group_check=True)
                        if diag:
                            ins = e.matmul(banks[sb][:, bt:bt + 128], lhsT=ident, rhs=TB, start=False, stop=True,
                                           skip_group_check=True)
                        return ins
                    rd = [RKS[c // 4], RQ[tg], R_cbf] + (R_ns if br == 1 else [])
                    S.op("pe", f, rd, [PB[sb]])

                def B():
                    sb, pi = rs_["sb"], rs_["pi"]
                    pt = ptb[pi]
                    S.op("act", lambda e: e.activation(out=pt[:, lo:hi], in_=banks[sb][:, lo:hi], func=AF.Exp),
                         [PB[sb]], [R_pt[pi]])

                def C():
                    pi = rs_["pi"]
                    pt = ptb[pi]

                    def f2(e):
                        ins = None
                        for tile in range(lo // 128, hi // 128):
                            ins = e.matmul(banks[ob][:, tile * 65:(tile + 1) * 65],
                                           lhsT=pt[:, tile * 128:(tile + 1) * 128], rhs=vaug[:, br - 1, c, g, :],
                                           start=(first and tile == lo // 128), stop=True, skip_group_check=True)
                        return ins
                    S.op("pe", f2, [R_pt[pi], RV[c]], [PB[ob]])
                    if last:
                        finalize(ob, 65, 64, 0, h, br, tg, False, False)
                return (A, B, C)

            def att_steps(tg, br):
                q0 = tg * 512
                heads = []
                for h in range(8):
                    steps = []
                    heads.append(steps)
                    g, hh = h // 4, h % 4
                    ob = 3 + (st["bank"] % 2)
                    st["bank"] += 1
                    if br == 1:
                        chunks = [(c, max(0, c * 128 - q0), 512, (c * 128 >= q0), T0) for c in range(4 * tg + 4)]
                    else:
                        chunks = []
                        for c in range(max(0, 4 * tg - 4), 4 * tg + 4):
                            m = c - (4 * tg - 4)
                            if m <= 3:
                                chunks.append((c, 0, 128 * (m + 1), True, T1))
                            else:
                                chunks.append((c, 128 * (m - 4), 512, True, T0))
                    for ci, (c, lo, hi, diag, TB) in enumerate(chunks):
                        steps.append(att_step(tg, br, h, g, hh, ob, c, lo, hi, diag, TB, ci == 0,
                                              ci == len(chunks) - 1))
                return heads

            def post_thunks(tg):
                q0 = tg * 512
                OAt = OAs[tg % 2]
                R_OAt = R_OAs[tg % 2]
                th = []

                def t0():
                    for tile in range(4):
                        S.op("act", lambda e, tile=tile: e.activation(out=junk[:, :], in_=OAt[:, tile, :],
                                                                      func=AF.Square, accum_out=ssq[:, tile, :]),
                             R_OAt, [R_junk, R_ssq])
                    S.op("act", lambda e: e.activation(out=rt4[:, :, :], in_=ssq[:, :, :], func=AF.Sqrt,
                                                       scale=1.0 / 512, bias=eps_ap), [R_ssq, R_pf], [R_rt4])
                    S.op("dve", lambda e: e.reciprocal(out=rs4[:, :, :], in_=rt4[:, :, :]), [R_rt4], [R_rs4])
                th.append(t0)
                for tile in range(4):
                    def tt(tile=tile):
                        oi = tile % 2
                        S.op("act", lambda e: e.activation(out=OAn[:, oi, :], in_=OAt[:, tile, :], func=AF.Identity,
                                                           scale=rs4[:, tile, :]), R_OAt + [R_rs4], [R_OAn[oi]])
                        pso = banks[7][:, 0:512].bitcast(BF16)[:, oi * 512:(oi + 1) * 512]

                        def ft(e):
                            ins = None
                            for j in range(4):
                                ins = e.transpose(out=pso[:, j * 128:(j + 1) * 128],
                                                  in_=OAn[:, oi, j * 128:(j + 1) * 128], identity=ident)
                            return ins
                        S.op("pe", ft, [R_OAn[oi], R_cbf], [PB[7]])
                        for j in range(4):
                            S.op("dve", lambda e, j=j: e.tensor_scalar(
                                out=oaT[:, j, tile * 128:(tile + 1) * 128], in0=pso[:, j * 128:(j + 1) * 128],
                                scalar1=pf[:, gmc + j:gmc + j + 1], scalar2=None, op0=ALU.mult),
                                [PB[7], R_pf], [R_oaT])
                    th.append(tt)
                for dc in range(8):
                    def to(dc=dc):
                        def f(e):
                            ins = None
                            for ic in range(8):
                                rhs = oaT[:, ic, :] if ic < 4 else ocT[:, ic - 4, q0:q0 + 512]
                                ins = e.matmul(banks[7][:, :], lhsT=wmo[:, ic, dc * 128:(dc + 1) * 128], rhs=rhs,
                                               start=(ic == 0), stop=(ic == 7))
                            return ins
                        S.op("pe", f, [R_wb[0], R_wb[1], R_oaT, ROC[tg]], [PB[7]])
                        S.op("dve", lambda e: e.scalar_tensor_tensor(
                            out=xT[:, dc, q0:q0 + 512], in0=banks[7][:, :], scalar=modG[:, s, l, 1, dc:dc + 1],
                            in1=xT[:, dc, q0:q0 + 512], op0=ALU.mult, op1=ALU.add), [PB[7], RX[dc][tg], R_modAG[l]],
                            [RX[dc][tg]])
                    th.append(to)
                return th

            fillers = []
            for tg in range(4):
                csteps = [cmp_step(tg, h, tail=((lambda tg=tg: select(tg)) if h == 7 else None)) for h in range(8)]
                wheads = att_steps(tg, 2)
                ssteps = [x for hd in att_steps(tg, 1) for x in hd]
                a0 = ssteps[0][0]
                ssteps[0] = ((lambda a0=a0: (sel_mask(), a0())), ssteps[0][1], ssteps[0][2])
                order = [csteps[0], csteps[1]]
                for h in range(8):
                    half = (len(wheads[h]) + 1) // 2
                    order += wheads[h][:half]
                    if h + 2 < 8:
                        order.append(csteps[h + 2])
                    order += wheads[h][half:]
                if tg == 3 and mid_hook is not None:
                    fillers = fillers + [mid_hook]
                pipeline(order + ssteps, 3, fillers)
                fillers = post_thunks(tg)
            for f_ in fillers:
                f_()

        out_res = []
        for s in range(NSEQ):
            for dc in range(8):
                S.dma("sp", xT[:, dc, :], xT_d[s, dc * 128:(dc + 1) * 128, :], [], RX[dc], "x%d" % dc)
            done = False
            for l in range(L):
                if s == 0 and l + 1 < L:
                    ada_pending.extend((l + 1, gq) for gq in range(36))
                ffn(s, l, 0, norm_done=(l > 0), tail_hook=(lambda stage, slots, s=s, l=l: mixer_norm(s, l, 0, stage, slots[0])))
                if stop == (l, "ffn1"):
                    done = True
                    break
                mixer(s, l, norm_done=True, mid_hook=(lambda s=s, l=l: ffn_norm(s, l, 1, 0)))
                if stop == (l, "mix"):
                    done = True
                    break
                ffn(s, l, 1, norm_done=True,
                    tail_hook=((lambda stage, slots, s=s, l=l: ffn_norm(s, l + 1, 0, 0, stage, slots)) if l + 1 < L else None))
                if stop == (l, "ffn2"):
                    done = True
                    break
            if done:
                for dc in range(8):
                    S.dma("sp", out_d[s, dc * 128:(dc + 1) * 128, :], xT[:, dc, :], RX[dc], [], "x%d" % dc)
                continue
            gfc = PO["gf"]
            for tg in range(4):
                t0 = tg * 512
                for dc in range(8):
                    b = dc % 2
                    S.op("act", lambda e, dc=dc, b=b, t0=t0: e.activation(out=sq[b][:, :], in_=xT[:, dc, t0:t0 + 512],
                                                                          func=AF.Square), [RX[dc][tg]], [R_sq[b]])
                    S.op("pe", lambda e, dc=dc, b=b: e.matmul(banks[7][:, :], lhsT=ones, rhs=sq[b][:, :],
                                                              start=(dc == 0), stop=(dc == 7)), [R_sq[b], R_cbf], [PB[7]])
                S.op("act", lambda e: e.activation(out=rt[:, :], in_=banks[7][:, :], func=AF.Sqrt, scale=1.0 / D,
                                                   bias=eps_ap), [PB[7], R_pf], [R_rt])
                S.op("dve", lambda e: e.reciprocal(out=rstd[:, :], in_=rt[:, :]), [R_rt], [R_rstd])
                for dc in range(8):
                    b = dc % 2
                    S.op("dve", lambda e, dc=dc, b=b, t0=t0: e.scalar_tensor_tensor(
                        out=tmpn[b][:, :], in0=xT[:, dc, t0:t0 + 512], scalar=pf[:, gfc + dc:gfc + dc + 1],
                        in1=rstd[:, :], op0=ALU.mult, op1=ALU.mult), [RX[dc][tg], R_pf, R_rstd], [R_tmpn[b]])
                    S.dma("sp", out_d[s, dc * 128:(dc + 1) * 128, t0:t0 + 512], tmpn[b][:, :], [R_tmpn[b]], [],
                          "tmpn%d" % b)
        allres = [r for row in RX for r in row] + R_tmpn
        S.final_wait("sp", allres)
        S.emit()
    return nc


def _consts():
    cb = np.zeros((128, CB_N), np.float32)
    k = np.arange(128)[:, None]
    t = np.arange(128)[None, :]
    cb[:, CB_ID:CB_ID + 128] = np.eye(128, dtype=np.float32)
    cb[:, CB_T0:CB_T0 + 128] = np.where(t >= k, 0.0, NEG)
    cb[:, CB_T1:CB_T1 + 128] = np.where(t < k, 0.0, NEG)
    cb[:, CB_ONE:CB_ONE + 128] = 1.0
    kk = np.arange(2048)[None, :]
    j = np.arange(128)[:, None]
    cb[:, CB_EP:CB_EP + 2048] = ((kk // 64) == j) & (j < 32)
    n = np.arange(128)[:, None]
    cb[:, CB_BC:CB_BC + 2048] = np.where((16 * n + 31 <= kk) & (n < 127), 0.0, NEG)
    jj = np.arange(32)[None, :]
    cb[:, CB_OV:CB_OV + 32] = (n >= 4 * jj - 1) & (n <= 4 * jj + 3) & (n < 127)
    tt = (np.arange(16)[None, :, None] * 128 + np.arange(128)[:, None, None])
    cur = tt // 64
    jb = np.arange(32)[None, None, :]
    fb = np.where(jb > cur, -1e30, np.where((jb == 0) | (jb == cur) | (jb == cur - 1), 1e4, 0.0)).astype(np.float32)
    return cb, fb.reshape(128, 512)


def _prep_shared(L, inp):
    f32 = np.float32
    sh = {}
    sh["w_ada"] = np.ascontiguousarray(inp["w_ada"][:L], dtype=f32)
    wfi = np.asarray(inp["w_ff_in"][:L], dtype=f32)
    g = wfi[..., :DFF].reshape(L, 2, D, 11, 256)
    u = wfi[..., DFF:].reshape(L, 2, D, 11, 256)
    sh["w_ff_in"] = np.ascontiguousarray(np.concatenate([g, u], axis=-1).reshape(L, 2, D, 2 * DFF))
    sh["w_ff_out"] = np.ascontiguousarray(inp["w_ff_out"][:L], dtype=f32)
    wmi = np.asarray(inp["w_mix_in"][:L], dtype=f32)
    q = wmi[:, :, 0:512].reshape(L, D, 8, 64)
    qperm = np.stack([q[:, :, [j, 4 + j], :].reshape(L, D, 128) for j in range(4)], axis=2).reshape(L, D, 512)
    kc, vc, ks, vs, kw, vw = (wmi[:, :, 512 + i * 128:640 + i * 128] for i in range(6))
    gates = wmi[:, :, 1280:1304]
    hc = wmi[:, :, 1304:1816].reshape(L, D, 4, 128)
    bg = wmi[:, :, 1816:2328].reshape(L, D, 4, 128)
    cg = wmi[:, :, 2328:2840].reshape(L, D, 4, 128)
    conv = np.stack([hc, bg, cg], axis=3).reshape(L, D, 1536)
    sh["w_mix_in"] = np.ascontiguousarray(np.concatenate([qperm, kc, vc, ks, kw, conv, vs, vw, gates], axis=-1))
    assert sh["w_mix_in"].shape[-1] == 2840
    sh["w_mix_out"] = np.ascontiguousarray(inp["w_mix_out"][:L], dtype=f32)
    cw = np.zeros((L, 128, CMPW), f32)
    for l in range(L):
        for kind in range(2):
            w1 = np.asarray(inp["w_cmp1"][l, kind], f32).reshape(32, 64, 64).transpose(1, 0, 2).reshape(64, 2048)
            cw[l, :, kind * 2048:(kind + 1) * 2048] = np.concatenate([w1, w1], axis=0)
            pos = np.asarray(inp["cmp_pos"][l, kind], f32).T
            cw[l, :, 4096 + kind * 34:4096 + kind * 34 + 32] = np.concatenate([pos, pos], axis=0)
        cw[l, 0:64, 4164:4228] = inp["w_cmp2"][l, 0]
        cw[l, 0:64, 4228:4292] = inp["w_cmp2"][l, 1]
    sh["cmpw"] = cw
    cb, fb = _consts()
    sh["cbf"] = cb
    return sh, fb


def _prep_pf(L, NSEQ, inp, fb, c_rows):
    PO = pf_offsets(L, NSEQ)
    pf = np.zeros((128, PO["_n"]), np.float32)

    def put(name, arr):
        a = np.asarray(arr, np.float32)
        a = a.reshape(-1, a.shape[-1] // 128, 128)
        a = a.transpose(2, 0, 1).reshape(128, -1)
        pf[:, PO[name]:PO[name] + a.shape[1]] = a

    put("g", inp["g_norm"][:L])
    put("b", inp["b_ada"][:L])
    put("cw", inp["conv_w"][:L])
    put("cb", inp["conv_b"][:L])
    put("gmo", inp["g_mix_out"][:L])
    put("gf", inp["g_final"])
    c = np.asarray(c_rows, np.float32)
    pf[:, PO["c"]:PO["c"] + 8 * NSEQ] = c.reshape(NSEQ, 8, 128).transpose(2, 1, 0).reshape(128, 8 * NSEQ)
    pf[:, PO["eps"]] = EPS
    pf[:, PO["fb"]:PO["fb"] + 512] = fb
    return pf


def run(inp, L=4, NSEQ=2, ncores=NCORES, stop=None, trace=False):
    sh, fb = _prep_shared(L, inp)
    x = np.asarray(inp["x"], np.float32)
    c = np.asarray(inp["c"], np.float32)
    in_maps = []
    for r in range(ncores):
        m = dict(sh)
        xs = x[r * NSEQ:(r + 1) * NSEQ]
        m["xT"] = np.ascontiguousarray(xs.transpose(0, 2, 1))
        m["pf32"] = _prep_pf(L, NSEQ, inp, fb, c[r * NSEQ:(r + 1) * NSEQ])
        in_maps.append(m)
    nc = build_program(L=L, NSEQ=NSEQ, stop=stop)
    res = run_bass_kernel_spmd(nc, in_maps, core_ids=list(range(ncores)), **({"trace": True} if trace else {}))
    out = np.concatenate([np.asarray(r["yT"]).transpose(0, 2, 1) for r in res.results], axis=0)
    return np.ascontiguousarray(out.astype(np.float32)), res


def kernel(x, c, w_ada, b_ada, g_norm, w_ff_in, w_ff_out, w_mix_in, cmp_pos, w_cmp1, w_cmp2, conv_w, conv_b,
           g_mix_out, w_mix_out, g_final):
    inp = dict(x=x, c=c, w_ada=w_ada, b_ada=b_ada, g_norm=g_norm, w_ff_in=w_ff_in, w_ff_out=w_ff_out,
               w_mix_in=w_mix_in, cmp_pos=cmp_pos, w_cmp1=w_cmp1, w_cmp2=w_cmp2, conv_w=conv_w, conv_b=conv_b,
               g_mix_out=g_mix_out, w_mix_out=w_mix_out, g_final=g_final)
    inp = {k: np.asarray(v) for k, v in inp.items()}
    out, _ = run(inp, L=4, NSEQ=2, ncores=NCORES)
    return out
```

```python
import numpy as np
import concourse.bass as bass
import concourse.mybir as mybir
from concourse.bass_utils import run_bass_kernel_spmd
from contextlib import ExitStack

F32 = mybir.dt.float32
BF16 = mybir.dt.bfloat16
AF = mybir.ActivationFunctionType
ALU = mybir.AluOpType

D = 1024
SEQ = 2048
DFF = 2816
NCORES = 8
EPS = 1e-6
NEG = -30000.0
STRICT_WAR = True
CMPW = 2 * 2048 + 2 * 34 + 64 + 64
PF_G = 0


def pf_offsets(L, NSEQ):
    o = {}
    c = 0
    for name, n in (("g", L * 24), ("b", L * 72), ("cw", L * 12), ("cb", L * 4), ("gmo", L * 8), ("gf", 8),
                    ("c", 8 * NSEQ), ("eps", 1), ("fb", 512)):
        o[name] = c
        c += n
    o["_n"] = c
    return o


CB_ID, CB_T0, CB_T1, CB_ONE, CB_EP, CB_BC, CB_OV, CB_N = 0, 128, 256, 384, 512, 2560, 4608, 4640


class Res:
    __slots__ = ("name", "w", "r", "excl")

    def __init__(self, name, excl=False):
        self.name = name
        self.w = None
        self.r = {}
        self.excl = excl


def fence(news, olds):
    acc = {}
    for o in olds:
        if o.w is not None:
            k, v, src = o.w
            if acc.get(k, (0, None))[0] < v:
                acc[k] = (v, src)
        for k, (v, src) in o.r.items():
            if acc.get(k, (0, None))[0] < v:
                acc[k] = (v, src)
    for n in news:
        for k, (v, src) in acc.items():
            if n.r.get(k, (0, None))[0] < v:
                n.r[k] = (v, src)


class Sched:
    ENGS = ("pe", "act", "dve", "pool", "sp")

    def __init__(self, nc, es):
        self.nc = nc
        self.es = es
        self.sems = {}
        self.ops = {n: [] for n in self.ENGS}
        self.cnt = {n: 0 for n in self.ENGS}
        self.seen = {n: {} for n in self.ENGS}
        for n in self.ENGS:
            self.sems[n] = es.enter_context(nc.semaphore("s_" + n))
        self.dma_tot = {}

    def _dma_sem(self, key):
        k = ("dma", key)
        if k not in self.sems:
            self.sems[k] = self.es.enter_context(self.nc.semaphore("d_%d" % len(self.sems)))
            self.dma_tot[k] = 0
        return k

    def _deps(self, en, reads, writes):
        need = {}

        def add(k, v, src, kind):
            if src == en:
                if en == "pe" or (kind == "r" and not STRICT_WAR):
                    return
            if k in self.dma_tot:
                v = max(v, self.dma_tot[k])
            if need.get(k, 0) < v:
                need[k] = v

        for r in reads:
            if r.w is not None:
                add(r.w[0], r.w[1], r.w[2], "w")
            if r.excl:
                for k, (v, src) in r.r.items():
                    add(k, v, src, "r")
        for w in writes:
            if w.w is not None:
                add(w.w[0], w.w[1], w.w[2], "w")
            for k, (v, src) in w.r.items():
                add(k, v, src, "r")
        seen = self.seen[en]
        out = []
        for k, v in need.items():
            if seen.get(k, 0) < v:
                seen[k] = v
                out.append((k, v))
        return out

    @staticmethod
    def _mark(tok, reads, writes):
        k, v, src = tok
        for r in reads:
            r.r[k] = (v, src)
        for w in writes:
            w.w = tok
            w.r = {}

    def op(self, en, fn, reads=(), writes=()):
        waits = self._deps(en, reads, writes)
        self.cnt[en] += 1
        self._mark((en, self.cnt[en], en), reads, writes)
        self.ops[en].append((waits, fn, None))

    def dma(self, q, out_ap, in_ap, reads, writes, key):
        waits = self._deps(q, reads, writes)
        k = self._dma_sem(key)
        self.dma_tot[k] += 16
        self._mark((k, self.dma_tot[k], "dma"), reads, writes)
        self.ops[q].append((waits, (out_ap, in_ap), k))

    def final_wait(self, en, ress):
        waits = self._deps(en, ress, ress)
        self.ops[en].append((waits, None, None))

    def emit(self):
        nc = self.nc
        sems = self.sems
        ops = self.ops
        with nc.Block() as block:
            def body(en, e):
                mysem = sems[en]
                for waits, fn, dk in ops[en]:
                    for k, v in waits:
                        e.wait_ge(sems[k], v)
                    if fn is None:
                        continue
                    if dk is not None:
                        e.dma_start(out=fn[0], in_=fn[1]).then_inc(sems[dk], 16)
                    else:
                        fn(e).then_inc(mysem, 1)

            @block.tensor
            def _(e):
                body("pe", e)

            @block.scalar
            def _(e):
                body("act", e)

            @block.vector
            def _(e):
                body("dve", e)

            @block.gpsimd
            def _(e):
                body("pool", e)

            @block.sync
            def _(e):
                body("sp", e)


class Arena:
    def __init__(self, nc, base=16640, top=229344):
        self.nc = nc
        self.base = base
        self.top = top
        self.cur = base
        self.n = 0

    def alloc(self, name, shape, dtype, at=None):
        esz = 4 if dtype == F32 else 2
        nbytes = int(np.prod(shape[1:])) * esz
        nbytes = (nbytes + 63) // 64 * 64
        if at is None:
            off = self.cur
            self.cur += nbytes
        else:
            off = self.base + at
        assert off + nbytes <= self.top, (name, off, nbytes, self.top)
        self.n += 1
        t = self.nc.alloc_sbuf_tensor_at("%s_%d" % (name, self.n), list(shape), dtype, offset=off)
        return t, (off - self.base) + nbytes


def build_program(L=4, NSEQ=2, stop=None):
    nc = bass.Bass("TRN2", target_bir_lowering=False)
    PO = pf_offsets(L, NSEQ)

    def dram(name, shape, kind="ExternalInput"):
        return nc.dram_tensor(name, list(shape), F32, kind=kind).ap()

    xT_d = dram("xT", [NSEQ, D, SEQ])
    wada_d = dram("w_ada", [L, D, 9216])
    wfi_d = dram("w_ff_in", [L, 2, D, 2 * DFF])
    wfo_d = dram("w_ff_out", [L, 2, DFF, D])
    wmi_d = dram("w_mix_in", [L, D, 2840])
    wmo_d = dram("w_mix_out", [L, D, D])
    cmpw_d = dram("cmpw", [L, 128, CMPW])
    pf_d = dram("pf32", [128, PO["_n"]])
    cbf_d = dram("cbf", [128, CB_N])
    out_d = dram("yT", [NSEQ, D, SEQ], kind="ExternalOutput")

    es = ExitStack()
    with es:
        S = Sched(nc, es)
        ar = Arena(nc)
        xT, _ = ar.alloc("xT", [128, 8, SEQ], F32)
        RX = [[Res("x%d_%d" % (dc, tg)) for tg in range(4)] for dc in range(8)]
        cbf, _ = ar.alloc("cbf", [128, CB_N], BF16)
        R_cbf = Res("cbf")
        pf, _ = ar.alloc("pf", [128, PO["_n"]], F32)
        R_pf = Res("pf")
        modT, _ = ar.alloc("modT", [128, NSEQ, L, 72], F32)
        R_mod = [Res("modT%d" % i) for i in range(L)]
        modA, _ = ar.alloc("modA", [128, NSEQ, L, 3, 8], F32)
        modG, _ = ar.alloc("modG", [128, NSEQ, L, 3, 8], F32)
        R_modAG = [Res("modAG%d" % i) for i in range(L)]
        cact, _ = ar.alloc("cact", [128, 8 * NSEQ], BF16)
        R_cact = Res("cact")
        hT_off = ar.cur - ar.base
        hT, _ = ar.alloc("hT", [128, 8, 1024], BF16)
        RH = [Res("h0"), Res("h1")]
        wbuf_all, _ = ar.alloc("wbuf", [128, 2, 8, 512], BF16)
        R_wb = [Res("wb0"), Res("wb1")]
        rt, _ = ar.alloc("rt", [128, 512], F32)
        R_rt = Res("rt")
        rstd, _ = ar.alloc("rstd", [128, 512], F32)
        R_rstd = Res("rstd")
        tmpn = []
        R_tmpn = []
        for i in range(2):
            t, _ = ar.alloc("tmpn", [128, 512], F32)
            tmpn.append(t)
            R_tmpn.append(Res("tmpn%d" % i))
        sq = []
        R_sq = []
        for i in range(2):
            t, _ = ar.alloc("sq", [128, 512], BF16)
            sq.append(t)
            R_sq.append(Res("sq%d" % i))
        PR = ar.cur - ar.base
        cap = ar.top - ar.base

        aT, o = ar.alloc("aT", [128, 22, 1024], BF16, at=PR)
        RA = [Res("a0"), Res("a1")]
        wout = []
        R_wo = []
        for i in range(2):
            t, o = ar.alloc("wout", [128, 22, 256], BF16, at=o)
            wout.append(t)
            R_wo.append(Res("wo%d" % i))
        sg = []
        R_sg = []
        for i in range(2):
            t, o = ar.alloc("sg", [128, 512], BF16, at=o)
            sg.append(t)
            R_sg.append(Res("sg%d" % i))
        rslot = [rstd]
        R_rslot = [R_rstd]
        for i in range(4):
            t, o = ar.alloc("rslot", [128, 512], F32, at=o)
            rslot.append(t)
            R_rslot.append(Res("rslot%d" % i))
        adab2 = []
        R_ada2 = []
        for i in range(2):
            t, o = ar.alloc("adab2", [128, 8, 256], BF16, at=o)
            adab2.append(t)
            R_ada2.append(Res("ada2_%d" % i))
        assert o <= cap, o
        FFN_RES = RA + R_wo + R_sg + R_rslot[1:] + R_ada2
        adab = []
        R_ada = []
        o2 = PR
        for i in range(2):
            t, o2 = ar.alloc("adab", [128, 8, 1152], BF16, at=o2)
            adab.append(t)
            R_ada.append(Res("ada%d" % i))

        qT, o = ar.alloc("qT", [128, 4, SEQ], BF16, at=PR)
        RQ = [Res("q%d" % i) for i in range(4)]
        kpad, o = ar.alloc("kpad", [128, 2, 2, SEQ], BF16, at=o)
        RKS = [Res("ks%d" % i) for i in range(4)]
        vaug, o = ar.alloc("vaug", [128, 2, 16, 2, 65], BF16, at=o)
        RV = [Res("v%d" % i) for i in range(16)]
        sig, o = ar.alloc("sig", [128, 16, 24], F32, at=o)
        RSIG = [Res("sig%d" % i) for i in range(4)]
        ocT, o = ar.alloc("ocT", [128, 4, SEQ], BF16, at=o)
        ROC = [Res("oc%d" % i) for i in range(4)]
        kcmpT, o = ar.alloc("kcmpT", [128, 2, 128], BF16, at=o)
        R_kcmp = Res("kcmp")
        Rc, o = ar.alloc("Rc", [128, 2, 97], BF16, at=o)
        R_Rc = Res("Rc")
        bvec, o = ar.alloc("bvec", [128, 2], F32, at=o)
        R_bvec = Res("bvec")
        slb = []
        R_sl = []
        for i in range(2):
            t, o = ar.alloc("sl", [128, 128], BF16, at=o)
            slb.append(t)
            R_sl.append(Res("sl%d" % i))
        ATT_P = o
        kcvc, o = ar.alloc("kcvc", [128, 2, SEQ], BF16, at=ATT_P)
        R_kcvc = Res("kcvc")
        A_X = o
        ubuf, o = ar.alloc("ubuf", [128, 4, 514], F32, at=A_X)
        RU = [Res("u%d" % i) for i in range(4)]
        ybuf, o = ar.alloc("ybuf", [128, 4, 512], F32, at=o)
        RY = [Res("y%d" % i) for i in range(4)]
        hcs, R_hcs = tmpn[0], R_tmpn[0]
        v0b, R_v0 = tmpn[1], R_tmpn[1]
        assert o <= cap, o
        A_RES = RU + RY
        cmpw, oc_ = ar.alloc("cmpw", [128, CMPW], BF16, at=A_X)
        R_cmpw = Res("cmpw")
        OA, o = ar.alloc("OA", [128, 4, 512], F32, at=ATT_P)
        OA2, _ = ar.alloc("OA2", [128, 4, 512], F32, at=hT_off)
        R_OA = Res("OA")
        negselT, o = ar.alloc("negselT", [128, 2, 512], BF16, at=o)
        R_ns = [Res("ns")]
        ptb = []
        R_pt = []
        for i in range(5):
            t, o = ar.alloc("pt", [128, 512], BF16, at=o)
            ptb.append(t)
            R_pt.append(Res("pt%d" % i))
        oaT, o = ar.alloc("oaT", [128, 4, 512], BF16, at=o)
        R_oaT = Res("oaT")
        OAn, o = ar.alloc("OAn", [128, 2, 512], BF16, at=o)
        R_OAn = [Res("OAn0"), Res("OAn1")]
        imp, o = ar.alloc("imp", [128, 2, 4, 32], F32, at=o)
        R_imp = Res("imp")
        nsb, o = ar.alloc("nsb", [128, 2, 4, 32], BF16, at=o)
        R_nsb = Res("nsb")
        top8, o = ar.alloc("top8", [128, 4, 8], F32, at=o)
        R_top8 = Res("top8")
        smalls = []
        R_small = []
        for i in range(6):
            t, o = ar.alloc("sm", [128, 4, 1], F32, at=o)
            smalls.append(t)
            R_small.append(Res("sm%d" % i))
        junk, o = ar.alloc("junk", [128, 512], BF16, at=o)
        R_junk = Res("junk")
        assert o <= cap, o
        B_RES = [R_OA] + R_ns + R_pt + R_OAn + [R_oaT, R_imp, R_nsb, R_top8, R_junk] + R_small
        ATT_RES = (RQ + RKS + RV + RSIG + ROC + [R_kcmp, R_Rc, R_bvec, R_kcvc, R_cmpw] + R_sl + A_RES + B_RES)

        banks = [nc.alloc_psum_tensor("bk%d" % i, [128, 512], F32) for i in range(8)]
        PB = [Res("pb%d" % i, excl=True) for i in range(8)]

        ident = cbf[:, CB_ID:CB_ID + 128]
        T0 = cbf[:, CB_T0:CB_T0 + 128]
        T1 = cbf[:, CB_T1:CB_T1 + 128]
        ones = cbf[:, CB_ONE:CB_ONE + 128]
        eps_ap = pf[:, PO["eps"]:PO["eps"] + 1]

        def wb(i):
            return wbuf_all[:, i]

        for c0 in range(0, CB_N, 2048):
            c1 = min(CB_N, c0 + 2048)
            S.dma("pool", cbf[:, c0:c1], cbf_d[:, c0:c1], [], [R_cbf], "cbf")
        S.dma("sp", pf[:, :], pf_d[:, :], [], [R_pf], "pf")
        S.op("act", lambda e: e.activation(out=cact[:, :], in_=pf[:, PO["c"]:PO["c"] + 8 * NSEQ], func=AF.Silu),
             [R_pf], [R_cact])
        gi = 0
        for l in range(1):
            for cg in range(8):
                ab = adab[gi % 2]
                Rab = R_ada[gi % 2]
                src = wada_d[l].rearrange("(kc p) c -> p kc c", p=128)[:, :, cg * 1152:(cg + 1) * 1152]
                S.dma("pool", ab[:, :, :], src, [], [Rab], "ada%d" % (gi % 2))
                bk = gi % 2

                def f(e, ab=ab, bk=bk):
                    ins = None
                    for ch in range(9):
                        for kc in range(8):
                            ins = e.matmul(banks[bk][:, ch * NSEQ:(ch + 1) * NSEQ], lhsT=ab[:, kc, ch * 128:(ch + 1) * 128],
                                           rhs=cact[:, kc * NSEQ:(kc + 1) * NSEQ], start=(kc == 0), stop=(kc == 7),
                                           skip_group_check=True)
                    return ins
                S.op("pe", f, [Rab, R_cact], [PB[bk]])
                for s in range(NSEQ):
                    psv = banks[bk][:, 0:9 * NSEQ].rearrange("p (a b) -> p a b", b=NSEQ)[:, :, s:s + 1]
                    bcol = PO["b"] + l * 72 + cg * 9
                    S.op("dve", lambda e, psv=psv, s=s, l=l, cg=cg, bcol=bcol: e.tensor_tensor(
                        out=modT[:, s, l, cg * 9:(cg + 1) * 9].unsqueeze(2), in0=psv,
                        in1=pf[:, bcol:bcol + 9].unsqueeze(2), op=ALU.add), [PB[bk], R_pf], [R_mod[l]])
                gi += 1

        def mod_derived(l):
            for s in range(NSEQ):
                for sub in range(3):
                    gcol = PO["g"] + (l * 3 + sub) * 8
                    S.op("dve", lambda e, s=s, l=l, sub=sub, gcol=gcol: e.scalar_tensor_tensor(
                        out=modA[:, s, l, sub, :], in0=modT[:, s, l, sub * 24 + 8:sub * 24 + 16], scalar=1.0,
                        in1=pf[:, gcol:gcol + 8], op0=ALU.add, op1=ALU.mult), [R_mod[l], R_pf], [R_modAG[l]])
                    gm = 1.0 if sub == 1 else 0.5
                    S.op("dve", lambda e, s=s, l=l, sub=sub, gm=gm: e.tensor_scalar(
                        out=modG[:, s, l, sub, :], in0=modT[:, s, l, sub * 24 + 16:sub * 24 + 24], scalar1=gm,
                        scalar2=None, op0=ALU.mult), [R_mod[l]], [R_modAG[l]])

        mod_derived(0)
        ada_pending = []

        def ada_group(l, gidx):
            i = st["ada"] % 2
            st["ada"] += 1
            ab = adab2[i]
            src = wada_d[l].rearrange("(kc p) c -> p kc c", p=128)[:, :, gidx * 256:(gidx + 1) * 256]
            S.dma("pool", ab[:, :, :], src, [], [R_ada2[i]], "ada2_%d" % i)

            def f(e):
                ins = None
                for ch in range(2):
                    for kc in range(8):
                        ins = e.matmul(banks[7][:, ch * NSEQ:(ch + 1) * NSEQ], lhsT=ab[:, kc, ch * 128:(ch + 1) * 128],
                                       rhs=cact[:, kc * NSEQ:(kc + 1) * NSEQ], start=(kc == 0), stop=(kc == 7),
                                       skip_group_check=True)
                return ins
            S.op("pe", f, [R_ada2[i], R_cact], [PB[7]])
            for s in range(NSEQ):
                psv = banks[7][:, 0:2 * NSEQ].rearrange("p (a b) -> p a b", b=NSEQ)[:, :, s:s + 1]
                bcol = PO["b"] + l * 72 + gidx * 2
                S.op("dve", lambda e, psv=psv, s=s, bcol=bcol: e.tensor_tensor(
                    out=modT[:, s, l, gidx * 2:(gidx + 1) * 2].unsqueeze(2), in0=psv,
                    in1=pf[:, bcol:bcol + 2].unsqueeze(2), op=ALU.add), [PB[7], R_pf], [R_mod[l]])
            if gidx == 35:
                mod_derived(l)

        def ada_hook(n=2):
            for _ in range(n):
                if ada_pending:
                    ada_group(*ada_pending.pop(0))

        st = {"wb": 0, "wo": 0, "bank": 0, "sb": 0, "pt": 0, "ada": 0}

        def next_wb():
            i = st["wb"] % 2
            st["wb"] += 1
            return i

        def prenorm(s, l, sub, tok0, dst, R_dst):
            norm_stats(tok0, 0)
            norm_apply(s, l, sub, tok0, 0, dst, R_dst)

        def norm_stats(tok0, slot):
            tg = tok0 // 512
            rstd = rslot[slot]
            R_rstd = R_rslot[slot]
            for dc in range(8):
                b = dc % 2
                S.op("act", lambda e, dc=dc, b=b: e.activation(out=sq[b][:, :], in_=xT[:, dc, tok0:tok0 + 512],
                                                              func=AF.Square), [RX[dc][tg]], [R_sq[b]])
                S.op("pe", lambda e, dc=dc, b=b: e.matmul(banks[7][:, :], lhsT=ones, rhs=sq[b][:, :], start=(dc == 0),
                                                         stop=(dc == 7)), [R_sq[b], R_cbf], [PB[7]])
            S.op("act", lambda e: e.activation(out=rt[:, :], in_=banks[7][:, :], func=AF.Sqrt, scale=1.0 / D,
                                               bias=eps_ap), [PB[7], R_pf], [R_rt])
            S.op("dve", lambda e: e.reciprocal(out=rstd[:, :], in_=rt[:, :]), [R_rt], [R_rstd])

        def norm_apply(s, l, sub, tok0, slot, dst, R_dst):
            tg = tok0 // 512
            rstd = rslot[slot]
            R_rstd = R_rslot[slot]
            for dc in range(8):
                b = dc % 2
                S.op("dve", lambda e, dc=dc, b=b: e.scalar_tensor_tensor(
                    out=tmpn[b][:, :], in0=xT[:, dc, tok0:tok0 + 512], scalar=modA[:, s, l, sub, dc:dc + 1],
                    in1=rstd[:, :], op0=ALU.mult, op1=ALU.mult), [RX[dc][tg], R_modAG[l], R_rstd], [R_tmpn[b]])
                S.op("act", lambda e, dc=dc, b=b: e.activation(
                    out=dst(dc), in_=tmpn[b][:, :], func=AF.Identity,
                    bias=modT[:, s, l, sub * 24 + dc:sub * 24 + dc + 1], scale=1.0), [R_tmpn[b], R_mod[l]], [R_dst])

        def ffn_norm(s, l, j, half, stage=None, slots=(0, 0)):
            sub = 0 if j == 0 else 2
            for tgi in range(2):
                tok0 = half * 1024 + tgi * 512
                if stage in (None, "stats"):
                    norm_stats(tok0, slots[tgi])
                if stage in (None, "apply"):
                    norm_apply(s, l, sub, tok0, slots[tgi],
                               lambda dc, tgi=tgi: hT[:, dc, tgi * 512:(tgi + 1) * 512], RH[tgi])

        def ffn(s, l, j, norm_done=False, tail_hook=None):
            sub = 0 if j == 0 else 2
            fence(FFN_RES, ATT_RES + R_ada)
            if not norm_done:
                ffn_norm(s, l, j, 0)
            ffn_norm(s, l, j, 1, "stats", (1, 2))
            for half in range(2):
                if half == 1 and tail_hook is not None:
                    tail_hook("stats", (3, 4))
                for fg in range(11):
                    wi = next_wb()
                    w = wb(wi)
                    src = wfi_d[l, j].rearrange("(kc p) c -> p kc c", p=128)[:, :, fg * 512:(fg + 1) * 512]
                    S.dma("pool", w[:, :, :], src, [], [R_wb[wi]], "wb%d" % wi)
                    for tgi in range(2):
                        bs = 4 * (st["bank"] % 2)
                        st["bank"] += 1
                        for oc in range(4):
                            def f(e, w=w, oc=oc, tgi=tgi, bs=bs):
                                ins = None
                                for kc in range(8):
                                    ins = e.matmul(banks[bs + oc][:, :], lhsT=w[:, kc, oc * 128:(oc + 1) * 128],
                                                   rhs=hT[:, kc, tgi * 512:(tgi + 1) * 512], start=(kc == 0),
                                                   stop=(kc == 7))
                                return ins
                            S.op("pe", f, [R_wb[wi], RH[tgi]], [PB[bs + oc]])
                        for i in range(2):
                            S.op("act", lambda e, i=i, bs=bs: e.activation(out=sg[i][:, :], in_=banks[bs + i][:, :],
                                                                           func=AF.Silu), [PB[bs + i]], [R_sg[i]])
                            S.op("dve", lambda e, i=i, bs=bs, fg=fg, tgi=tgi: e.tensor_tensor(
                                out=aT[:, 2 * fg + i, tgi * 512:(tgi + 1) * 512], in0=sg[i][:, :],
                                in1=banks[bs + 2 + i][:, :], op=ALU.mult), [R_sg[i], PB[bs + 2 + i]], [RA[tgi]])
                    if j == 0 and s == 0:
                        ada_hook(2)
                if half == 0:
                    ffn_norm(s, l, j, 1, "apply", (1, 2))
                elif tail_hook is not None:
                    tail_hook("apply", (3, 4))
                for dmp in range(4):
                    oi = st["wo"] % 2
                    st["wo"] += 1
                    wo = wout[oi]
                    src = wfo_d[l, j].rearrange("(fc p) c -> p fc c", p=128)[:, :, dmp * 256:(dmp + 1) * 256]
                    S.dma("pool", wo[:, :, :], src, [], [R_wo[oi]], "wo%d" % oi)
                    for dmi in range(2):
                        dc = dmp * 2 + dmi
                        for tgi in range(2):
                            bk = st["sb"] % 8
                            st["sb"] += 1
                            tg = half * 2 + tgi

                            def f(e, wo=wo, dmi=dmi, tgi=tgi, bk=bk):
                                ins = None
                                for fc in range(22):
                                    ins = e.matmul(banks[bk][:, :], lhsT=wo[:, fc, dmi * 128:(dmi + 1) * 128],
                                                   rhs=aT[:, fc, tgi * 512:(tgi + 1) * 512], start=(fc == 0),
                                                   stop=(fc == 21))
                                return ins
                            S.op("pe", f, [R_wo[oi], RA[tgi]], [PB[bk]])
                            S.op("dve", lambda e, dc=dc, tg=tg, bk=bk: e.scalar_tensor_tensor(
                                out=xT[:, dc, tg * 512:(tg + 1) * 512], in0=banks[bk][:, :],
                                scalar=modG[:, s, l, sub, dc:dc + 1], in1=xT[:, dc, tg * 512:(tg + 1) * 512],
                                op0=ALU.mult, op1=ALU.add), [PB[bk], RX[dc][tg], R_modAG[l]], [RX[dc][tg]])

        def mixer_norm(s, l, tg, stage=None, slot=0):
            hb = tg % 2
            if stage in (None, "stats"):
                norm_stats(tg * 512, slot)
            if stage in (None, "apply"):
                norm_apply(s, l, 1, tg * 512, slot, lambda dc, hb=hb: hT[:, dc, hb * 512:(hb + 1) * 512], RH[hb])

        def mixer(s, l, norm_done=False, mid_hook=None):
            fence(ATT_RES, FFN_RES + R_ada)
            cwc = PO["cw"] + l * 12
            cbc = PO["cb"] + l * 4
            gmc = PO["gmo"] + l * 8
            S.op("dve", lambda e: e.memset(vaug[:, :, :, :, 64:65], 1.0), [], RV)
            S.op("dve", lambda e: e.memset(Rc[:, :, :], 0.0), [], [R_Rc])
            for g in range(2):
                S.op("dve", lambda e, g=g: e.tensor_copy(out=Rc[:, g, 0:32], in_=cbf[:, CB_OV:CB_OV + 32]),
                     [R_cbf], [R_Rc])
            S.op("dve", lambda e: e.memset(Rc[:, :, 96:97], 1.0), [], [R_Rc])
            S.op("dve", lambda e: e.memset(kcmpT[:, :, :], 0.0), [], [R_kcmp])
            S.op("dve", lambda e: e.memset(kpad[64:128, :, 0, :], 0.0), [], RKS)
            S.op("dve", lambda e: e.memset(kpad[0:64, :, 1, :], 0.0), [], RKS)
            S.op("dve", lambda e: e.memset(ubuf[:, :, 0:2], 0.0), [], RU)

            def load_w(c0, n):
                wi = next_wb()
                w = wb(wi)
                src = wmi_d[l].rearrange("(kc p) c -> p kc c", p=128)[:, :, c0:c0 + n]
                S.dma("pool", w[:, :, 0:n], src, [], [R_wb[wi]], "wb%d" % wi)
                return w, R_wb[wi]

            def proj(w, Rw, col, hb, bk):
                def f(e):
                    ins = None
                    for kc in range(8):
                        ins = e.matmul(banks[bk][:, :], lhsT=w[:, kc, col:col + 128],
                                       rhs=hT[:, kc, hb * 512:(hb + 1) * 512], start=(kc == 0), stop=(kc == 7))
                    return ins
                S.op("pe", f, [Rw, RH[hb]], [PB[bk]])

            def phaseA(tg):
                hb = tg % 2
                cs = slice(tg * 512, (tg + 1) * 512)
                if tg == 0 and not norm_done:
                    mixer_norm(s, l, 0)
                w, Rw = load_w(0, 512)
                for c in range(4):
                    proj(w, Rw, c * 128, hb, c)
                for c in range(4):
                    if c % 2 == 0:
                        S.op("act", lambda e, c=c: e.activation(out=qT[:, c, cs], in_=banks[c][:, :], func=AF.Identity,
                                                                scale=0.125), [PB[c]], [RQ[tg]])
                    else:
                        S.op("dve", lambda e, c=c: e.tensor_scalar(out=qT[:, c, cs], in0=banks[c][:, :], scalar1=0.125,
                                                                   scalar2=None, op0=ALU.mult), [PB[c]], [RQ[tg]])
                w, Rw = load_w(512, 512)
                for c in range(4):
                    proj(w, Rw, c * 128, hb, 4 + c)
                S.op("act", lambda e: e.copy(out=kcvc[:, 0, cs], in_=banks[4][:, :]), [PB[4]], [R_kcvc])
                S.op("dve", lambda e: e.tensor_copy(out=kcvc[:, 1, cs], in_=banks[5][:, :]), [PB[5]], [R_kcvc])
                for kind in range(2):
                    S.op("act", lambda e, kind=kind: e.copy(out=kpad[0:64, kind, 0, cs], in_=banks[6 + kind][0:64, :]),
                         [PB[6 + kind]], [RKS[tg]])
                    S.op("dve", lambda e, kind=kind: e.tensor_copy(out=kpad[64:128, kind, 1, cs],
                                                                   in_=banks[6 + kind][64:128, :]),
                         [PB[6 + kind]], [RKS[tg]])
                if tg < 3:
                    mixer_norm(s, l, tg + 1)
                for cc in range(4):
                    w, Rw = load_w(1024 + cc * 384, 384)
                    b0 = (cc * 3) % 8
                    bh, bb, bc_ = b0, (b0 + 1) % 8, (b0 + 2) % 8
                    proj(w, Rw, 0, hb, bh)
                    proj(w, Rw, 128, hb, bb)
                    proj(w, Rw, 256, hb, bc_)
                    S.op("act", lambda e, bh=bh: e.copy(out=hcs[:, :], in_=banks[bh][:, :]), [PB[bh]], [R_hcs])
                    S.op("dve", lambda e, cc=cc, bc_=bc_: e.tensor_tensor(out=ubuf[:, cc, 2:514], in0=hcs[:, :],
                                                                          in1=banks[bc_][:, :], op=ALU.mult),
                         [R_hcs, PB[bc_]], [RU[cc]])
                    S.op("act", lambda e, cc=cc: e.activation(
                        out=v0b[:, :], in_=ubuf[:, cc, 2:514], func=AF.Identity,
                        scale=pf[:, cwc + 8 + cc:cwc + 9 + cc], bias=pf[:, cbc + cc:cbc + cc + 1]),
                        [RU[cc], R_pf], [R_v0])
                    S.op("dve", lambda e, cc=cc: e.scalar_tensor_tensor(
                        out=v0b[:, :], in0=ubuf[:, cc, 1:513], scalar=pf[:, cwc + 4 + cc:cwc + 5 + cc], in1=v0b[:, :],
                        op0=ALU.mult, op1=ALU.add), [RU[cc], R_pf, R_v0], [R_v0])
                    S.op("dve", lambda e, cc=cc: e.scalar_tensor_tensor(
                        out=v0b[:, :], in0=ubuf[:, cc, 0:512], scalar=pf[:, cwc + cc:cwc + cc + 1], in1=v0b[:, :],
                        op0=ALU.mult, op1=ALU.add), [RU[cc], R_pf, R_v0], [R_v0])
                    S.op("dve", lambda e, cc=cc, bb=bb: e.tensor_tensor(out=ybuf[:, cc, :], in0=v0b[:, :],
                                                                        in1=banks[bb][:, :], op=ALU.mult),
                         [R_v0, PB[bb]], [RY[cc]])
                    S.op("act", lambda e, cc=cc: e.copy(out=ubuf[:, cc, 0:2], in_=ubuf[:, cc, 512:514]),
                         [RU[cc]], [RU[cc]])
                w, Rw = load_w(2560, 280)
                for tile in range(4):
                    ti = tg * 4 + tile
                    bk = 3 + tile

                    def f(e, w=w, tile=tile, bk=bk):
                        ins = None
                        for kc in range(8):
                            ins = e.matmul(banks[bk][:, 0:280],
                                           lhsT=hT[:, kc, hb * 512 + tile * 128:hb * 512 + (tile + 1) * 128],
                                           rhs=w[:, kc, 0:280], start=(kc == 0), stop=(kc == 7))
                        return ins
                    S.op("pe", f, [Rw, RH[hb]], [PB[bk]])
                    S.op("act", lambda e, ti=ti, bk=bk: e.copy(
                        out=vaug[:, 0, ti, :, 0:64], in_=banks[bk][:, 0:128].rearrange("p (a b) -> p a b", a=2)),
                        [PB[bk]], [RV[ti]])
                    S.op("dve", lambda e, ti=ti, bk=bk: e.tensor_copy(
                        out=vaug[:, 1, ti, :, 0:64], in_=banks[bk][:, 128:256].rearrange("p (a b) -> p a b", a=2)),
                        [PB[bk]], [RV[ti]])
                    S.op("act", lambda e, ti=ti, bk=bk: e.activation(out=sig[:, ti, :], in_=banks[bk][:, 256:280],
                                                                     func=AF.Sigmoid), [PB[bk]], [RSIG[tg]])
                for cc in range(4):
                    b = cc % 2
                    S.op("act", lambda e, cc=cc, b=b: e.activation(out=sq[b][:, :], in_=ybuf[:, cc, :], func=AF.Square),
                         [RY[cc]], [R_sq[b]])
                    S.op("pe", lambda e, cc=cc, b=b: e.matmul(banks[7][:, :], lhsT=ones, rhs=sq[b][:, :],
                                                              start=(cc == 0), stop=(cc == 3)),
                         [R_sq[b], R_cbf], [PB[7]])
                S.op("act", lambda e: e.activation(out=rt[:, :], in_=banks[7][:, :], func=AF.Sqrt, scale=1.0 / 512,
                                                   bias=eps_ap), [PB[7], R_pf], [R_rt])
                S.op("dve", lambda e: e.reciprocal(out=rstd[:, :], in_=rt[:, :]), [R_rt], [R_rstd])
                for cc in range(4):
                    S.op("dve", lambda e, cc=cc: e.scalar_tensor_tensor(
                        out=ocT[:, cc, cs], in0=ybuf[:, cc, :], scalar=pf[:, gmc + 4 + cc:gmc + 5 + cc],
                        in1=rstd[:, :], op0=ALU.mult, op1=ALU.mult), [RY[cc], R_pf, R_rstd], [ROC[tg]])

            for tg_ in range(4):
                phaseA(tg_)

            fence([R_cmpw], A_RES)
            for c0 in range(0, CMPW, 2048):
                c1 = min(CMPW, c0 + 2048)
                S.dma("pool", cmpw[:, c0:c1], cmpw_d[l, :, c0:c1], [], [R_cmpw], "cmpw")
            w2k = cmpw[0:64, 4164:4228]
            w2v = cmpw[0:64, 4228:4292]
            for kind in range(2):
                w1 = cmpw[:, kind * 2048:(kind + 1) * 2048]
                pos = cmpw[:, 4096 + kind * 34:4096 + (kind + 1) * 34]

                def fb(e, w1=w1, pos=pos, kind=kind):
                    ins = None
                    for lp in range(32):
                        ins = e.matmul(banks[2][0:64, kind * 2:kind * 2 + 2], lhsT=w1[0:64, lp * 64:(lp + 1) * 64],
                                       rhs=pos[0:64, lp:lp + 2], start=(lp == 0), stop=(lp == 31), skip_group_check=True)
                    return ins
                S.op("pe", fb, [R_cmpw], [PB[2]])
                S.op("dve", lambda e, kind=kind: e.tensor_copy(out=bvec[0:64, kind:kind + 1],
                                                               in_=banks[2][0:64, kind * 2:kind * 2 + 1]),
                     [PB[2]], [R_bvec])
                for g in range(2):
                    rows = slice(64 * g, 64 * g + 64)
                    sl = slb[g]

                    def fc(e, w1=w1, kind=kind, g=g, rows=rows):
                        ins = None
                        for lp in range(32):
                            ins = e.matmul(banks[g][0:64, 0:127], lhsT=w1[rows, lp * 64:(lp + 1) * 64],
                                           rhs=kcvc[rows, kind, lp:lp + 16 * 126 + 1:16], start=(lp == 0), stop=(lp == 31))
                        return ins
                    S.op("pe", fc, [R_cmpw, R_kcvc], [PB[g]])
                    S.op("act", lambda e, g=g, sl=sl, kind=kind: e.activation(
                        out=sl[0:64, 0:127], in_=banks[g][0:64, 0:127], func=AF.Silu, bias=bvec[0:64, kind:kind + 1],
                        scale=1.0), [PB[g], R_bvec], [R_sl[g]])
                    if kind == 0:
                        S.op("pe", lambda e, sl=sl, rows=rows: e.matmul(banks[3][rows, 0:127], lhsT=w2k,
                                                                        rhs=sl[0:64, 0:127], start=True, stop=True,
                                                                        skip_group_check=True),
                             [R_cmpw, R_sl[g]], [PB[3]])
                        S.op("dve", lambda e, rows=rows, g=g: e.tensor_copy(out=kcmpT[rows, g, 0:127],
                                                                            in_=banks[3][rows, 0:127]),
                             [PB[3]], [R_kcmp])
                    else:
                        S.op("pe", lambda e, sl=sl, g=g: e.matmul(banks[3][0:127, 128 + g * 64:192 + g * 64],
                                                                  lhsT=sl[0:64, 0:127], rhs=w2v, start=True, stop=True,
                                                                  skip_group_check=True),
                             [R_cmpw, R_sl[g]], [PB[3]])
                        S.op("dve", lambda e, g=g: e.tensor_copy(out=Rc[0:127, g, 32:96],
                                                                 in_=banks[3][0:127, 128 + g * 64:192 + g * 64]),
                             [PB[3]], [R_Rc])

            fence(B_RES, A_RES + [R_cmpw, R_kcvc])
            S.op("dve", lambda e: e.memset(negselT[:, :, :], 0.0), [], R_ns)
            wmo = wbuf_all.rearrange("p a k c -> p (a k c)").rearrange("p (k c) -> p k c", c=1024)
            for hf in range(2):
                src = wmo_d[l].rearrange("(kc p) c -> p kc c", p=128)[:, hf * 4:(hf + 1) * 4, :]
                S.dma("pool", wmo[:, hf * 4:(hf + 1) * 4, :], src, [], [R_wb[hf]], "wb%d" % hf)
            st["wb"] = 0
            rz, zc, rzg, ssq, rt4, rs4 = smalls
            R_rz, R_zc, R_rzg, R_ssq, R_rt4, R_rs4 = R_small
            fbc = PO["fb"]

            def next_sb():
                i = st["sb"] % 3
                st["sb"] += 1
                return i

            def next_pt():
                i = st["pt"] % 5
                st["pt"] += 1
                return i

            def next_sb5():
                i = (0, 1, 2, 5, 6)[st["sb"] % 5]
                st["sb"] += 1
                return i

            OAs = [OA2, OA]
            R_OAs = [[RH[0], RH[1]], [R_OA]]

            def finalize(bk, width, zcol, vcol, h, br, tg, first, clampz):
                OAt = OAs[tg % 2]
                R_OAt = R_OAs[tg % 2]
                view = banks[bk][:, 0:4 * width].rearrange("p (a b) -> p a b", b=width)
                if clampz:
                    S.op("dve", lambda e: e.tensor_scalar(out=zc[:, :, :], in0=view[:, :, zcol:zcol + 1], scalar1=1e-30,
                                                          scalar2=None, op0=ALU.max), [PB[bk]], [R_zc])
                    S.op("dve", lambda e: e.reciprocal(out=rz[:, :, :], in_=zc[:, :, :]), [R_zc], [R_rz])
                else:
                    S.op("dve", lambda e: e.reciprocal(out=rz[:, :, :], in_=view[:, :, zcol:zcol + 1]), [PB[bk]], [R_rz])
                gcol = h * 3 + br
                S.op("dve", lambda e: e.tensor_tensor(out=rzg[:, :, :], in0=rz[:, :, :],
                                                      in1=sig[:, tg * 4:(tg + 1) * 4, gcol:gcol + 1], op=ALU.mult),
                     [R_rz, RSIG[tg]], [R_rzg])
                for tile in range(4):
                    if first:
                        S.op("dve", lambda e, tile=tile: e.tensor_scalar(
                            out=OAt[:, tile, h * 64:(h + 1) * 64], in0=view[:, tile, vcol:vcol + 64],
                            scalar1=rzg[:, tile, :], scalar2=None, op0=ALU.mult), [PB[bk], R_rzg], R_OAt)
                    else:
                        S.op("dve", lambda e, tile=tile: e.scalar_tensor_tensor(
                            out=OAt[:, tile, h * 64:(h + 1) * 64], in0=view[:, tile, vcol:vcol + 64],
                            scalar=rzg[:, tile, :], in1=OAt[:, tile, h * 64:(h + 1) * 64], op0=ALU.mult, op1=ALU.add),
                            [PB[bk], R_rzg] + R_OAt, R_OAt)

            def pipeline(steps, skew, fillers=(), every=2, start_at=3):
                n = len(steps)
                fi = 0
                for i in range(n + skew):
                    if i < n:
                        steps[i][0]()
                        steps[i][1]()
                    if i - skew >= 0:
                        steps[i - skew][2]()
                    if fi < len(fillers) and i >= start_at and (i - start_at) % every == 0:
                        fillers[fi]()
                        fi += 1
                while fi < len(fillers):
                    fillers[fi]()
                    fi += 1

            def next_s4():
                i = (0, 1, 2, 5)[st["sb"] % 4]
                st["sb"] += 1
                return i

            def cmp_step(tg, h, tail=None):
                q0 = tg * 512
                g, hh = h // 4, h % 4
                cb_ = 6
                rs_ = {}

                def A():
                    sb = rs_["sb"] = next_s4()
                    rs_["pi"] = next_pt()

                    def f(e):
                        e.matmul(banks[sb][:, :], lhsT=kcmpT[:, g, :], rhs=qT[:, hh, q0:q0 + 512], start=True,
                                 stop=False)
                        return e.matmul(banks[sb][:, :], lhsT=ident, rhs=cbf[:, CB_BC + q0:CB_BC + q0 + 512],
                                        start=False, stop=True)
                    S.op("pe", f, [R_kcmp, RQ[tg], R_cbf], [PB[sb]])

                def B():
                    sb, pi = rs_["sb"], rs_["pi"]
                    pt = ptb[pi]
                    S.op("act", lambda e: e.activation(out=pt[:, :], in_=banks[sb][:, :], func=AF.Exp),
                         [PB[sb]], [R_pt[pi]])

                def C():
                    pi = rs_["pi"]
                    pt = ptb[pi]

                    def f2(e):
                        ins = None
                        for tile in range(4):
                            ins = e.matmul(banks[cb_][:, tile * 97:(tile + 1) * 97],
                                           lhsT=pt[:, tile * 128:(tile + 1) * 128], rhs=Rc[:, g, :],
                                           start=(tile == 0), stop=(tile == 3), skip_group_check=True)
                        return ins
                    S.op("pe", f2, [R_pt[pi], R_Rc], [PB[cb_]])
                    finalize(cb_, 97, 96, 32, h, 0, tg, True, True)
                    view = banks[cb_][:, 0:4 * 97].rearrange("p (a b) -> p a b", b=97)
                    for tile in range(4):
                        if hh == 0:
                            S.op("dve", lambda e, tile=tile: e.tensor_scalar(
                                out=imp[:, g, tile, :], in0=view[:, tile, 0:32], scalar1=rz[:, tile, :],
                                scalar2=None, op0=ALU.mult), [PB[cb_], R_rz], [R_imp])
                        else:
                            S.op("dve", lambda e, tile=tile: e.scalar_tensor_tensor(
                                out=imp[:, g, tile, :], in0=view[:, tile, 0:32], scalar=rz[:, tile, :],
                                in1=imp[:, g, tile, :], op0=ALU.mult, op1=ALU.add), [PB[cb_], R_rz, R_imp],
                                [R_imp])
                    if tail is not None:
                        tail()
                return (A, B, C)

            def select(tg):
                for g in range(2):
                    S.op("dve", lambda e, g=g: e.tensor_tensor(
                        out=imp[:, g, :, :], in0=imp[:, g, :, :],
                        in1=pf[:, fbc + tg * 128:fbc + (tg + 1) * 128].rearrange("p (a b) -> p a b", b=32), op=ALU.add),
                        [R_imp, R_pf], [R_imp])
                    for tile in range(4):
                        S.op("dve", lambda e, tile=tile, g=g: e.max(out=top8[:, tile, :], in_=imp[:, g, tile, :]),
                             [R_imp], [R_top8])
                        S.op("dve", lambda e, tile=tile, g=g: e.tensor_scalar(
                            out=nsb[:, g, tile, :], in0=imp[:, g, tile, :], scalar1=top8[:, tile, 7:8], scalar2=NEG,
                            op0=ALU.is_lt, op1=ALU.mult), [R_imp, R_top8], [R_nsb])

            pst = banks[7][0:32, 0:512].bitcast(BF16)

            def sel_mask():
                def ftn(e):
                    ins = None
                    for g in range(2):
                        for tile in range(4):
                            ins = e.transpose(out=pst[:, (g * 4 + tile) * 128:(g * 4 + tile + 1) * 128],
                                              in_=nsb[:, g, tile, :], identity=ident)
                    return ins
                S.op("pe", ftn, [R_nsb, R_cbf], [PB[7]])
                S.op("act", lambda e: e.copy(out=negselT[0:32, :, :], in_=pst.rearrange("p (g c) -> p g c", g=2)),
                     [PB[7]], R_ns)

            def att_step(tg, br, h, g, hh, ob, c, lo, hi, diag, TB, first, last):
                q0 = tg * 512
                rs_ = {}
                bt = lo if TB is T0 else hi - 128

                def A():
                    sb = rs_["sb"] = next_s4()
                    rs_["pi"] = next_pt()

                    def f(e):
                        ins = e.matmul(banks[sb][:, lo:hi], lhsT=kpad[:, br - 1, g, c * 128:(c + 1) * 128],
                                       rhs=qT[:, hh, q0 + lo:q0 + hi], start=True, stop=False,
                                       skip_group_check=True)
                        if br == 1:
                            ins = e.matmul(banks[sb][:, lo:hi], lhsT=cbf[:, CB_EP + c * 128:CB_EP + (c + 1) * 128],
                                           rhs=negselT[:, g, lo:hi], start=False, stop=(not diag),
                                           skip_group_check=True)
                        if diag:
                            ins = e.matmul(banks[sb][:, bt:bt + 128], lhsT=ident, rhs=TB, start=False, stop=True,
                                           skip_group_check=True)
                        return ins
                    rd = [RKS[c // 4], RQ[tg], R_cbf] + (R_ns if br == 1 else [])
                    S.op("pe", f, rd, [PB[sb]])

                def B():
                    sb, pi = rs_["sb"], rs_["pi"]
                    pt = ptb[pi]
                    S.op("act", lambda e: e.activation(out=pt[:, lo:hi], in_=banks[sb][:, lo:hi], func=AF.Exp),
                         [PB[sb]], [R_pt[pi]])

                def C():
                    pi = rs_["pi"]
                    pt = ptb[pi]

                    def f2(e):
                        ins = None
                        for tile in range(lo // 128, hi // 128):
                            ins = e.matmul(banks[ob][:, tile * 65:(tile + 1) * 65],
                                           lhsT=pt[:, tile * 128:(tile + 1) * 128], rhs=vaug[:, br - 1, c, g, :],
                                           start=(first and tile == lo // 128), stop=True, skip_group_check=True)
                        return ins
                    S.op("pe", f2, [R_pt[pi], RV[c]], [PB[ob]])
                    if last:
                        finalize(ob, 65, 64, 0, h, br, tg, False, False)
                return (A, B, C)

            def att_steps(tg, br):
                q0 = tg * 512
                heads = []
                for h in range(8):
                    steps = []
                    heads.append(steps)
                    g, hh = h // 4, h % 4
                    ob = 3 + (st["bank"] % 2)
                    st["bank"] += 1
                    if br == 1:
                        chunks = [(c, max(0, c * 128 - q0), 512, (c * 128 >= q0), T0) for c in range(4 * tg + 4)]
                    else:
                        chunks = []
                        for c in range(max(0, 4 * tg - 4), 4 * tg + 4):
                            m = c - (4 * tg - 4)
                            if m <= 3:
                                chunks.append((c, 0, 128 * (m + 1), True, T1))
                            else:
                                chunks.append((c, 128 * (m - 4), 512, True, T0))
                    for ci, (c, lo, hi, diag, TB) in enumerate(chunks):
                        steps.append(att_step(tg, br, h, g, hh, ob, c, lo, hi, diag, TB, ci == 0,
                                              ci == len(chunks) - 1))
                return heads

            def post_thunks(tg):
                q0 = tg * 512
                OAt = OAs[tg % 2]
                R_OAt = R_OAs[tg % 2]
                th = []

                def t0():
                    for tile in range(4):
                        S.op("act", lambda e, tile=tile: e.activation(out=junk[:, :], in_=OAt[:, tile, :],
                                                                      func=AF.Square, accum_out=ssq[:, tile, :]),
                             R_OAt, [R_junk, R_ssq])
                    S.op("act", lambda e: e.activation(out=rt4[:, :, :], in_=ssq[:, :, :], func=AF.Sqrt,
                                                       scale=1.0 / 512, bias=eps_ap), [R_ssq, R_pf], [R_rt4])
                    S.op("dve", lambda e: e.reciprocal(out=rs4[:, :, :], in_=rt4[:, :, :]), [R_rt4], [R_rs4])
                th.append(t0)
                for tile in range(4):
                    def tt(tile=tile):
                        oi = tile % 2
                        S.op("act", lambda e: e.activation(out=OAn[:, oi, :], in_=OAt[:, tile, :], func=AF.Identity,
                                                           scale=rs4[:, tile, :]), R_OAt + [R_rs4], [R_OAn[oi]])
                        pso = banks[7][:, 0:512].bitcast(BF16)[:, oi * 512:(oi + 1) * 512]

                        def ft(e):
                            ins = None
                            for j in range(4):
                                ins = e.transpose(out=pso[:, j * 128:(j + 1) * 128],
                                                  in_=OAn[:, oi, j * 128:(j + 1) * 128], identity=ident)
                            return ins
                        S.op("pe", ft, [R_OAn[oi], R_cbf], [PB[7]])
                        for j in range(4):
                            S.op("dve", lambda e, j=j: e.tensor_scalar(
                                out=oaT[:, j, tile * 128:(tile + 1) * 128], in0=pso[:, j * 128:(j + 1) * 128],
                                scalar1=pf[:, gmc + j:gmc + j + 1], scalar2=None, op0=ALU.mult),
                                [PB[7], R_pf], [R_oaT])
                    th.append(tt)
                for dc in range(8):
                    def to(dc=dc):
                        def f(e):
                            ins = None
                            for ic in range(8):
                                rhs = oaT[:, ic, :] if ic < 4 else ocT[:, ic - 4, q0:q0 + 512]
                                ins = e.matmul(banks[7][:, :], lhsT=wmo[:, ic, dc * 128:(dc + 1) * 128], rhs=rhs,
                                               start=(ic == 0), stop=(ic == 7))
                            return ins
                        S.op("pe", f, [R_wb[0], R_wb[1], R_oaT, ROC[tg]], [PB[7]])
                        S.op("dve", lambda e: e.scalar_tensor_tensor(
                            out=xT[:, dc, q0:q0 + 512], in0=banks[7][:, :], scalar=modG[:, s, l, 1, dc:dc + 1],
                            in1=xT[:, dc, q0:q0 + 512], op0=ALU.mult, op1=ALU.add), [PB[7], RX[dc][tg], R_modAG[l]],
                            [RX[dc][tg]])
                    th.append(to)
                return th

            fillers = []
            for tg in range(4):
                csteps = [cmp_step(tg, h, tail=((lambda tg=tg: select(tg)) if h == 7 else None)) for h in range(8)]
                wheads = att_steps(tg, 2)
                ssteps = [x for hd in att_steps(tg, 1) for x in hd]
                a0 = ssteps[0][0]
                ssteps[0] = ((lambda a0=a0: (sel_mask(), a0())), ssteps[0][1], ssteps[0][2])
                order = [csteps[0], csteps[1]]
                for h in range(8):
                    half = (len(wheads[h]) + 1) // 2
                    order += wheads[h][:half]
                    if h + 2 < 8:
                        order.append(csteps[h + 2])
                    order += wheads[h][half:]
                if tg == 3 and mid_hook is not None:
                    fillers = fillers + [mid_hook]
                pipeline(order + ssteps, 3, fillers)
                fillers = post_thunks(tg)
            for f_ in fillers:
                f_()

        out_res = []
        for s in range(NSEQ):
            for dc in range(8):
                S.dma("sp", xT[:, dc, :], xT_d[s, dc * 128:(dc + 1) * 128, :], [], RX[dc], "x%d" % dc)
            done = False
            for l in range(L):
                if s == 0 and l + 1 < L:
                    ada_pending.extend((l + 1, gq) for gq in range(36))
                ffn(s, l, 0, norm_done=(l > 0), tail_hook=(lambda stage, slots, s=s, l=l: mixer_norm(s, l, 0, stage, slots[0])))
                if stop == (l, "ffn1"):
                    done = True
                    break
                mixer(s, l, norm_done=True, mid_hook=(lambda s=s, l=l: ffn_norm(s, l, 1, 0)))
                if stop == (l, "mix"):
                    done = True
                    break
                ffn(s, l, 1, norm_done=True,
                    tail_hook=((lambda stage, slots, s=s, l=l: ffn_norm(s, l + 1, 0, 0, stage, slots)) if l + 1 < L else None))
                if stop == (l, "ffn2"):
                    done = True
                    break
            if done:
                for dc in range(8):
                    S.dma("sp", out_d[s, dc * 128:(dc + 1) * 128, :], xT[:, dc, :], RX[dc], [], "x%d" % dc)
                continue
            gfc = PO["gf"]
            for tg in range(4):
                t0 = tg * 512
                for dc in range(8):
                    b = dc % 2
                    S.op("act", lambda e, dc=dc, b=b, t0=t0: e.activation(out=sq[b][:, :], in_=xT[:, dc, t0:t0 + 512],
                                                                          func=AF.Square), [RX[dc][tg]], [R_sq[b]])
                    S.op("pe", lambda e, dc=dc, b=b: e.matmul(banks[7][:, :], lhsT=ones, rhs=sq[b][:, :],
                                                              start=(dc == 0), stop=(dc == 7)), [R_sq[b], R_cbf], [PB[7]])
                S.op("act", lambda e: e.activation(out=rt[:, :], in_=banks[7][:, :], func=AF.Sqrt, scale=1.0 / D,
                                                   bias=eps_ap), [PB[7], R_pf], [R_rt])
                S.op("dve", lambda e: e.reciprocal(out=rstd[:, :], in_=rt[:, :]), [R_rt], [R_rstd])
                for dc in range(8):
                    b = dc % 2
                    S.op("dve", lambda e, dc=dc, b=b, t0=t0: e.scalar_tensor_tensor(
                        out=tmpn[b][:, :], in0=xT[:, dc, t0:t0 + 512], scalar=pf[:, gfc + dc:gfc + dc + 1],
                        in1=rstd[:, :], op0=ALU.mult, op1=ALU.mult), [RX[dc][tg], R_pf, R_rstd], [R_tmpn[b]])
                    S.dma("sp", out_d[s, dc * 128:(dc + 1) * 128, t0:t0 + 512], tmpn[b][:, :], [R_tmpn[b]], [],
                          "tmpn%d" % b)
        allres = [r for row in RX for r in row] + R_tmpn
        S.final_wait("sp", allres)
        S.emit()
    return nc


def _consts():
    cb = np.zeros((128, CB_N), np.float32)
    k = np.arange(128)[:, None]
    t = np.arange(128)[None, :]
    cb[:, CB_ID:CB_ID + 128] = np.eye(128, dtype=np.float32)
    cb[:, CB_T0:CB_T0 + 128] = np.where(t >= k, 0.0, NEG)
    cb[:, CB_T1:CB_T1 + 128] = np.where(t < k, 0.0, NEG)
    cb[:, CB_ONE:CB_ONE + 128] = 1.0
    kk = np.arange(2048)[None, :]
    j = np.arange(128)[:, None]
    cb[:, CB_EP:CB_EP + 2048] = ((kk // 64) == j) & (j < 32)
    n = np.arange(128)[:, None]
    cb[:, CB_BC:CB_BC + 2048] = np.where((16 * n + 31 <= kk) & (n < 127), 0.0, NEG)
    jj = np.arange(32)[None, :]
    cb[:, CB_OV:CB_OV + 32] = (n >= 4 * jj - 1) & (n <= 4 * jj + 3) & (n < 127)
    tt = (np.arange(16)[None, :, None] * 128 + np.arange(128)[:, None, None])
    cur = tt // 64
    jb = np.arange(32)[None, None, :]
    fb = np.where(jb > cur, -1e30, np.where((jb == 0) | (jb == cur) | (jb == cur - 1), 1e4, 0.0)).astype(np.float32)
    return cb, fb.reshape(128, 512)


def _prep_shared(L, inp):
    f32 = np.float32
    sh = {}
    sh["w_ada"] = np.ascontiguousarray(inp["w_ada"][:L], dtype=f32)
    wfi = np.asarray(inp["w_ff_in"][:L], dtype=f32)
    g = wfi[..., :DFF].reshape(L, 2, D, 11, 256)
    u = wfi[..., DFF:].reshape(L, 2, D, 11, 256)
    sh["w_ff_in"] = np.ascontiguousarray(np.concatenate([g, u], axis=-1).reshape(L, 2, D, 2 * DFF))
    sh["w_ff_out"] = np.ascontiguousarray(inp["w_ff_out"][:L], dtype=f32)
    wmi = np.asarray(inp["w_mix_in"][:L], dtype=f32)
    q = wmi[:, :, 0:512].reshape(L, D, 8, 64)
    qperm = np.stack([q[:, :, [j, 4 + j], :].reshape(L, D, 128) for j in range(4)], axis=2).reshape(L, D, 512)
    kc, vc, ks, vs, kw, vw = (wmi[:, :, 512 + i * 128:640 + i * 128] for i in range(6))
    gates = wmi[:, :, 1280:1304]
    hc = wmi[:, :, 1304:1816].reshape(L, D, 4, 128)
    bg = wmi[:, :, 1816:2328].reshape(L, D, 4, 128)
    cg = wmi[:, :, 2328:2840].reshape(L, D, 4, 128)
    conv = np.stack([hc, bg, cg], axis=3).reshape(L, D, 1536)
    sh["w_mix_in"] = np.ascontiguousarray(np.concatenate([qperm, kc, vc, ks, kw, conv, vs, vw, gates], axis=-1))
    assert sh["w_mix_in"].shape[-1] == 2840
    sh["w_mix_out"] = np.ascontiguousarray(inp["w_mix_out"][:L], dtype=f32)
    cw = np.zeros((L, 128, CMPW), f32)
    for l in range(L):
        for kind in range(2):
            w1 = np.asarray(inp["w_cmp1"][l, kind], f32).reshape(32, 64, 64).transpose(1, 0, 2).reshape(64, 2048)
            cw[l, :, kind * 2048:(kind + 1) * 2048] = np.concatenate([w1, w1], axis=0)
            pos = np.asarray(inp["cmp_pos"][l, kind], f32).T
            cw[l, :, 4096 + kind * 34:4096 + kind * 34 + 32] = np.concatenate([pos, pos], axis=0)
        cw[l, 0:64, 4164:4228] = inp["w_cmp2"][l, 0]
        cw[l, 0:64, 4228:4292] = inp["w_cmp2"][l, 1]
    sh["cmpw"] = cw
    cb, fb = _consts()
    sh["cbf"] = cb
    return sh, fb


def _prep_pf(L, NSEQ, inp, fb, c_rows):
    PO = pf_offsets(L, NSEQ)
    pf = np.zeros((128, PO["_n"]), np.float32)

    def put(name, arr):
        a = np.asarray(arr, np.float32)
        a = a.reshape(-1, a.shape[-1] // 128, 128)
        a = a.transpose(2, 0, 1).reshape(128, -1)
        pf[:, PO[name]:PO[name] + a.shape[1]] = a

    put("g", inp["g_norm"][:L])
    put("b", inp["b_ada"][:L])
    put("cw", inp["conv_w"][:L])
    put("cb", inp["conv_b"][:L])
    put("gmo", inp["g_mix_out"][:L])
    put("gf", inp["g_final"])
    c = np.asarray(c_rows, np.float32)
    pf[:, PO["c"]:PO["c"] + 8 * NSEQ] = c.reshape(NSEQ, 8, 128).transpose(2, 1, 0).reshape(128, 8 * NSEQ)
    pf[:, PO["eps"]] = EPS
    pf[:, PO["fb"]:PO["fb"] + 512] = fb
    return pf


def run(inp, L=4, NSEQ=2, ncores=NCORES, stop=None, trace=False):
    sh, fb = _prep_shared(L, inp)
    x = np.asarray(inp["x"], np.float32)
    c = np.asarray(inp["c"], np.float32)
    in_maps = []
    for r in range(ncores):
        m = dict(sh)
        xs = x[r * NSEQ:(r + 1) * NSEQ]
        m["xT"] = np.ascontiguousarray(xs.transpose(0, 2, 1))
        m["pf32"] = _prep_pf(L, NSEQ, inp, fb, c[r * NSEQ:(r + 1) * NSEQ])
        in_maps.append(m)
    nc = build_program(L=L, NSEQ=NSEQ, stop=stop)
    res = run_bass_kernel_spmd(nc, in_maps, core_ids=list(range(ncores)), **({"trace": True} if trace else {}))
    out = np.concatenate([np.asarray(r["yT"]).transpose(0, 2, 1) for r in res.results], axis=0)
    return np.ascontiguousarray(out.astype(np.float32)), res


def kernel(x, c, w_ada, b_ada, g_norm, w_ff_in, w_ff_out, w_mix_in, cmp_pos, w_cmp1, w_cmp2, conv_w, conv_b,
           g_mix_out, w_mix_out, g_final):
    inp = dict(x=x, c=c, w_ada=w_ada, b_ada=b_ada, g_norm=g_norm, w_ff_in=w_ff_in, w_ff_out=w_ff_out,
               w_mix_in=w_mix_in, cmp_pos=cmp_pos, w_cmp1=w_cmp1, w_cmp2=w_cmp2, conv_w=conv_w, conv_b=conv_b,
               g_mix_out=g_mix_out, w_mix_out=w_mix_out, g_final=g_final)
    inp = {k: np.asarray(v) for k, v in inp.items()}
    out, _ = run(inp, L=4, NSEQ=2, ncores=NCORES)
    return out
```

```python
import numpy as np
import concourse.bass as bass
import concourse.mybir as mybir
from concourse.bass_utils import run_bass_kernel_spmd
from contextlib import ExitStack

F32 = mybir.dt.float32
BF16 = mybir.dt.bfloat16
AF = mybir.ActivationFunctionType
ALU = mybir.AluOpType

D = 1024
SEQ = 2048
DFF = 2816
NCORES = 8
EPS = 1e-6
NEG = -30000.0
STRICT_WAR = True
CMPW = 2 * 2048 + 2 * 34 + 64 + 64
PF_G = 0


def pf_offsets(L, NSEQ):
    o = {}
    c = 0
    for name, n in (("g", L * 24), ("b", L * 72), ("cw", L * 12), ("cb", L * 4), ("gmo", L * 8), ("gf", 8),
                    ("c", 8 * NSEQ), ("eps", 1), ("fb", 512)):
        o[name] = c
        c += n
    o["_n"] = c
    return o


CB_ID, CB_T0, CB_T1, CB_ONE, CB_EP, CB_BC, CB_OV, CB_N = 0, 128, 256, 384, 512, 2560, 4608, 4640


class Res:
    __slots__ = ("name", "w", "r", "excl")

    def __init__(self, name, excl=False):
        self.name = name
        self.w = None
        self.r = {}
        self.excl = excl


def fence(news, olds):
    acc = {}
    for o in olds:
        if o.w is not None:
            k, v, src = o.w
            if acc.get(k, (0, None))[0] < v:
                acc[k] = (v, src)
        for k, (v, src) in o.r.items():
            if acc.get(k, (0, None))[0] < v:
                acc[k] = (v, src)
    for n in news:
        for k, (v, src) in acc.items():
            if n.r.get(k, (0, None))[0] < v:
                n.r[k] = (v, src)


class Sched:
    ENGS = ("pe", "act", "dve", "pool", "sp")

    def __init__(self, nc, es):
        self.nc = nc
        self.es = es
        self.sems = {}
        self.ops = {n: [] for n in self.ENGS}
        self.cnt = {n: 0 for n in self.ENGS}
        self.seen = {n: {} for n in self.ENGS}
        for n in self.ENGS:
            self.sems[n] = es.enter_context(nc.semaphore("s_" + n))
        self.dma_tot = {}

    def _dma_sem(self, key):
        k = ("dma", key)
        if k not in self.sems:
            self.sems[k] = self.es.enter_context(self.nc.semaphore("d_%d" % len(self.sems)))
            self.dma_tot[k] = 0
        return k

    def _deps(self, en, reads, writes):
        need = {}

        def add(k, v, src, kind):
            if src == en:
                if en == "pe" or (kind == "r" and not STRICT_WAR):
                    return
            if k in self.dma_tot:
                v = max(v, self.dma_tot[k])
            if need.get(k, 0) < v:
                need[k] = v

        for r in reads:
            if r.w is not None:
                add(r.w[0], r.w[1], r.w[2], "w")
            if r.excl:
                for k, (v, src) in r.r.items():
                    add(k, v, src, "r")
        for w in writes:
            if w.w is not None:
                add(w.w[0], w.w[1], w.w[2], "w")
            for k, (v, src) in w.r.items():
                add(k, v, src, "r")
        seen = self.seen[en]
        out = []
        for k, v in need.items():
            if seen.get(k, 0) < v:
                seen[k] = v
                out.append((k, v))
        return out

    @staticmethod
    def _mark(tok, reads, writes):
        k, v, src = tok
        for r in reads:
            r.r[k] = (v, src)
        for w in writes:
            w.w = tok
            w.r = {}

    def op(self, en, fn, reads=(), writes=()):
        waits = self._deps(en, reads, writes)
        self.cnt[en] += 1
        self._mark((en, self.cnt[en], en), reads, writes)
        self.ops[en].append((waits, fn, None))

    def dma(self, q, out_ap, in_ap, reads, writes, key):
        waits = self._deps(q, reads, writes)
        k = self._dma_sem(key)
        self.dma_tot[k] += 16
        self._mark((k, self.dma_tot[k], "dma"), reads, writes)
        self.ops[q].append((waits, (out_ap, in_ap), k))

    def final_wait(self, en, ress):
        waits = self._deps(en, ress, ress)
        self.ops[en].append((waits, None, None))

    def emit(self):
        nc = self.nc
        sems = self.sems
        ops = self.ops
        with nc.Block() as block:
            def body(en, e):
                mysem = sems[en]
                for waits, fn, dk in ops[en]:
                    for k, v in waits:
                        e.wait_ge(sems[k], v)
                    if fn is None:
                        continue
                    if dk is not None:
                        e.dma_start(out=fn[0], in_=fn[1]).then_inc(sems[dk], 16)
                    else:
                        fn(e).then_inc(mysem, 1)

            @block.tensor
            def _(e):
                body("pe", e)

            @block.scalar
            def _(e):
                body("act", e)

            @block.vector
            def _(e):
                body("dve", e)

            @block.gpsimd
            def _(e):
                body("pool", e)

            @block.sync
            def _(e):
                body("sp", e)


class Arena:
    def __init__(self, nc, base=16640, top=229344):
        self.nc = nc
        self.base = base
        self.top = top
        self.cur = base
        self.n = 0

    def alloc(self, name, shape, dtype, at=None):
        esz = 4 if dtype == F32 else 2
        nbytes = int(np.prod(shape[1:])) * esz
        nbytes = (nbytes + 63) // 64 * 64
        if at is None:
            off = self.cur
            self.cur += nbytes
        else:
            off = self.base + at
        assert off + nbytes <= self.top, (name, off, nbytes, self.top)
        self.n += 1
        t = self.nc.alloc_sbuf_tensor_at("%s_%d" % (name, self.n), list(shape), dtype, offset=off)
        return t, (off - self.base) + nbytes


def build_program(L=4, NSEQ=2, stop=None):
    nc = bass.Bass("TRN2", target_bir_lowering=False)
    PO = pf_offsets(L, NSEQ)

    def dram(name, shape, kind="ExternalInput"):
        return nc.dram_tensor(name, list(shape), F32, kind=kind).ap()

    xT_d = dram("xT", [NSEQ, D, SEQ])
    wada_d = dram("w_ada", [L, D, 9216])
    wfi_d = dram("w_ff_in", [L, 2, D, 2 * DFF])
    wfo_d = dram("w_ff_out", [L, 2, DFF, D])
    wmi_d = dram("w_mix_in", [L, D, 2840])
    wmo_d = dram("w_mix_out", [L, D, D])
    cmpw_d = dram("cmpw", [L, 128, CMPW])
    pf_d = dram("pf32", [128, PO["_n"]])
    cbf_d = dram("cbf", [128, CB_N])
    out_d = dram("yT", [NSEQ, D, SEQ], kind="ExternalOutput")

    es = ExitStack()
    with es:
        S = Sched(nc, es)
        ar = Arena(nc)
        xT, _ = ar.alloc("xT", [128, 8, SEQ], F32)
        RX = [[Res("x%d_%d" % (dc, tg)) for tg in range(4)] for dc in range(8)]
        cbf, _ = ar.alloc("cbf", [128, CB_N], BF16)
        R_cbf = Res("cbf")
        pf, _ = ar.alloc("pf", [128, PO["_n"]], F32)
        R_pf = Res("pf")
        modT, _ = ar.alloc("modT", [128, NSEQ, L, 72], F32)
        R_mod = [Res("modT%d" % i) for i in range(L)]
        modA, _ = ar.alloc("modA", [128, NSEQ, L, 3, 8], F32)
        modG, _ = ar.alloc("modG", [128, NSEQ, L, 3, 8], F32)
        R_modAG = [Res("modAG%d" % i) for i in range(L)]
        cact, _ = ar.alloc("cact", [128, 8 * NSEQ], BF16)
        R_cact = Res("cact")
        hT_off = ar.cur - ar.base
        hT, _ = ar.alloc("hT", [128, 8, 1024], BF16)
        RH = [Res("h0"), Res("h1")]
        wbuf_all, _ = ar.alloc("wbuf", [128, 2, 8, 512], BF16)
        R_wb = [Res("wb0"), Res("wb1")]
        rt, _ = ar.alloc("rt", [128, 512], F32)
        R_rt = Res("rt")
        rstd, _ = ar.alloc("rstd", [128, 512], F32)
        R_rstd = Res("rstd")
        tmpn = []
        R_tmpn = []
        for i in range(2):
            t, _ = ar.alloc("tmpn", [128, 512], F32)
            tmpn.append(t)
            R_tmpn.append(Res("tmpn%d" % i))
        sq = []
        R_sq = []
        for i in range(2):
            t, _ = ar.alloc("sq", [128, 512], BF16)
            sq.append(t)
            R_sq.append(Res("sq%d" % i))
        PR = ar.cur - ar.base
        cap = ar.top - ar.base

        aT, o = ar.alloc("aT", [128, 22, 1024], BF16, at=PR)
        RA = [Res("a0"), Res("a1")]
        wout = []
        R_wo = []
        for i in range(2):
            t, o = ar.alloc("wout", [128, 22, 256], BF16, at=o)
            wout.append(t)
            R_wo.append(Res("wo%d" % i))
        sg = []
        R_sg = []
        for i in range(2):
            t, o = ar.alloc("sg", [128, 512], BF16, at=o)
            sg.append(t)
            R_sg.append(Res("sg%d" % i))
        rslot = [rstd]
        R_rslot = [R_rstd]
        for i in range(4):
            t, o = ar.alloc("rslot", [128, 512], F32, at=o)
            rslot.append(t)
            R_rslot.append(Res("rslot%d" % i))
        adab2 = []
        R_ada2 = []
        for i in range(2):
            t, o = ar.alloc("adab2", [128, 8, 256], BF16, at=o)
            adab2.append(t)
            R_ada2.append(Res("ada2_%d" % i))
        assert o <= cap, o
        FFN_RES = RA + R_wo + R_sg + R_rslot[1:] + R_ada2
        adab = []
        R_ada = []
        o2 = PR
        for i in range(2):
            t, o2 = ar.alloc("adab", [128, 8, 1152], BF16, at=o2)
            adab.append(t)
            R_ada.append(Res("ada%d" % i))

        qT, o = ar.alloc("qT", [128, 4, SEQ], BF16, at=PR)
        RQ = [Res("q%d" % i) for i in range(4)]
        kpad, o = ar.alloc("kpad", [128, 2, 2, SEQ], BF16, at=o)
        RKS = [Res("ks%d" % i) for i in range(4)]
        vaug, o = ar.alloc("vaug", [128, 2, 16, 2, 65], BF16, at=o)
        RV = [Res("v%d" % i) for i in range(16)]
        sig, o = ar.alloc("sig", [128, 16, 24], F32, at=o)
        RSIG = [Res("sig%d" % i) for i in range(4)]
        ocT, o = ar.alloc("ocT", [128, 4, SEQ], BF16, at=o)
        ROC = [Res("oc%d" % i) for i in range(4)]
        kcmpT, o = ar.alloc("kcmpT", [128, 2, 128], BF16, at=o)
        R_kcmp = Res("kcmp")
        Rc, o = ar.alloc("Rc", [128, 2, 97], BF16, at=o)
        R_Rc = Res("Rc")
        bvec, o = ar.alloc("bvec", [128, 2], F32, at=o)
        R_bvec = Res("bvec")
        slb = []
        R_sl = []
        for i in range(2):
            t, o = ar.alloc("sl", [128, 128], BF16, at=o)
            slb.append(t)
            R_sl.append(Res("sl%d" % i))
        ATT_P = o
        kcvc, o = ar.alloc("kcvc", [128, 2, SEQ], BF16, at=ATT_P)
        R_kcvc = Res("kcvc")
        A_X = o
        ubuf, o = ar.alloc("ubuf", [128, 4, 514], F32, at=A_X)
        RU = [Res("u%d" % i) for i in range(4)]
        ybuf, o = ar.alloc("ybuf", [128, 4, 512], F32, at=o)
        RY = [Res("y%d" % i) for i in range(4)]
        hcs, R_hcs = tmpn[0], R_tmpn[0]
        v0b, R_v0 = tmpn[1], R_tmpn[1]
        assert o <= cap, o
        A_RES = RU + RY
        cmpw, oc_ = ar.alloc("cmpw", [128, CMPW], BF16, at=A_X)
        R_cmpw = Res("cmpw")
        OA, o = ar.alloc("OA", [128, 4, 512], F32, at=ATT_P)
        OA2, _ = ar.alloc("OA2", [128, 4, 512], F32, at=hT_off)
        R_OA = Res("OA")
        negselT, o = ar.alloc("negselT", [128, 2, 512], BF16, at=o)
        R_ns = [Res("ns")]
        ptb = []
        R_pt = []
        for i in range(5):
            t, o = ar.alloc("pt", [128, 512], BF16, at=o)
            ptb.append(t)
            R_pt.append(Res("pt%d" % i))
        oaT, o = ar.alloc("oaT", [128, 4, 512], BF16, at=o)
        R_oaT = Res("oaT")
        OAn, o = ar.alloc("OAn", [128, 2, 512], BF16, at=o)
        R_OAn = [Res("OAn0"), Res("OAn1")]
        imp, o = ar.alloc("imp", [128, 2, 4, 32], F32, at=o)
        R_imp = Res("imp")
        nsb, o = ar.alloc("nsb", [128, 2, 4, 32], BF16, at=o)
        R_nsb = Res("nsb")
        top8, o = ar.alloc("top8", [128, 4, 8], F32, at=o)
        R_top8 = Res("top8")
        smalls = []
        R_small = []
        for i in range(6):
            t, o = ar.alloc("sm", [128, 4, 1], F32, at=o)
            smalls.append(t)
            R_small.append(Res("sm%d" % i))
        junk, o = ar.alloc("junk", [128, 512], BF16, at=o)
        R_junk = Res("junk")
        assert o <= cap, o
        B_RES = [R_OA] + R_ns + R_pt + R_OAn + [R_oaT, R_imp, R_nsb, R_top8, R_junk] + R_small
        ATT_RES = (RQ + RKS + RV + RSIG + ROC + [R_kcmp, R_Rc, R_bvec, R_kcvc, R_cmpw] + R_sl + A_RES + B_RES)

        banks = [nc.alloc_psum_tensor("bk%d" % i, [128, 512], F32) for i in range(8)]
        PB = [Res("pb%d" % i, excl=True) for i in range(8)]

        ident = cbf[:, CB_ID:CB_ID + 128]
        T0 = cbf[:, CB_T0:CB_T0 + 128]
        T1 = cbf[:, CB_T1:CB_T1 + 128]
        ones = cbf[:, CB_ONE:CB_ONE + 128]
        eps_ap = pf[:, PO["eps"]:PO["eps"] + 1]

        def wb(i):
            return wbuf_all[:, i]

        for c0 in range(0, CB_N, 2048):
            c1 = min(CB_N, c0 + 2048)
            S.dma("pool", cbf[:, c0:c1], cbf_d[:, c0:c1], [], [R_cbf], "cbf")
        S.dma("sp", pf[:, :], pf_d[:, :], [], [R_pf], "pf")
        S.op("act", lambda e: e.activation(out=cact[:, :], in_=pf[:, PO["c"]:PO["c"] + 8 * NSEQ], func=AF.Silu),
             [R_pf], [R_cact])
        gi = 0
        for l in range(1):
            for cg in range(8):
                ab = adab[gi % 2]
                Rab = R_ada[gi % 2]
                src = wada_d[l].rearrange("(kc p) c -> p kc c", p=128)[:, :, cg * 1152:(cg + 1) * 1152]
                S.dma("pool", ab[:, :, :], src, [], [Rab], "ada%d" % (gi % 2))
                bk = gi % 2

                def f(e, ab=ab, bk=bk):
                    ins = None
                    for ch in range(9):
                        for kc in range(8):
                            ins = e.matmul(banks[bk][:, ch * NSEQ:(ch + 1) * NSEQ], lhsT=ab[:, kc, ch * 128:(ch + 1) * 128],
                                           rhs=cact[:, kc * NSEQ:(kc + 1) * NSEQ], start=(kc == 0), stop=(kc == 7),
                                           skip_group_check=True)
                    return ins
                S.op("pe", f, [Rab, R_cact], [PB[bk]])
                for s in range(NSEQ):
                    psv = banks[bk][:, 0:9 * NSEQ].rearrange("p (a b) -> p a b", b=NSEQ)[:, :, s:s + 1]
                    bcol = PO["b"] + l * 72 + cg * 9
                    S.op("dve", lambda e, psv=psv, s=s, l=l, cg=cg, bcol=bcol: e.tensor_tensor(
                        out=modT[:, s, l, cg * 9:(cg + 1) * 9].unsqueeze(2), in0=psv,
                        in1=pf[:, bcol:bcol + 9].unsqueeze(2), op=ALU.add), [PB[bk], R_pf], [R_mod[l]])
                gi += 1

        def mod_derived(l):
            for s in range(NSEQ):
                for sub in range(3):
                    gcol = PO["g"] + (l * 3 + sub) * 8
                    S.op("dve", lambda e, s=s, l=l, sub=sub, gcol=gcol: e.scalar_tensor_tensor(
                        out=modA[:, s, l, sub, :], in0=modT[:, s, l, sub * 24 + 8:sub * 24 + 16], scalar=1.0,
                        in1=pf[:, gcol:gcol + 8], op0=ALU.add, op1=ALU.mult), [R_mod[l], R_pf], [R_modAG[l]])
                    gm = 1.0 if sub == 1 else 0.5
                    S.op("dve", lambda e, s=s, l=l, sub=sub, gm=gm: e.tensor_scalar(
                        out=modG[:, s, l, sub, :], in0=modT[:, s, l, sub * 24 + 16:sub * 24 + 24], scalar1=gm,
                        scalar2=None, op0=ALU.mult), [R_mod[l]], [R_modAG[l]])

        mod_derived(0)
        ada_pending = []

        def ada_group(l, gidx):
            i = st["ada"] % 2
            st["ada"] += 1
            ab = adab2[i]
            src = wada_d[l].rearrange("(kc p) c -> p kc c", p=128)[:, :, gidx * 256:(gidx + 1) * 256]
            S.dma("pool", ab[:, :, :], src, [], [R_ada2[i]], "ada2_%d" % i)

            def f(e):
                ins = None
                for ch in range(2):
                    for kc in range(8):
                        ins = e.matmul(banks[7][:, ch * NSEQ:(ch + 1) * NSEQ], lhsT=ab[:, kc, ch * 128:(ch + 1) * 128],
                                       rhs=cact[:, kc * NSEQ:(kc + 1) * NSEQ], start=(kc == 0), stop=(kc == 7),
                                       skip_group_check=True)
                return ins
            S.op("pe", f, [R_ada2[i], R_cact], [PB[7]])
            for s in range(NSEQ):
                psv = banks[7][:, 0:2 * NSEQ].rearrange("p (a b) -> p a b", b=NSEQ)[:, :, s:s + 1]
                bcol = PO["b"] + l * 72 + gidx * 2
                S.op("dve", lambda e, psv=psv, s=s, bcol=bcol: e.tensor_tensor(
                    out=modT[:, s, l, gidx * 2:(gidx + 1) * 2].unsqueeze(2), in0=psv,
                    in1=pf[:, bcol:bcol + 2].unsqueeze(2), op=ALU.add), [PB[7], R_pf], [R_mod[l]])
            if gidx == 35:
                mod_derived(l)

        def ada_hook(n=2):
            for _ in range(n):
                if ada_pending:
                    ada_group(*ada_pending.pop(0))

        st = {"wb": 0, "wo": 0, "bank": 0, "sb": 0, "pt": 0, "ada": 0}

        def next_wb():
            i = st["wb"] % 2
            st["wb"] += 1
            return i

        def prenorm(s, l, sub, tok0, dst, R_dst):
            norm_stats(tok0, 0)
            norm_apply(s, l, sub, tok0, 0, dst, R_dst)

        def norm_stats(tok0, slot):
            tg = tok0 // 512
            rstd = rslot[slot]
            R_rstd = R_rslot[slot]
            for dc in range(8):
                b = dc % 2
                S.op("act", lambda e, dc=dc, b=b: e.activation(out=sq[b][:, :], in_=xT[:, dc, tok0:tok0 + 512],
                                                              func=AF.Square), [RX[dc][tg]], [R_sq[b]])
                S.op("pe", lambda e, dc=dc, b=b: e.matmul(banks[7][:, :], lhsT=ones, rhs=sq[b][:, :], start=(dc == 0),
                                                         stop=(dc == 7)), [R_sq[b], R_cbf], [PB[7]])
            S.op("act", lambda e: e.activation(out=rt[:, :], in_=banks[7][:, :], func=AF.Sqrt, scale=1.0 / D,
                                               bias=eps_ap), [PB[7], R_pf], [R_rt])
            S.op("dve", lambda e: e.reciprocal(out=rstd[:, :], in_=rt[:, :]), [R_rt], [R_rstd])

        def norm_apply(s, l, sub, tok0, slot, dst, R_dst):
            tg = tok0 // 512
            rstd = rslot[slot]
            R_rstd = R_rslot[slot]
            for dc in range(8):
                b = dc % 2
                S.op("dve", lambda e, dc=dc, b=b: e.scalar_tensor_tensor(
                    out=tmpn[b][:, :], in0=xT[:, dc, tok0:tok0 + 512], scalar=modA[:, s, l, sub, dc:dc + 1],
                    in1=rstd[:, :], op0=ALU.mult, op1=ALU.mult), [RX[dc][tg], R_modAG[l], R_rstd], [R_tmpn[b]])
                S.op("act", lambda e, dc=dc, b=b: e.activation(
                    out=dst(dc), in_=tmpn[b][:, :], func=AF.Identity,
                    bias=modT[:, s, l, sub * 24 + dc:sub * 24 + dc + 1], scale=1.0), [R_tmpn[b], R_mod[l]], [R_dst])

        def ffn_norm(s, l, j, half, stage=None, slots=(0, 0)):
            sub = 0 if j == 0 else 2
            for tgi in range(2):
                tok0 = half * 1024 + tgi * 512
                if stage in (None, "stats"):
                    norm_stats(tok0, slots[tgi])
                if stage in (None, "apply"):
                    norm_apply(s, l, sub, tok0, slots[tgi],
                               lambda dc, tgi=tgi: hT[:, dc, tgi * 512:(tgi + 1) * 512], RH[tgi])

        def ffn(s, l, j, norm_done=False, tail_hook=None):
            sub = 0 if j == 0 else 2
            fence(FFN_RES, ATT_RES + R_ada)
            if not norm_done:
                ffn_norm(s, l, j, 0)
            for half in range(2):
                for fg in range(11):
                    if fg == 2:
                        if half == 0:
                            ffn_norm(s, l, j, 1, "stats", (1, 2))
                        elif tail_hook is not None:
                            tail_hook("stats", (3, 4))
                    wi = next_wb()
                    w = wb(wi)
                    src = wfi_d[l, j].rearrange("(kc p) c -> p kc c", p=128)[:, :, fg * 512:(fg + 1) * 512]
                    S.dma("pool", w[:, :, :], src, [], [R_wb[wi]], "wb%d" % wi)
                    for tgi in range(2):
                        bs = 4 * (st["bank"] % 2)
                        st["bank"] += 1
                        for oc in range(4):
                            def f(e, w=w, oc=oc, tgi=tgi, bs=bs):
                                ins = None
                                for kc in range(8):
                                    ins = e.matmul(banks[bs + oc][:, :], lhsT=w[:, kc, oc * 128:(oc + 1) * 128],
                                                   rhs=hT[:, kc, tgi * 512:(tgi + 1) * 512], start=(kc == 0),
                                                   stop=(kc == 7))
                                return ins
                            S.op("pe", f, [R_wb[wi], RH[tgi]], [PB[bs + oc]])
                        for i in range(2):
                            S.op("act", lambda e, i=i, bs=bs: e.activation(out=sg[i][:, :], in_=banks[bs + i][:, :],
                                                                           func=AF.Silu), [PB[bs + i]], [R_sg[i]])
                            S.op("dve", lambda e, i=i, bs=bs, fg=fg, tgi=tgi: e.tensor_tensor(
                                out=aT[:, 2 * fg + i, tgi * 512:(tgi + 1) * 512], in0=sg[i][:, :],
                                in1=banks[bs + 2 + i][:, :], op=ALU.mult), [R_sg[i], PB[bs + 2 + i]], [RA[tgi]])
                    if j == 0 and s == 0:
                        ada_hook(2)
                if half == 0:
                    ffn_norm(s, l, j, 1, "apply", (1, 2))
                elif tail_hook is not None:
                    tail_hook("apply", (3, 4))
                for dmp in range(4):
                    oi = st["wo"] % 2
                    st["wo"] += 1
                    wo = wout[oi]
                    src = wfo_d[l, j].rearrange("(fc p) c -> p fc c", p=128)[:, :, dmp * 256:(dmp + 1) * 256]
                    S.dma("pool", wo[:, :, :], src, [], [R_wo[oi]], "wo%d" % oi)
                    for dmi in range(2):
                        dc = dmp * 2 + dmi
                        for tgi in range(2):
                            bk = st["sb"] % 8
                            st["sb"] += 1
                            tg = half * 2 + tgi

                            def f(e, wo=wo, dmi=dmi, tgi=tgi, bk=bk):
                                ins = None
                                for fc in range(22):
                                    ins = e.matmul(banks[bk][:, :], lhsT=wo[:, fc, dmi * 128:(dmi + 1) * 128],
                                                   rhs=aT[:, fc, tgi * 512:(tgi + 1) * 512], start=(fc == 0),
                                                   stop=(fc == 21))
                                return ins
                            S.op("pe", f, [R_wo[oi], RA[tgi]], [PB[bk]])
                            S.op("dve", lambda e, dc=dc, tg=tg, bk=bk: e.scalar_tensor_tensor(
                                out=xT[:, dc, tg * 512:(tg + 1) * 512], in0=banks[bk][:, :],
                                scalar=modG[:, s, l, sub, dc:dc + 1], in1=xT[:, dc, tg * 512:(tg + 1) * 512],
                                op0=ALU.mult, op1=ALU.add), [PB[bk], RX[dc][tg], R_modAG[l]], [RX[dc][tg]])

        def mixer_norm(s, l, tg, stage=None, slot=0):
            hb = tg % 2
            if stage in (None, "stats"):
                norm_stats(tg * 512, slot)
            if stage in (None, "apply"):
                norm_apply(s, l, 1, tg * 512, slot, lambda dc, hb=hb: hT[:, dc, hb * 512:(hb + 1) * 512], RH[hb])

        def mixer(s, l, norm_done=False, mid_hook=None):
            fence(ATT_RES, FFN_RES + R_ada)
            cwc = PO["cw"] + l * 12
            cbc = PO["cb"] + l * 4
            gmc = PO["gmo"] + l * 8
            S.op("dve", lambda e: e.memset(vaug[:, :, :, :, 64:65], 1.0), [], RV)
            S.op("dve", lambda e: e.memset(Rc[:, :, :], 0.0), [], [R_Rc])
            for g in range(2):
                S.op("dve", lambda e, g=g: e.tensor_copy(out=Rc[:, g, 0:32], in_=cbf[:, CB_OV:CB_OV + 32]),
                     [R_cbf], [R_Rc])
            S.op("dve", lambda e: e.memset(Rc[:, :, 96:97], 1.0), [], [R_Rc])
            S.op("dve", lambda e: e.memset(kcmpT[:, :, :], 0.0), [], [R_kcmp])
            S.op("dve", lambda e: e.memset(kpad[64:128, :, 0, :], 0.0), [], RKS)
            S.op("dve", lambda e: e.memset(kpad[0:64, :, 1, :], 0.0), [], RKS)
            S.op("dve", lambda e: e.memset(ubuf[:, :, 0:2], 0.0), [], RU)

            def load_w(c0, n):
                wi = next_wb()
                w = wb(wi)
                src = wmi_d[l].rearrange("(kc p) c -> p kc c", p=128)[:, :, c0:c0 + n]
                S.dma("pool", w[:, :, 0:n], src, [], [R_wb[wi]], "wb%d" % wi)
                return w, R_wb[wi]

            def proj(w, Rw, col, hb, bk):
                def f(e):
                    ins = None
                    for kc in range(8):
                        ins = e.matmul(banks[bk][:, :], lhsT=w[:, kc, col:col + 128],
                                       rhs=hT[:, kc, hb * 512:(hb + 1) * 512], start=(kc == 0), stop=(kc == 7))
                    return ins
                S.op("pe", f, [Rw, RH[hb]], [PB[bk]])

            def phaseA(tg):
                hb = tg % 2
                cs = slice(tg * 512, (tg + 1) * 512)
                if tg == 0 and not norm_done:
                    mixer_norm(s, l, 0)
                w, Rw = load_w(0, 512)
                for c in range(4):
                    proj(w, Rw, c * 128, hb, c)
                for c in range(4):
                    if c % 2 == 0:
                        S.op("act", lambda e, c=c: e.activation(out=qT[:, c, cs], in_=banks[c][:, :], func=AF.Identity,
                                                                scale=0.125), [PB[c]], [RQ[tg]])
                    else:
                        S.op("dve", lambda e, c=c: e.tensor_scalar(out=qT[:, c, cs], in0=banks[c][:, :], scalar1=0.125,
                                                                   scalar2=None, op0=ALU.mult), [PB[c]], [RQ[tg]])
                w, Rw = load_w(512, 512)
                for c in range(4):
                    proj(w, Rw, c * 128, hb, 4 + c)
                S.op("act", lambda e: e.copy(out=kcvc[:, 0, cs], in_=banks[4][:, :]), [PB[4]], [R_kcvc])
                S.op("dve", lambda e: e.tensor_copy(out=kcvc[:, 1, cs], in_=banks[5][:, :]), [PB[5]], [R_kcvc])
                for kind in range(2):
                    S.op("act", lambda e, kind=kind: e.copy(out=kpad[0:64, kind, 0, cs], in_=banks[6 + kind][0:64, :]),
                         [PB[6 + kind]], [RKS[tg]])
                    S.op("dve", lambda e, kind=kind: e.tensor_copy(out=kpad[64:128, kind, 1, cs],
                                                                   in_=banks[6 + kind][64:128, :]),
                         [PB[6 + kind]], [RKS[tg]])
                if tg < 3:
                    mixer_norm(s, l, tg + 1)
                for cc in range(4):
                    w, Rw = load_w(1024 + cc * 384, 384)
                    b0 = (cc * 3) % 8
                    bh, bb, bc_ = b0, (b0 + 1) % 8, (b0 + 2) % 8
                    proj(w, Rw, 0, hb, bh)
                    proj(w, Rw, 128, hb, bb)
                    proj(w, Rw, 256, hb, bc_)
                    S.op("act", lambda e, bh=bh: e.copy(out=hcs[:, :], in_=banks[bh][:, :]), [PB[bh]], [R_hcs])
                    S.op("dve", lambda e, cc=cc, bc_=bc_: e.tensor_tensor(out=ubuf[:, cc, 2:514], in0=hcs[:, :],
                                                                          in1=banks[bc_][:, :], op=ALU.mult),
                         [R_hcs, PB[bc_]], [RU[cc]])
                    S.op("act", lambda e, cc=cc: e.activation(
                        out=v0b[:, :], in_=ubuf[:, cc, 2:514], func=AF.Identity,
                        scale=pf[:, cwc + 8 + cc:cwc + 9 + cc], bias=pf[:, cbc + cc:cbc + cc + 1]),
                        [RU[cc], R_pf], [R_v0])
                    S.op("dve", lambda e, cc=cc: e.scalar_tensor_tensor(
                        out=v0b[:, :], in0=ubuf[:, cc, 1:513], scalar=pf[:, cwc + 4 + cc:cwc + 5 + cc], in1=v0b[:, :],
                        op0=ALU.mult, op1=ALU.add), [RU[cc], R_pf, R_v0], [R_v0])
                    S.op("dve", lambda e, cc=cc: e.scalar_tensor_tensor(
                        out=v0b[:, :], in0=ubuf[:, cc, 0:512], scalar=pf[:, cwc + cc:cwc + cc + 1], in1=v0b[:, :],
                        op0=ALU.mult, op1=ALU.add), [RU[cc], R_pf, R_v0], [R_v0])
                    S.op("dve", lambda e, cc=cc, bb=bb: e.tensor_tensor(out=ybuf[:, cc, :], in0=v0b[:, :],
                                                                        in1=banks[bb][:, :], op=ALU.mult),
                         [R_v0, PB[bb]], [RY[cc]])
                    S.op("act", lambda e, cc=cc: e.copy(out=ubuf[:, cc, 0:2], in_=ubuf[:, cc, 512:514]),
                         [RU[cc]], [RU[cc]])
                w, Rw = load_w(2560, 280)
                for tile in range(4):
                    ti = tg * 4 + tile
                    bk = 3 + tile

                    def f(e, w=w, tile=tile, bk=bk):
                        ins = None
                        for kc in range(8):
                            ins = e.matmul(banks[bk][:, 0:280],
                                           lhsT=hT[:, kc, hb * 512 + tile * 128:hb * 512 + (tile + 1) * 128],
                                           rhs=w[:, kc, 0:280], start=(kc == 0), stop=(kc == 7))
                        return ins
                    S.op("pe", f, [Rw, RH[hb]], [PB[bk]])
                    S.op("act", lambda e, ti=ti, bk=bk: e.copy(
                        out=vaug[:, 0, ti, :, 0:64], in_=banks[bk][:, 0:128].rearrange("p (a b) -> p a b", a=2)),
                        [PB[bk]], [RV[ti]])
                    S.op("dve", lambda e, ti=ti, bk=bk: e.tensor_copy(
                        out=vaug[:, 1, ti, :, 0:64], in_=banks[bk][:, 128:256].rearrange("p (a b) -> p a b", a=2)),
                        [PB[bk]], [RV[ti]])
                    S.op("act", lambda e, ti=ti, bk=bk: e.activation(out=sig[:, ti, :], in_=banks[bk][:, 256:280],
                                                                     func=AF.Sigmoid), [PB[bk]], [RSIG[tg]])
                for cc in range(4):
                    b = cc % 2
                    S.op("act", lambda e, cc=cc, b=b: e.activation(out=sq[b][:, :], in_=ybuf[:, cc, :], func=AF.Square),
                         [RY[cc]], [R_sq[b]])
                    S.op("pe", lambda e, cc=cc, b=b: e.matmul(banks[7][:, :], lhsT=ones, rhs=sq[b][:, :],
                                                              start=(cc == 0), stop=(cc == 3)),
                         [R_sq[b], R_cbf], [PB[7]])
                S.op("act", lambda e: e.activation(out=rt[:, :], in_=banks[7][:, :], func=AF.Sqrt, scale=1.0 / 512,
                                                   bias=eps_ap), [PB[7], R_pf], [R_rt])
                S.op("dve", lambda e: e.reciprocal(out=rstd[:, :], in_=rt[:, :]), [R_rt], [R_rstd])
                for cc in range(4):
                    S.op("dve", lambda e, cc=cc: e.scalar_tensor_tensor(
                        out=ocT[:, cc, cs], in0=ybuf[:, cc, :], scalar=pf[:, gmc + 4 + cc:gmc + 5 + cc],
                        in1=rstd[:, :], op0=ALU.mult, op1=ALU.mult), [RY[cc], R_pf, R_rstd], [ROC[tg]])

            for tg_ in range(4):
                phaseA(tg_)

            fence([R_cmpw], A_RES)
            for c0 in range(0, CMPW, 2048):
                c1 = min(CMPW, c0 + 2048)
                S.dma("pool", cmpw[:, c0:c1], cmpw_d[l, :, c0:c1], [], [R_cmpw], "cmpw")
            w2k = cmpw[0:64, 4164:4228]
            w2v = cmpw[0:64, 4228:4292]
            for kind in range(2):
                w1 = cmpw[:, kind * 2048:(kind + 1) * 2048]
                pos = cmpw[:, 4096 + kind * 34:4096 + (kind + 1) * 34]

                def fb(e, w1=w1, pos=pos, kind=kind):
                    ins = None
                    for lp in range(32):
                        ins = e.matmul(banks[2][0:64, kind * 2:kind * 2 + 2], lhsT=w1[0:64, lp * 64:(lp + 1) * 64],
                                       rhs=pos[0:64, lp:lp + 2], start=(lp == 0), stop=(lp == 31), skip_group_check=True)
                    return ins
                S.op("pe", fb, [R_cmpw], [PB[2]])
                S.op("dve", lambda e, kind=kind: e.tensor_copy(out=bvec[0:64, kind:kind + 1],
                                                               in_=banks[2][0:64, kind * 2:kind * 2 + 1]),
                     [PB[2]], [R_bvec])
                for g in range(2):
                    rows = slice(64 * g, 64 * g + 64)
                    sl = slb[g]

                    def fc(e, w1=w1, kind=kind, g=g, rows=rows):
                        ins = None
                        for lp in range(32):
                            ins = e.matmul(banks[g][0:64, 0:127], lhsT=w1[rows, lp * 64:(lp + 1) * 64],
                                           rhs=kcvc[rows, kind, lp:lp + 16 * 126 + 1:16], start=(lp == 0), stop=(lp == 31))
                        return ins
                    S.op("pe", fc, [R_cmpw, R_kcvc], [PB[g]])
                    S.op("act", lambda e, g=g, sl=sl, kind=kind: e.activation(
                        out=sl[0:64, 0:127], in_=banks[g][0:64, 0:127], func=AF.Silu, bias=bvec[0:64, kind:kind + 1],
                        scale=1.0), [PB[g], R_bvec], [R_sl[g]])
                    if kind == 0:
                        S.op("pe", lambda e, sl=sl, rows=rows: e.matmul(banks[3][rows, 0:127], lhsT=w2k,
                                                                        rhs=sl[0:64, 0:127], start=True, stop=True,
                                                                        skip_group_check=True),
                             [R_cmpw, R_sl[g]], [PB[3]])
                        S.op("dve", lambda e, rows=rows, g=g: e.tensor_copy(out=kcmpT[rows, g, 0:127],
                                                                            in_=banks[3][rows, 0:127]),
                             [PB[3]], [R_kcmp])
                    else:
                        S.op("pe", lambda e, sl=sl, g=g: e.matmul(banks[3][0:127, 128 + g * 64:192 + g * 64],
                                                                  lhsT=sl[0:64, 0:127], rhs=w2v, start=True, stop=True,
                                                                  skip_group_check=True),
                             [R_cmpw, R_sl[g]], [PB[3]])
                        S.op("dve", lambda e, g=g: e.tensor_copy(out=Rc[0:127, g, 32:96],
                                                                 in_=banks[3][0:127, 128 + g * 64:192 + g * 64]),
                             [PB[3]], [R_Rc])

            fence(B_RES, A_RES + [R_cmpw, R_kcvc])
            S.op("dve", lambda e: e.memset(negselT[:, :, :], 0.0), [], R_ns)
            wmo = wbuf_all.rearrange("p a k c -> p (a k c)").rearrange("p (k c) -> p k c", c=1024)
            for hf in range(2):
                src = wmo_d[l].rearrange("(kc p) c -> p kc c", p=128)[:, hf * 4:(hf + 1) * 4, :]
                S.dma("pool", wmo[:, hf * 4:(hf + 1) * 4, :], src, [], [R_wb[hf]], "wb%d" % hf)
            st["wb"] = 0
            rz, zc, rzg, ssq, rt4, rs4 = smalls
            R_rz, R_zc, R_rzg, R_ssq, R_rt4, R_rs4 = R_small
            fbc = PO["fb"]

            def next_sb():
                i = st["sb"] % 3
                st["sb"] += 1
                return i

            def next_pt():
                i = st["pt"] % 5
                st["pt"] += 1
                return i

            def next_sb5():
                i = (0, 1, 2, 5, 6)[st["sb"] % 5]
                st["sb"] += 1
                return i

            OAs = [OA, OA2]
            R_OAs = [[R_OA], [RH[0], RH[1]]]

            def finalize(bk, width, zcol, vcol, h, br, tg, first, clampz):
                OAt = OAs[tg % 2]
                R_OAt = R_OAs[tg % 2]
                view = banks[bk][:, 0:4 * width].rearrange("p (a b) -> p a b", b=width)
                if clampz:
                    S.op("dve", lambda e: e.tensor_scalar(out=zc[:, :, :], in0=view[:, :, zcol:zcol + 1], scalar1=1e-30,
                                                          scalar2=None, op0=ALU.max), [PB[bk]], [R_zc])
                    S.op("dve", lambda e: e.reciprocal(out=rz[:, :, :], in_=zc[:, :, :]), [R_zc], [R_rz])
                else:
                    S.op("dve", lambda e: e.reciprocal(out=rz[:, :, :], in_=view[:, :, zcol:zcol + 1]), [PB[bk]], [R_rz])
                gcol = h * 3 + br
                S.op("dve", lambda e: e.tensor_tensor(out=rzg[:, :, :], in0=rz[:, :, :],
                                                      in1=sig[:, tg * 4:(tg + 1) * 4, gcol:gcol + 1], op=ALU.mult),
                     [R_rz, RSIG[tg]], [R_rzg])
                for tile in range(4):
                    if first:
                        S.op("dve", lambda e, tile=tile: e.tensor_scalar(
                            out=OAt[:, tile, h * 64:(h + 1) * 64], in0=view[:, tile, vcol:vcol + 64],
                            scalar1=rzg[:, tile, :], scalar2=None, op0=ALU.mult), [PB[bk], R_rzg], R_OAt)
                    else:
                        S.op("dve", lambda e, tile=tile: e.scalar_tensor_tensor(
                            out=OAt[:, tile, h * 64:(h + 1) * 64], in0=view[:, tile, vcol:vcol + 64],
                            scalar=rzg[:, tile, :], in1=OAt[:, tile, h * 64:(h + 1) * 64], op0=ALU.mult, op1=ALU.add),
                            [PB[bk], R_rzg] + R_OAt, R_OAt)

            def pipeline(steps, skew, fillers=(), every=2, start_at=3):
                n = len(steps)
                fi = 0
                for i in range(n + skew):
                    if i < n:
                        steps[i][0]()
                        steps[i][1]()
                    if i - skew >= 0:
                        steps[i - skew][2]()
                    if fi < len(fillers) and i >= start_at and (i - start_at) % every == 0:
                        fillers[fi]()
                        fi += 1
                while fi < len(fillers):
                    fillers[fi]()
                    fi += 1

            def next_s4():
                i = (0, 1, 2, 5)[st["sb"] % 4]
                st["sb"] += 1
                return i

            def cmp_step(tg, h, tail=None):
                q0 = tg * 512
                g, hh = h // 4, h % 4
                cb_ = 6
                rs_ = {}

                def A():
                    sb = rs_["sb"] = next_s4()
                    rs_["pi"] = next_pt()

                    def f(e):
                        e.matmul(banks[sb][:, :], lhsT=kcmpT[:, g, :], rhs=qT[:, hh, q0:q0 + 512], start=True,
                                 stop=False)
                        return e.matmul(banks[sb][:, :], lhsT=ident, rhs=cbf[:, CB_BC + q0:CB_BC + q0 + 512],
                                        start=False, stop=True)
                    S.op("pe", f, [R_kcmp, RQ[tg], R_cbf], [PB[sb]])

                def B():
                    sb, pi = rs_["sb"], rs_["pi"]
                    pt = ptb[pi]
                    S.op("act", lambda e: e.activation(out=pt[:, :], in_=banks[sb][:, :], func=AF.Exp),
                         [PB[sb]], [R_pt[pi]])

                def C():
                    pi = rs_["pi"]
                    pt = ptb[pi]

                    def f2(e):
                        ins = None
                        for tile in range(4):
                            ins = e.matmul(banks[cb_][:, tile * 97:(tile + 1) * 97],
                                           lhsT=pt[:, tile * 128:(tile + 1) * 128], rhs=Rc[:, g, :],
                                           start=(tile == 0), stop=(tile == 3), skip_group_check=True)
                        return ins
                    S.op("pe", f2, [R_pt[pi], R_Rc], [PB[cb_]])
                    finalize(cb_, 97, 96, 32, h, 0, tg, True, True)
                    view = banks[cb_][:, 0:4 * 97].rearrange("p (a b) -> p a b", b=97)
                    for tile in range(4):
                        if hh == 0:
                            S.op("dve", lambda e, tile=tile: e.tensor_scalar(
                                out=imp[:, g, tile, :], in0=view[:, tile, 0:32], scalar1=rz[:, tile, :],
                                scalar2=None, op0=ALU.mult), [PB[cb_], R_rz], [R_imp])
                        else:
                            S.op("dve", lambda e, tile=tile: e.scalar_tensor_tensor(
                                out=imp[:, g, tile, :], in0=view[:, tile, 0:32], scalar=rz[:, tile, :],
                                in1=imp[:, g, tile, :], op0=ALU.mult, op1=ALU.add), [PB[cb_], R_rz, R_imp],
                                [R_imp])
                    if tail is not None:
                        tail()
                return (A, B, C)

            def select(tg):
                for g in range(2):
                    S.op("dve", lambda e, g=g: e.tensor_tensor(
                        out=imp[:, g, :, :], in0=imp[:, g, :, :],
                        in1=pf[:, fbc + tg * 128:fbc + (tg + 1) * 128].rearrange("p (a b) -> p a b", b=32), op=ALU.add),
                        [R_imp, R_pf], [R_imp])
                    for tile in range(4):
                        S.op("dve", lambda e, tile=tile, g=g: e.max(out=top8[:, tile, :], in_=imp[:, g, tile, :]),
                             [R_imp], [R_top8])
                        S.op("dve", lambda e, tile=tile, g=g: e.tensor_scalar(
                            out=nsb[:, g, tile, :], in0=imp[:, g, tile, :], scalar1=top8[:, tile, 7:8], scalar2=NEG,
                            op0=ALU.is_lt, op1=ALU.mult), [R_imp, R_top8], [R_nsb])

            pst = banks[7][0:32, 0:512].bitcast(BF16)

            def sel_mask():
                def ftn(e):
                    ins = None
                    for g in range(2):
                        for tile in range(4):
                            ins = e.transpose(out=pst[:, (g * 4 + tile) * 128:(g * 4 + tile + 1) * 128],
                                              in_=nsb[:, g, tile, :], identity=ident)
                    return ins
                S.op("pe", ftn, [R_nsb, R_cbf], [PB[7]])
                S.op("act", lambda e: e.copy(out=negselT[0:32, :, :], in_=pst.rearrange("p (g c) -> p g c", g=2)),
                     [PB[7]], R_ns)

            def att_step(tg, br, h, g, hh, ob, c, lo, hi, diag, TB, first, last):
                q0 = tg * 512
                rs_ = {}
                bt = lo if TB is T0 else hi - 128

                def A():
                    sb = rs_["sb"] = next_s4()
                    rs_["pi"] = next_pt()

                    def f(e):
                        ins = e.matmul(banks[sb][:, lo:hi], lhsT=kpad[:, br - 1, g, c * 128:(c + 1) * 128],
                                       rhs=qT[:, hh, q0 + lo:q0 + hi], start=True, stop=False,
                                       skip_group_check=True)
                        if br == 1:
                            ins = e.matmul(banks[sb][:, lo:hi], lhsT=cbf[:, CB_EP + c * 128:CB_EP + (c + 1) * 128],
                                           rhs=negselT[:, g, lo:hi], start=False, stop=(not diag),
                                           skip_group_check=True)
                        if diag:
                            ins = e.matmul(banks[sb][:, bt:bt + 128], lhsT=ident, rhs=TB, start=False, stop=True,
                                           skip_group_check=True)
                        return ins
                    rd = [RKS[c // 4], RQ[tg], R_cbf] + (R_ns if br == 1 else [])
                    S.op("pe", f, rd, [PB[sb]])

                def B():
                    sb, pi = rs_["sb"], rs_["pi"]
                    pt = ptb[pi]
                    S.op("act", lambda e: e.activation(out=pt[:, lo:hi], in_=banks[sb][:, lo:hi], func=AF.Exp),
                         [PB[sb]], [R_pt[pi]])

                def C():
                    pi = rs_["pi"]
                    pt = ptb[pi]

                    def f2(e):
                        ins = None
                        for tile in range(lo // 128, hi // 128):
                            ins = e.matmul(banks[ob][:, tile * 65:(tile + 1) * 65],
                                           lhsT=pt[:, tile * 128:(tile + 1) * 128], rhs=vaug[:, br - 1, c, g, :],
                                           start=(first and tile == lo // 128), stop=True, skip_group_check=True)
                        return ins
                    S.op("pe", f2, [R_pt[pi], RV[c]], [PB[ob]])
                    if last:
                        finalize(ob, 65, 64, 0, h, br, tg, False, False)
                return (A, B, C)

            def att_steps(tg, br):
                q0 = tg * 512
                heads = []
                for h in range(8):
                    steps = []
                    heads.append(steps)
                    g, hh = h // 4, h % 4
                    ob = 3 + (st["bank"] % 2)
                    st["bank"] += 1
                    if br == 1:
                        chunks = [(c, max(0, c * 128 - q0), 512, (c * 128 >= q0), T0) for c in range(4 * tg + 4)]
                    else:
                        chunks = []
                        for c in range(max(0, 4 * tg - 4), 4 * tg + 4):
                            m = c - (4 * tg - 4)
                            if m <= 3:
                                chunks.append((c, 0, 128 * (m + 1), True, T1))
                            else:
                                chunks.append((c, 128 * (m - 4), 512, True, T0))
                    for ci, (c, lo, hi, diag, TB) in enumerate(chunks):
                        steps.append(att_step(tg, br, h, g, hh, ob, c, lo, hi, diag, TB, ci == 0,
                                              ci == len(chunks) - 1))
                return heads

            def post_thunks(tg):
                q0 = tg * 512
                OAt = OAs[tg % 2]
                R_OAt = R_OAs[tg % 2]
                th = []

                def t0():
                    for tile in range(4):
                        S.op("act", lambda e, tile=tile: e.activation(out=junk[:, :], in_=OAt[:, tile, :],
                                                                      func=AF.Square, accum_out=ssq[:, tile, :]),
                             R_OAt, [R_junk, R_ssq])
                    S.op("act", lambda e: e.activation(out=rt4[:, :, :], in_=ssq[:, :, :], func=AF.Sqrt,
                                                       scale=1.0 / 512, bias=eps_ap), [R_ssq, R_pf], [R_rt4])
                    S.op("dve", lambda e: e.reciprocal(out=rs4[:, :, :], in_=rt4[:, :, :]), [R_rt4], [R_rs4])
                th.append(t0)
                for tile in range(4):
                    def tt(tile=tile):
                        oi = tile % 2
                        S.op("act", lambda e: e.activation(out=OAn[:, oi, :], in_=OAt[:, tile, :], func=AF.Identity,
                                                           scale=rs4[:, tile, :]), R_OAt + [R_rs4], [R_OAn[oi]])
                        pso = banks[7][:, 0:512].bitcast(BF16)[:, oi * 512:(oi + 1) * 512]

                        def ft(e):
                            ins = None
                            for j in range(4):
                                ins = e.transpose(out=pso[:, j * 128:(j + 1) * 128],
                                                  in_=OAn[:, oi, j * 128:(j + 1) * 128], identity=ident)
                            return ins
                        S.op("pe", ft, [R_OAn[oi], R_cbf], [PB[7]])
                        for j in range(4):
                            S.op("dve", lambda e, j=j: e.tensor_scalar(
                                out=oaT[:, j, tile * 128:(tile + 1) * 128], in0=pso[:, j * 128:(j + 1) * 128],
                                scalar1=pf[:, gmc + j:gmc + j + 1], scalar2=None, op0=ALU.mult),
                                [PB[7], R_pf], [R_oaT])
                    th.append(tt)
                for dc in range(8):
                    def to(dc=dc):
                        def f(e):
                            ins = None
                            for ic in range(8):
                                rhs = oaT[:, ic, :] if ic < 4 else ocT[:, ic - 4, q0:q0 + 512]
                                ins = e.matmul(banks[7][:, :], lhsT=wmo[:, ic, dc * 128:(dc + 1) * 128], rhs=rhs,
                                               start=(ic == 0), stop=(ic == 7))
                            return ins
                        S.op("pe", f, [R_wb[0], R_wb[1], R_oaT, ROC[tg]], [PB[7]])
                        S.op("dve", lambda e: e.scalar_tensor_tensor(
                            out=xT[:, dc, q0:q0 + 512], in0=banks[7][:, :], scalar=modG[:, s, l, 1, dc:dc + 1],
                            in1=xT[:, dc, q0:q0 + 512], op0=ALU.mult, op1=ALU.add), [PB[7], RX[dc][tg], R_modAG[l]],
                            [RX[dc][tg]])
                    th.append(to)
                return th

            fillers = []
            for tg in range(4):
                csteps = [cmp_step(tg, h, tail=((lambda tg=tg: select(tg)) if h == 7 else None)) for h in range(8)]
                wheads = att_steps(tg, 2)
                ssteps = [x for hd in att_steps(tg, 1) for x in hd]
                a0 = ssteps[0][0]
                ssteps[0] = ((lambda a0=a0: (sel_mask(), a0())), ssteps[0][1], ssteps[0][2])
                order = [csteps[0], csteps[1]]
                for h in range(8):
                    half = (len(wheads[h]) + 1) // 2
                    order += wheads[h][:half]
                    if h + 2 < 8:
                        order.append(csteps[h + 2])
                    order += wheads[h][half:]
                pipeline(order + ssteps, 3, fillers)
                fillers = post_thunks(tg)
            for f_ in fillers:
                f_()
            if mid_hook is not None:
                mid_hook()

        out_res = []
        for s in range(NSEQ):
            for dc in range(8):
                S.dma("sp", xT[:, dc, :], xT_d[s, dc * 128:(dc + 1) * 128, :], [], RX[dc], "x%d" % dc)
            done = False
            for l in range(L):
                if s == 0 and l + 1 < L:
                    ada_pending.extend((l + 1, gq) for gq in range(36))
                ffn(s, l, 0, norm_done=(l > 0), tail_hook=(lambda stage, slots, s=s, l=l: mixer_norm(s, l, 0, stage, slots[0])))
                if stop == (l, "ffn1"):
                    done = True
                    break
                mixer(s, l, norm_done=True, mid_hook=None)
                if stop == (l, "mix"):
                    done = True
                    break
                ffn(s, l, 1, norm_done=False,
                    tail_hook=((lambda stage, slots, s=s, l=l: ffn_norm(s, l + 1, 0, 0, stage, slots)) if l + 1 < L else None))
                if stop == (l, "ffn2"):
                    done = True
                    break
            if done:
                for dc in range(8):
                    S.dma("sp", out_d[s, dc * 128:(dc + 1) * 128, :], xT[:, dc, :], RX[dc], [], "x%d" % dc)
                continue
            gfc = PO["gf"]
            for tg in range(4):
                t0 = tg * 512
                for dc in range(8):
                    b = dc % 2
                    S.op("act", lambda e, dc=dc, b=b, t0=t0: e.activation(out=sq[b][:, :], in_=xT[:, dc, t0:t0 + 512],
                                                                          func=AF.Square), [RX[dc][tg]], [R_sq[b]])
                    S.op("pe", lambda e, dc=dc, b=b: e.matmul(banks[7][:, :], lhsT=ones, rhs=sq[b][:, :],
                                                              start=(dc == 0), stop=(dc == 7)), [R_sq[b], R_cbf], [PB[7]])
                S.op("act", lambda e: e.activation(out=rt[:, :], in_=banks[7][:, :], func=AF.Sqrt, scale=1.0 / D,
                                                   bias=eps_ap), [PB[7], R_pf], [R_rt])
                S.op("dve", lambda e: e.reciprocal(out=rstd[:, :], in_=rt[:, :]), [R_rt], [R_rstd])
                for dc in range(8):
                    b = dc % 2
                    S.op("dve", lambda e, dc=dc, b=b, t0=t0: e.scalar_tensor_tensor(
                        out=tmpn[b][:, :], in0=xT[:, dc, t0:t0 + 512], scalar=pf[:, gfc + dc:gfc + dc + 1],
                        in1=rstd[:, :], op0=ALU.mult, op1=ALU.mult), [RX[dc][tg], R_pf, R_rstd], [R_tmpn[b]])
                    S.dma("sp", out_d[s, dc * 128:(dc + 1) * 128, t0:t0 + 512], tmpn[b][:, :], [R_tmpn[b]], [],
                          "tmpn%d" % b)
        allres = [r for row in RX for r in row] + R_tmpn
        S.final_wait("sp", allres)
        S.emit()
    return nc


def _consts():
    cb = np.zeros((128, CB_N), np.float32)
    k = np.arange(128)[:, None]
    t = np.arange(128)[None, :]
    cb[:, CB_ID:CB_ID + 128] = np.eye(128, dtype=np.float32)
    cb[:, CB_T0:CB_T0 + 128] = np.where(t >= k, 0.0, NEG)
    cb[:, CB_T1:CB_T1 + 128] = np.where(t < k, 0.0, NEG)
    cb[:, CB_ONE:CB_ONE + 128] = 1.0
    kk = np.arange(2048)[None, :]
    j = np.arange(128)[:, None]
    cb[:, CB_EP:CB_EP + 2048] = ((kk // 64) == j) & (j < 32)
    n = np.arange(128)[:, None]
    cb[:, CB_BC:CB_BC + 2048] = np.where((16 * n + 31 <= kk) & (n < 127), 0.0, NEG)
    jj = np.arange(32)[None, :]
    cb[:, CB_OV:CB_OV + 32] = (n >= 4 * jj - 1) & (n <= 4 * jj + 3) & (n < 127)
    tt = (np.arange(16)[None, :, None] * 128 + np.arange(128)[:, None, None])
    cur = tt // 64
    jb = np.arange(32)[None, None, :]
    fb = np.where(jb > cur, -1e30, np.where((jb == 0) | (jb == cur) | (jb == cur - 1), 1e4, 0.0)).astype(np.float32)
    return cb, fb.reshape(128, 512)


def _prep_shared(L, inp):
    f32 = np.float32
    sh = {}
    sh["w_ada"] = np.ascontiguousarray(inp["w_ada"][:L], dtype=f32)
    wfi = np.asarray(inp["w_ff_in"][:L], dtype=f32)
    g = wfi[..., :DFF].reshape(L, 2, D, 11, 256)
    u = wfi[..., DFF:].reshape(L, 2, D, 11, 256)
    sh["w_ff_in"] = np.ascontiguousarray(np.concatenate([g, u], axis=-1).reshape(L, 2, D, 2 * DFF))
    sh["w_ff_out"] = np.ascontiguousarray(inp["w_ff_out"][:L], dtype=f32)
    wmi = np.asarray(inp["w_mix_in"][:L], dtype=f32)
    q = wmi[:, :, 0:512].reshape(L, D, 8, 64)
    qperm = np.stack([q[:, :, [j, 4 + j], :].reshape(L, D, 128) for j in range(4)], axis=2).reshape(L, D, 512)
    kc, vc, ks, vs, kw, vw = (wmi[:, :, 512 + i * 128:640 + i * 128] for i in range(6))
    gates = wmi[:, :, 1280:1304]
    hc = wmi[:, :, 1304:1816].reshape(L, D, 4, 128)
    bg = wmi[:, :, 1816:2328].reshape(L, D, 4, 128)
    cg = wmi[:, :, 2328:2840].reshape(L, D, 4, 128)
    conv = np.stack([hc, bg, cg], axis=3).reshape(L, D, 1536)
    sh["w_mix_in"] = np.ascontiguousarray(np.concatenate([qperm, kc, vc, ks, kw, conv, vs, vw, gates], axis=-1))
    assert sh["w_mix_in"].shape[-1] == 2840
    sh["w_mix_out"] = np.ascontiguousarray(inp["w_mix_out"][:L], dtype=f32)
    cw = np.zeros((L, 128, CMPW), f32)
    for l in range(L):
        for kind in range(2):
            w1 = np.asarray(inp["w_cmp1"][l, kind], f32).reshape(32, 64, 64).transpose(1, 0, 2).reshape(64, 2048)
            cw[l, :, kind * 2048:(kind + 1) * 2048] = np.concatenate([w1, w1], axis=0)
            pos = np.asarray(inp["cmp_pos"][l, kind], f32).T
            cw[l, :, 4096 + kind * 34:4096 + kind * 34 + 32] = np.concatenate([pos, pos], axis=0)
        cw[l, 0:64, 4164:4228] = inp["w_cmp2"][l, 0]
        cw[l, 0:64, 4228:4292] = inp["w_cmp2"][l, 1]
    sh["cmpw"] = cw
    cb, fb = _consts()
    sh["cbf"] = cb
    return sh, fb


def _prep_pf(L, NSEQ, inp, fb, c_rows):
    PO = pf_offsets(L, NSEQ)
    pf = np.zeros((128, PO["_n"]), np.float32)

    def put(name, arr):
        a = np.asarray(arr, np.float32)
        a = a.reshape(-1, a.shape[-1] // 128, 128)
        a = a.transpose(2, 0, 1).reshape(128, -1)
        pf[:, PO[name]:PO[name] + a.shape[1]] = a

    put("g", inp["g_norm"][:L])
    put("b", inp["b_ada"][:L])
    put("cw", inp["conv_w"][:L])
    put("cb", inp["conv_b"][:L])
    put("gmo", inp["g_mix_out"][:L])
    put("gf", inp["g_final"])
    c = np.asarray(c_rows, np.float32)
    pf[:, PO["c"]:PO["c"] + 8 * NSEQ] = c.reshape(NSEQ, 8, 128).transpose(2, 1, 0).reshape(128, 8 * NSEQ)
    pf[:, PO["eps"]] = EPS
    pf[:, PO["fb"]:PO["fb"] + 512] = fb
    return pf


def run(inp, L=4, NSEQ=2, ncores=NCORES, stop=None, trace=False):
    sh, fb = _prep_shared(L, inp)
    x = np.asarray(inp["x"], np.float32)
    c = np.asarray(inp["c"], np.float32)
    in_maps = []
    for r in range(ncores):
        m = dict(sh)
        xs = x[r * NSEQ:(r + 1) * NSEQ]
        m["xT"] = np.ascontiguousarray(xs.transpose(0, 2, 1))
        m["pf32"] = _prep_pf(L, NSEQ, inp, fb, c[r * NSEQ:(r + 1) * NSEQ])
        in_maps.append(m)
    nc = build_program(L=L, NSEQ=NSEQ, stop=stop)
    res = run_bass_kernel_spmd(nc, in_maps, core_ids=list(range(ncores)), **({"trace": True} if trace else {}))
    out = np.concatenate([np.asarray(r["yT"]).transpose(0, 2, 1) for r in res.results], axis=0)
    return np.ascontiguousarray(out.astype(np.float32)), res


def kernel(x, c, w_ada, b_ada, g_norm, w_ff_in, w_ff_out, w_mix_in, cmp_pos, w_cmp1, w_cmp2, conv_w, conv_b,
           g_mix_out, w_mix_out, g_final):
    inp = dict(x=x, c=c, w_ada=w_ada, b_ada=b_ada, g_norm=g_norm, w_ff_in=w_ff_in, w_ff_out=w_ff_out,
               w_mix_in=w_mix_in, cmp_pos=cmp_pos, w_cmp1=w_cmp1, w_cmp2=w_cmp2, conv_w=conv_w, conv_b=conv_b,
               g_mix_out=g_mix_out, w_mix_out=w_mix_out, g_final=g_final)
    inp = {k: np.asarray(v) for k, v in inp.items()}
    out, _ = run(inp, L=4, NSEQ=2, ncores=NCORES)
    return out
```
